# Optimizing a Trainium2 kernel written in Bass

```python
import math
import jax
import jax.numpy as jnp
from jax import lax
import numpy as np

D_MODEL = 1024
BATCH = 8
SEQ = 4096
DEPTH = 2

GRID_W = 64
CTX_LEN = 256
EPS = 1e-6
NEG_INF = -1e30

POOL_GROUPS = 4
POOL_GROUP_DIM = 128
POOL_DIM = POOL_GROUPS * POOL_GROUP_DIM
POOL_WINDOWS = (2, 4, 8, 16)
HEAD_DIM = 64
N_HEADS = 8
N_KV_HEADS = 2
GQA_GROUP = N_HEADS // N_KV_HEADS
Q_DIM = N_HEADS * HEAD_DIM
KV_DIM = N_KV_HEADS * HEAD_DIM
AB_IN_DIM = POOL_DIM + Q_DIM + 2 * KV_DIM
AB_OUT_DIM = POOL_DIM + Q_DIM
WINDOW = 128
BLOCK = 128
ROPE_THETA = 10000.0

GDN_HEADS = 8
GDN_HEAD_DIM = 128
GDN_DIM = GDN_HEADS * GDN_HEAD_DIM
CONV_K = 5
CHUNK = 64
GDN_IN_DIM = 4 * GDN_DIM + 4 * GDN_HEADS

D_FF = 2816
N_EXPERTS = 8
TOP_K = 2
D_EXPERT = 3584

kernel_name = 'hybrid_pool_swa_gdn_moe_dit'


def rmsnorm(x, g):
    xf = x.astype(jnp.float32)
    y = xf * lax.rsqrt(jnp.mean(xf * xf, axis=-1, keepdims=True) + EPS)
    return y.astype(x.dtype) * g


def l2norm(x):
    xf = x.astype(jnp.float32)
    return (xf * lax.rsqrt(jnp.sum(xf * xf, axis=-1, keepdims=True) + EPS)).astype(x.dtype)


def swiglu(h, w1, w3, w2):
    return (jax.nn.silu(h @ w1) * (h @ w3)) @ w2


def multiscale_pool(u, pool_w, pool_scale):
    B, L, _ = u.shape
    cs = jnp.cumsum(u.astype(jnp.float32), axis=1)
    cs = jnp.concatenate([jnp.zeros_like(cs[:, :1]), cs], axis=1)
    t = jnp.arange(L)
    means = []
    for gi, w in enumerate(POOL_WINDOWS):
        lo = jnp.clip(t - w // 2, 0, L)
        hi = jnp.clip(t + w // 2, 0, L)
        seg = cs[:, :, gi * POOL_GROUP_DIM:(gi + 1) * POOL_GROUP_DIM]
        means.append((seg[:, hi] - seg[:, lo]) / (hi - lo).astype(jnp.float32)[None, :, None])
    pooled = jnp.stack(means, axis=2)
    delta = (pooled - u.reshape(B, L, POOL_GROUPS, POOL_GROUP_DIM).astype(jnp.float32)).astype(u.dtype)
    y = jnp.einsum('blgc,gcd->blgd', delta, pool_w) * pool_scale.reshape(POOL_GROUPS, POOL_GROUP_DIM)
    return y.reshape(B, L, POOL_DIM)


def axial_rope(x, rows, cols):
    half = HEAD_DIM // 2
    inv = ROPE_THETA ** (-jnp.arange(0, half, 2, dtype=jnp.float32) / half)

    def rot(xa, pos):
        ang = pos[:, None] * inv[None, :]
        cos = jnp.cos(ang)[None, :, None, :].astype(x.dtype)
        sin = jnp.sin(ang)[None, :, None, :].astype(x.dtype)
        x1, x2 = xa[..., :half // 2], xa[..., half // 2:]
        return jnp.concatenate([x1 * cos - x2 * sin, x2 * cos + x1 * sin], axis=-1)

    return jnp.concatenate([rot(x[..., :half], rows), rot(x[..., half:], cols)], axis=-1)


def split_ab(p):
    B, L, _ = p.shape
    u = p[..., :POOL_DIM]
    q = p[..., POOL_DIM:POOL_DIM + Q_DIM].reshape(B, L, N_HEADS, HEAD_DIM)
    k = p[..., POOL_DIM + Q_DIM:POOL_DIM + Q_DIM + KV_DIM].reshape(B, L, N_KV_HEADS, HEAD_DIM)
    v = p[..., POOL_DIM + Q_DIM + KV_DIM:].reshape(B, L, N_KV_HEADS, HEAD_DIM)
    return u, q, k, v


def banded_attention(q, k, v, k_ctx, v_ctx, sinks):
    B, L, _, _ = q.shape
    Lc = k_ctx.shape[1]
    nb = L // BLOCK
    scale = HEAD_DIM ** -0.5
    qb = q.reshape(B, nb, BLOCK, N_KV_HEADS, GQA_GROUP, HEAD_DIM)
    pad = ((0, 0), (BLOCK, BLOCK), (0, 0), (0, 0))
    kp = jnp.pad(k, pad).reshape(B, nb + 2, BLOCK, N_KV_HEADS, HEAD_DIM)
    vp = jnp.pad(v, pad).reshape(B, nb + 2, BLOCK, N_KV_HEADS, HEAD_DIM)
    k_band = jnp.concatenate([kp[:, :-2], kp[:, 1:-1], kp[:, 2:]], axis=2)
    v_band = jnp.concatenate([vp[:, :-2], vp[:, 1:-1], vp[:, 2:]], axis=2)
    s_band = jnp.einsum('bnqhgd,bnkhd->bnhgqk', qb, k_band, preferred_element_type=jnp.float32) * scale
    qpos = jnp.arange(nb)[:, None] * BLOCK + jnp.arange(BLOCK)[None, :]
    kpos = (jnp.arange(nb)[:, None] - 1) * BLOCK + jnp.arange(3 * BLOCK)[None, :]
    allowed = ((jnp.abs(kpos[:, None, :] - qpos[:, :, None]) <= WINDOW)
               & (kpos[:, None, :] >= 0) & (kpos[:, None, :] < L))
    s_band = jnp.where(allowed[None, :, None, None], s_band, NEG_INF)
    s_ctx = jnp.einsum('bnqhgd,bkhd->bnhgqk', qb, k_ctx, preferred_element_type=jnp.float32) * scale
    sink = sinks.astype(jnp.float32).reshape(N_KV_HEADS, GQA_GROUP)
    s_sink = jnp.broadcast_to(sink[None, None, :, :, None, None], s_ctx.shape[:-1] + (1,))
    p = jax.nn.softmax(jnp.concatenate([s_sink, s_ctx, s_band], axis=-1), axis=-1)
    p_ctx = p[..., 1:1 + Lc].astype(v.dtype)
    p_band = p[..., 1 + Lc:].astype(v.dtype)
    o = (jnp.einsum('bnhgqk,bkhd->bnqhgd', p_ctx, v_ctx)
         + jnp.einsum('bnhgqk,bnkhd->bnqhgd', p_band, v_band))
    return o.reshape(B, L, Q_DIM)


def context_attention(q, k, v, sinks):
    B, Lc, _, _ = q.shape
    qc = q.reshape(B, Lc, N_KV_HEADS, GQA_GROUP, HEAD_DIM)
    s = jnp.einsum('bqhgd,bkhd->bhgqk', qc, k, preferred_element_type=jnp.float32) * HEAD_DIM ** -0.5
    sink = sinks.astype(jnp.float32).reshape(N_KV_HEADS, GQA_GROUP)
    s_sink = jnp.broadcast_to(sink[None, :, :, None, None], s.shape[:-1] + (1,))
    p = jax.nn.softmax(jnp.concatenate([s_sink, s], axis=-1), axis=-1)[..., 1:].astype(v.dtype)
    o = jnp.einsum('bhgqk,bkhd->bqhgd', p, v)
    return o.reshape(B, Lc, Q_DIM)


def pool_swa_mixer(h_lat, h_ctx, w_in, pool_w, pool_scale, sinks, w_out, ctx_out):
    L = h_lat.shape[1]
    rows_n = L // GRID_W
    rows = jnp.repeat(jnp.arange(rows_n, dtype=jnp.float32), GRID_W)
    cols = jnp.tile(jnp.arange(GRID_W, dtype=jnp.float32), rows_n)
    u_l, q_l, k_l, v_l = split_ab(h_lat @ w_in)
    u_c, q_c, k_c, v_c = split_ab(h_ctx @ w_in)
    q_l = axial_rope(q_l, rows, cols)
    k_l = axial_rope(k_l, rows, cols)
    a_l = banded_attention(q_l, k_l, v_l, k_c, v_c, sinks)
    y_lat = jnp.concatenate([multiscale_pool(u_l, pool_w, pool_scale), a_l], axis=-1) @ w_out
    if not ctx_out:
        return y_lat, None
    a_c = context_attention(q_c, k_c, v_c, sinks)
    y_ctx = jnp.concatenate([multiscale_pool(u_c, pool_w, pool_scale), a_c], axis=-1) @ w_out
    return y_lat, y_ctx


def centred_dwconv(x, w):
    return lax.conv_general_dilated(x, w[:, None, :], window_strides=(1,),
                                    padding=[(CONV_K // 2, CONV_K // 2)],
                                    dimension_numbers=('NWC', 'WIO', 'NWC'),
                                    feature_group_count=x.shape[-1])


def gated_delta_chunked(q, k, v, g, beta, s0):
    B, H, L, _ = q.shape
    DV = v.shape[-1]
    n = L // CHUNK

    def chunks(t):
        return t.astype(jnp.float32).reshape((B, H, n, CHUNK) + t.shape[3:])

    q, k, v, g, beta = chunks(q), chunks(k), chunks(v), chunks(g), chunks(beta)
    gam = jnp.cumsum(g, axis=-1)
    idx = jnp.arange(CHUNK)
    incl = idx[:, None] >= idx[None, :]
    strict = idx[:, None] > idx[None, :]
    diff = gam[..., :, None] - gam[..., None, :]
    decay = jnp.where(incl, jnp.exp(jnp.where(incl, diff, 0.0)), 0.0)
    kb = k * beta[..., None]
    m = jnp.where(strict, jnp.einsum('bhnid,bhnjd->bhnij', kb, k) * decay, 0.0)
    a = m + jnp.eye(CHUNK, dtype=jnp.float32)
    u = lax.linalg.triangular_solve(a, v * beta[..., None], left_side=True, lower=True, unit_diagonal=True)
    w = lax.linalg.triangular_solve(a, kb * jnp.exp(gam)[..., None], left_side=True, lower=True,
                                    unit_diagonal=True)
    qk = jnp.einsum('bhnid,bhnjd->bhnij', q, k) * decay
    qd = q * jnp.exp(gam)[..., None]
    kd = k * jnp.exp(gam[..., -1:] - gam)[..., None]
    cd = jnp.exp(gam[..., -1])
    xs = tuple(jnp.moveaxis(t, 2, 0) for t in (u, w, qk, qd, kd, cd))

    def step(s, inp):
        u_c, w_c, qk_c, qd_c, kd_c, cd_c = inp
        v_new = u_c - jnp.einsum('bhik,bhkv->bhiv', w_c, s)
        o_c = jnp.einsum('bhik,bhkv->bhiv', qd_c, s) + jnp.einsum('bhij,bhjv->bhiv', qk_c, v_new)
        s = s * cd_c[..., None, None] + jnp.einsum('bhik,bhiv->bhkv', kd_c, v_new)
        return s, o_c

    s_fin, o = lax.scan(step, s0, xs)
    o = jnp.moveaxis(o, 0, 2).reshape(B, H, L, DV)
    return o, s_fin


def gdn_project(h, w_in, conv_w, a_log, dt_bias):
    B, L, _ = h.shape
    p = h @ w_in
    qkv = jax.nn.silu(centred_dwconv(p[..., :3 * GDN_DIM], conv_w))

    def heads(t):
        return t.reshape(B, L, GDN_HEADS, GDN_HEAD_DIM).transpose(0, 2, 1, 3)

    q = l2norm(heads(qkv[..., :GDN_DIM])) * (GDN_HEAD_DIM ** -0.5)
    k = l2norm(heads(qkv[..., GDN_DIM:2 * GDN_DIM]))
    v = heads(qkv[..., 2 * GDN_DIM:])
    z = p[..., 3 * GDN_DIM:4 * GDN_DIM].reshape(B, L, GDN_HEADS, GDN_HEAD_DIM)
    ab = p[..., 4 * GDN_DIM:].astype(jnp.float32).reshape(B, L, 2, 2, GDN_HEADS)
    g = -jnp.exp(a_log.astype(jnp.float32)) * jax.nn.softplus(ab[:, :, :, 0] + dt_bias.astype(jnp.float32))
    beta = jax.nn.sigmoid(ab[:, :, :, 1])
    g = jnp.transpose(g, (2, 0, 3, 1))
    beta = jnp.transpose(beta, (2, 0, 3, 1))
    return q, k, v, z, g, beta


def gdn_output(o, z, out_norm, w_out):
    B, H, L, DV = o.shape
    o = jnp.transpose(o, (0, 2, 1, 3))
    y = rmsnorm(o, out_norm.astype(jnp.float32)) * jax.nn.silu(z.astype(jnp.float32))
    return y.astype(z.dtype).reshape(B, L, H * DV) @ w_out


def gdn_mixer(h_lat, h_ctx, w_in, conv_w, a_log, dt_bias, out_norm, w_out, ctx_out):
    ql, kl, vl, zl, gl, bl = gdn_project(h_lat, w_in, conv_w, a_log, dt_bias)
    qc, kc, vc, zc, gc, bc = gdn_project(h_ctx, w_in, conv_w, a_log, dt_bias)
    B = h_lat.shape[0]
    s0 = jnp.zeros((B, GDN_HEADS, GDN_HEAD_DIM, GDN_HEAD_DIM), jnp.float32)
    outs_l, outs_c = [], []
    for d in range(2):
        rev = (lambda t: jnp.flip(t, axis=2)) if d == 1 else (lambda t: t)
        oc, sc = gated_delta_chunked(rev(qc), rev(kc), rev(vc), rev(gc[d]), rev(bc[d]), s0)
        ol, _ = gated_delta_chunked(rev(ql), rev(kl), rev(vl), rev(gl[d]), rev(bl[d]), sc)
        outs_l.append(rev(ol))
        outs_c.append(rev(oc))
    y_lat = gdn_output(outs_l[0] + outs_l[1], zl, out_norm, w_out)
    if not ctx_out:
        return y_lat, None
    y_ctx = gdn_output(outs_c[0] + outs_c[1], zc, out_norm, w_out)
    return y_lat, y_ctx


def moe_swiglu(h, router, w1, w3, w2):
    shp = h.shape
    t = h.reshape(-1, shp[-1])
    logits = jnp.matmul(t, router, preferred_element_type=jnp.float32)
    top_v, top_i = lax.top_k(logits, TOP_K)
    gates = jax.nn.softmax(top_v, axis=-1)
    combine = jnp.sum(jax.nn.one_hot(top_i, N_EXPERTS, dtype=jnp.float32) * gates[..., None], axis=1)
    y = jnp.zeros(t.shape, jnp.float32)
    for e in range(N_EXPERTS):
        y = y + combine[:, e:e + 1] * swiglu(t, w1[e], w3[e], w2[e]).astype(jnp.float32)
    return y.astype(h.dtype).reshape(shp)


def run_layer(layer, x_lat, x_ctx, c, c_ctx, p, last):
    mod = jax.nn.silu(c) @ p['mod_w'] + p['mod_b']
    mod_c = jax.nn.silu(c_ctx) @ p['mod_w'] + p['mod_b']
    sh_m, sc_m, g_m, sh_f, sc_f, g_f = [t[:, None, :] for t in jnp.split(mod, 6, axis=-1)]
    csh_m, csc_m, cg_m, csh_f, csc_f, cg_f = jnp.split(mod_c, 6)
    h_lat = rmsnorm(x_lat, p['mix_pre']) * (1.0 + sc_m) + sh_m
    h_ctx = rmsnorm(x_ctx, p['mix_pre']) * (1.0 + csc_m) + csh_m
    if layer % 2 == 0:
        y_lat, y_ctx = pool_swa_mixer(h_lat, h_ctx, p['w_in'], p['pool_w'], p['pool_scale'], p['sinks'],
                                      p['w_out'], not last)

        def channel_mixer(h):
            return swiglu(h, p['ffn_w1'], p['ffn_w3'], p['ffn_w2'])
    else:
        y_lat, y_ctx = gdn_mixer(h_lat, h_ctx, p['w_in'], p['conv_w'], p['a_log'], p['dt_bias'],
                                 p['out_norm'], p['w_out'], not last)

        def channel_mixer(h):
            return moe_swiglu(h, p['router'], p['moe_w1'], p['moe_w3'], p['moe_w2'])
    x_lat = x_lat + g_m * rmsnorm(y_lat, p['mix_post'])
    h = rmsnorm(x_lat, p['ffn_pre']) * (1.0 + sc_f) + sh_f
    x_lat = x_lat + g_f * rmsnorm(channel_mixer(h), p['ffn_post'])
    if not last:
        x_ctx = x_ctx + cg_m * rmsnorm(y_ctx, p['mix_post'])
        hc = rmsnorm(x_ctx, p['ffn_pre']) * (1.0 + csc_f) + csh_f
        x_ctx = x_ctx + cg_f * rmsnorm(channel_mixer(hc), p['ffn_post'])
    return x_lat, x_ctx


def setup_inputs(seed: int = 0) -> dict:
    key = jax.random.key(seed)
    ks = jax.random.split(key, 40)
    f32 = jnp.float32
    D = D_MODEL

    def nrm(i, shape, scale):
        return jax.random.normal(ks[i], shape, f32) * scale

    def gain(i, n):
        return 1.0 + 0.05 * jax.random.normal(ks[i], (n,), f32)

    dt = jnp.exp(jax.random.uniform(ks[27], (2, GDN_HEADS), f32, minval=math.log(1e-3), maxval=math.log(1e-1)))
    dt_bias = dt + jnp.log(-jnp.expm1(-dt))
    return {
        'x': nrm(0, (BATCH, SEQ, D), 1.0),
        'c': nrm(1, (BATCH, D), 1.0),
        'ctx': nrm(2, (BATCH, CTX_LEN, D), 1.0),
        'c_ctx': nrm(3, (D,), 1.0),
        'l0_mod_w': nrm(4, (D, 6 * D), 0.5 * D ** -0.5),
        'l0_mod_b': nrm(5, (6 * D,), 0.02),
        'l0_mix_pre': gain(6, D),
        'l0_mix_post': gain(7, D),
        'l0_ffn_pre': gain(8, D),
        'l0_ffn_post': gain(9, D),
        'l0_w_in': nrm(10, (D, AB_IN_DIM), D ** -0.5),
        'l0_pool_w': nrm(11, (POOL_GROUPS, POOL_GROUP_DIM, POOL_GROUP_DIM), POOL_GROUP_DIM ** -0.5),
        'l0_pool_scale': gain(12, POOL_DIM),
        'l0_sinks': nrm(13, (N_HEADS,), 0.5),
        'l0_w_out': nrm(14, (AB_OUT_DIM, D), AB_OUT_DIM ** -0.5),
        'l0_ffn_w1': nrm(15, (D, D_FF), D ** -0.5),
        'l0_ffn_w3': nrm(16, (D, D_FF), D ** -0.5),
        'l0_ffn_w2': nrm(17, (D_FF, D), D_FF ** -0.5),
        'l1_mod_w': nrm(18, (D, 6 * D), 0.5 * D ** -0.5),
        'l1_mod_b': nrm(19, (6 * D,), 0.02),
        'l1_mix_pre': gain(20, D),
        'l1_mix_post': gain(21, D),
        'l1_ffn_pre': gain(22, D),
        'l1_ffn_post': gain(23, D),
        'l1_w_in': nrm(24, (D, GDN_IN_DIM), D ** -0.5),
        'l1_conv_w': nrm(25, (CONV_K, 3 * GDN_DIM), CONV_K ** -0.5),
        'l1_a_log': jnp.log(jax.random.uniform(ks[26], (2, GDN_HEADS), f32, minval=1.0, maxval=16.0)),
        'l1_dt_bias': dt_bias,
        'l1_out_norm': gain(28, GDN_HEAD_DIM),
        'l1_w_out': nrm(29, (GDN_DIM, D), GDN_DIM ** -0.5),
        'l1_router': nrm(30, (D, N_EXPERTS), D ** -0.5),
        'l1_moe_w1': nrm(31, (N_EXPERTS, D, D_EXPERT), D ** -0.5),
        'l1_moe_w3': nrm(32, (N_EXPERTS, D, D_EXPERT), D ** -0.5),
        'l1_moe_w2': nrm(33, (N_EXPERTS, D_EXPERT, D), D_EXPERT ** -0.5),
    }


def reference(x, c, ctx, c_ctx,
              l0_mod_w, l0_mod_b, l0_mix_pre, l0_mix_post, l0_ffn_pre, l0_ffn_post,
              l0_w_in, l0_pool_w, l0_pool_scale, l0_sinks, l0_w_out, l0_ffn_w1, l0_ffn_w3, l0_ffn_w2,
              l1_mod_w, l1_mod_b, l1_mix_pre, l1_mix_post, l1_ffn_pre, l1_ffn_post,
              l1_w_in, l1_conv_w, l1_a_log, l1_dt_bias, l1_out_norm, l1_w_out,
              l1_router, l1_moe_w1, l1_moe_w3, l1_moe_w2):
    layers = [
        dict(mod_w=l0_mod_w, mod_b=l0_mod_b, mix_pre=l0_mix_pre, mix_post=l0_mix_post,
             ffn_pre=l0_ffn_pre, ffn_post=l0_ffn_post, w_in=l0_w_in, pool_w=l0_pool_w,
             pool_scale=l0_pool_scale, sinks=l0_sinks, w_out=l0_w_out,
             ffn_w1=l0_ffn_w1, ffn_w3=l0_ffn_w3, ffn_w2=l0_ffn_w2),
        dict(mod_w=l1_mod_w, mod_b=l1_mod_b, mix_pre=l1_mix_pre, mix_post=l1_mix_post,
             ffn_pre=l1_ffn_pre, ffn_post=l1_ffn_post, w_in=l1_w_in, conv_w=l1_conv_w,
             a_log=l1_a_log, dt_bias=l1_dt_bias, out_norm=l1_out_norm, w_out=l1_w_out,
             router=l1_router, moe_w1=l1_moe_w1, moe_w3=l1_moe_w3, moe_w2=l1_moe_w2),
    ]
    x_lat, x_ctx = x, ctx
    for layer in range(DEPTH):
        x_lat, x_ctx = run_layer(layer, x_lat, x_ctx, c, c_ctx, layers[layer], layer == DEPTH - 1)
    return x_lat
```

```python
import os
import numpy as np
from contextlib import ExitStack
import concourse.bass as bass
import concourse.mybir as mybir
from concourse.bass_utils import run_bass_kernel_spmd

F32 = mybir.dt.float32
BF16 = mybir.dt.bfloat16
AF = mybir.ActivationFunctionType
ALU = mybir.AluOpType
AX = mybir.AxisListType

D = 1024
LAT = 4096
CTX = 256
TT = LAT + CTX
EPS = 1e-6
NCORES = 8


class Tr:
    __slots__ = ("w", "r", "x")

    def __init__(self, x=False):
        self.w = None
        self.r = {}
        self.x = x


class Prog:
    NDMA = 24

    def __init__(self, nc):
        self.nc = nc
        self.es = ExitStack()
        self.eng = {"pe": nc.tensor, "act": nc.scalar, "dve": nc.vector, "pool": nc.gpsimd, "sp": nc.sync}
        self.semh = {}
        for k in self.eng:
            self.semh[k] = self.es.enter_context(nc.semaphore("s_" + k))
        self.cnt = {k: 0 for k in self.eng}
        self.epoch = {k: 0 for k in self.eng}
        self.ekey = {k: k for k in self.eng}
        self.hist = []
        self.waited = {k: {} for k in self.eng}
        self.dcnt = [0] * self.NDMA
        self.dnext = 0
        for i in range(self.NDMA):
            self.semh["d%d" % i] = self.es.enter_context(nc.semaphore("sd%d" % i))
        self.nps = 0
        self.psl = []
        for i in range(8):
            t = self.es.enter_context(nc.psum_tensor("ps%d" % i, [128, 512], F32))
            self.psl.append((t, Tr(True)))
        self.n_ins = 0

    def ps(self, banks, ctr):
        t = self.psl[banks[ctr[0] % len(banks)]]
        ctr[0] += 1
        return t

    def _wait(self, e, deps):
        best = {}
        for d in deps:
            if d is None:
                continue
            k, v = d
            if v > best.get(k, 0):
                best[k] = v
        for k, v in best.items():
            if e == "pe" and (k == "pe" or k.startswith("pe#")):
                continue
            if self.waited[e].get(k, 0) >= v:
                continue
            self.eng[e].wait_ge(self.semh[k], v)
            self.waited[e][k] = v

    @staticmethod
    def _deps(reads, writes):
        deps = []
        for t in reads:
            if t.w is not None:
                deps.append(t.w)
            if t.x:
                deps.extend(t.r.items())
        for t in writes:
            if t.w is not None:
                deps.append(t.w)
            deps.extend(t.r.items())
        return deps

    @staticmethod
    def _mark(me, reads, writes):
        k, v = me
        for t in reads:
            if t.r.get(k, 0) < v:
                t.r[k] = v
        for t in writes:
            t.w = me
            t.r = {}

    EPOCH = 12000

    def op(self, e, fn, reads=(), writes=()):
        if self.cnt[e] >= self.EPOCH:
            self.epoch[e] += 1
            self.ekey[e] = "%s#%d" % (e, self.epoch[e])
            self.semh[self.ekey[e]] = self.es.enter_context(self.nc.semaphore("s_%s_%d" % (e, self.epoch[e])))
            self.cnt[e] = 0
        self._wait(e, self._deps(reads, writes))
        ins = fn(self.eng[e])
        self.cnt[e] += 1
        key = self.ekey[e]
        ins.then_inc(self.semh[key], 1)
        self._mark((key, self.cnt[e]), reads, writes)
        self.n_ins += 1
        return ins

    def dma(self, q, out, in_, reads=(), writes=()):
        slot = self.dnext
        self.dnext = (slot + 1) % self.NDMA
        deps = self._deps(reads, writes)
        key = "d%d" % slot
        if self.dcnt[slot] > 0:
            deps.append((key, 16 * self.dcnt[slot]))
        self._wait(q, deps)
        ins = self.eng[q].dma_start(out=out, in_=in_)
        self.dcnt[slot] += 1
        ins.then_inc(self.semh[key], 16)
        self._mark((key, 16 * self.dcnt[slot]), reads, writes)
        self.n_ins += 1
        return ins

    def barrier(self):
        deps = [(self.ekey[k], self.cnt[k]) for k in self.eng if self.cnt[k] > 0]
        deps += [("d%d" % i, 16 * self.dcnt[i]) for i in range(self.NDMA) if self.dcnt[i] > 0]
        for e in self.eng:
            best = {}
            for k, v in deps:
                best[k] = v
            for k, v in best.items():
                if self.waited[e].get(k, 0) >= v:
                    continue
                self.eng[e].wait_ge(self.semh[k], v)
                self.waited[e][k] = v
        for (t, tr) in self.psl:
            tr.w = None
            tr.r = {}


def _rope_tables():
    half = 32
    inv = 10000.0 ** (-np.arange(0, half, 2, dtype=np.float32) / half)
    t = np.arange(LAT)
    rows = (t // 64).astype(np.float32)
    cols = (t % 64).astype(np.float32)
    cos = np.zeros((64, LAT), np.float32)
    sin = np.zeros((64, LAT), np.float32)
    for i in range(64):
        pos = rows if i < 32 else cols
        ang = pos * inv[i % 16]
        cos[i] = np.cos(ang)
        sin[i] = np.sin(ang)
    cos = np.concatenate([cos, cos], 0)
    sin = np.concatenate([sin, sin], 0)
    return cos, sin


def _rot_lhsT():
    R = np.zeros((128, 128), np.float32)
    for hb in (0, 64):
        for blk in (0, 32):
            for i in range(16):
                R[hb + blk + i, hb + blk + i + 16] = -1.0
                R[hb + blk + i + 16, hb + blk + i] = 1.0
    return np.ascontiguousarray(R.T)


def _pool_invcnt(L):
    t = np.arange(L)
    out = np.zeros((4, L), np.float32)
    for gi, w in enumerate((2, 4, 8, 16)):
        lo = np.clip(t - w // 2, 0, L)
        hi = np.clip(t + w // 2, 0, L)
        out[gi] = 1.0 / (hi - lo).astype(np.float32)
    return out


def _colvec(v):
    return np.ascontiguousarray(np.asarray(v, np.float32).reshape(-1, 128).T)


class VecPack:
    def __init__(self):
        self.cols = []
        self.off = {}
        self.n = 0

    def add(self, name, arr):
        arr = np.asarray(arr, np.float32)
        assert arr.shape[0] == 128
        self.off[name] = self.n
        self.cols.append(arr)
        self.n += arr.shape[1]

    def pack(self):
        return np.ascontiguousarray(np.concatenate(self.cols, axis=1))


def vec_layout(inp=None):
    z = lambda *s: np.zeros(s, np.float32)
    g = (lambda k, shp: np.asarray(inp[k], np.float32)) if inp is not None else (lambda k, shp: z(*shp))
    vp = VecPack()
    for l in (0, 1):
        for nm in ("mix_pre", "mix_post", "ffn_pre", "ffn_post"):
            vp.add("l%d_%s" % (l, nm), _colvec(g("l%d_%s" % (l, nm), (1024,))))
        vp.add("l%d_mod_b" % l, _colvec(g("l%d_mod_b" % l, (6144,))))
    vp.add("pool_scale", _colvec(g("l0_pool_scale", (512,))))
    sk = np.asarray(g("l0_sinks", (8,)), np.float32).reshape(1, 8)
    vp.add("sinks", np.broadcast_to(sk, (128, 8)))
    cw = g("l1_conv_w", (5, 3072))
    vp.add("conv_w", np.concatenate([_colvec(cw[j]) for j in range(5)], axis=1))
    vp.add("out_norm", np.asarray(g("l1_out_norm", (128,)), np.float32).reshape(128, 1))
    al = g("l1_a_log", (2, 8)).reshape(1, 16)
    db = g("l1_dt_bias", (2, 8)).reshape(1, 16)
    vp.add("a_log", np.broadcast_to(al, (128, 16)))
    vp.add("dt_bias", np.broadcast_to(db, (128, 16)))
    return vp


TILES512 = [(0, 256)] + [(256 + 512 * i, 512) for i in range(8)]
TILES256 = [(256 * i, 256) for i in range(17)]
NBLK = TT // 128


def build(upto="all", dbg=()):
    nc = bass.Bass("TRN2", target_bir_lowering=False)
    P = Prog(nc)
    es = P.es
    VO = vec_layout(None).off
    NV = vec_layout(None).n

    def din(name, shape, dt=F32):
        return nc.dram_tensor(name, list(shape), dt, kind="ExternalInput").ap()

    def dscr(name, shape, dt=F32):
        kind = "ExternalOutput" if name in dbg else "Internal"
        return nc.dram_tensor(name, list(shape), dt, kind=kind).ap()

    xin = din("xin", [8, 128, TT])
    cT = din("cT", [128, 8, 2])
    vecs = din("vecs", [128, NV])
    modw = [din("l%d_mod_w" % l, [128, 8, 6144]) for l in (0, 1)]
    w0_in = din("w0_in", [128, 8, 1280])
    pool_w = din("pool_w", [128, 4, 128])
    w0_outp = din("w0_outp", [128, 4, 1024])
    w0_outa = din("w0_outa", [64, 8, 1024])
    ffn_w1 = din("ffn_w1", [128, 8, 2816])
    ffn_w3 = din("ffn_w3", [128, 8, 2816])
    ffn_w2 = din("ffn_w2", [128, 22, 1024])
    rope_cos = din("rope_cos", [64, LAT])
    rope_sin = din("rope_sin", [64, LAT])
    rotT = din("rotT", [64, 64])
    masks = din("masks", [128, 2, 512])
    invc_lat = din("invc_lat", [128, 4, LAT])
    invc_ctx = din("invc_ctx", [128, 4, CTX])
    identf = din("identf", [128, 128])
    outT = nc.dram_tensor("outT", [8, 128, LAT], F32, kind="ExternalOutput").ap()

    UT = dscr("UT", [4, 128, TT])
    XM = dscr("XM", [8, 128, TT])
    X1 = dscr("X1", [8, 128, TT])

    def sb(name, shape, dt=F32, stack=None):
        t = (stack or es).enter_context(nc.sbuf_tensor(name, list(shape), dt))
        return t, Tr()

    def ACT(out, in_, func, reads, writes, **kw):
        return P.op("act", lambda e: e.activation(out=out, in_=in_, func=func, **kw), reads, writes)

    def TTo(out, a, b, op, reads, writes, eng="dve"):
        return P.op(eng, lambda e: e.tensor_tensor(out=out, in0=a, in1=b, op=op), reads, writes)

    def STT(out, in0, scalar, in1, op0, op1, reads, writes):
        return P.op("dve", lambda e: e.scalar_tensor_tensor(out=out, in0=in0, scalar=scalar, in1=in1, op0=op0, op1=op1),
                    reads, writes)

    def TS(out, in0, s1, s2, op0, op1, reads, writes):
        return P.op("dve", lambda e: e.tensor_scalar(out=out, in0=in0, scalar1=s1, scalar2=s2, op0=op0, op1=op1),
                    reads, writes)

    def CP(out, in_, reads, writes, eng="dve"):
        return P.op(eng, lambda e: e.tensor_copy(out=out, in_=in_), reads, writes)

    def MM(ps, lhsT, rhs, start, stop, reads, pst):
        return P.op("pe", lambda e: e.matmul(ps, lhsT, rhs, start=start, stop=stop), reads, [pst])

    V, Vt = sb("V", [128, NV])
    DV, DVt = sb("DV", [128, 2 * 2 * 48])
    ones_bf, ones_t = sb("ones_bf", [128, 128], BF16)
    ones_f, onesf_t = sb("ones_f", [128, 128], F32)
    epsc, eps_t = sb("epsc", [128, 1])
    identF, identF_t = sb("identF", [128, 128], F32)
    identB, identB_t = sb("identB", [128, 128], BF16)
    P.op("dve", lambda e: e.memset(ones_bf[:], 1.0), writes=[ones_t])
    P.op("dve", lambda e: e.memset(ones_f[:], 1.0), writes=[onesf_t])
    P.op("dve", lambda e: e.memset(epsc[:], EPS), writes=[eps_t])
    P.dma("sp", V[:], vecs, writes=[Vt])
    P.dma("sp", identF[:], identf, writes=[identF_t])
    CP(identB[:], identF[:], [identF_t], [identB_t])

    def dv(l, s, which, k=None):
        o = ((l * 2 + s) * 6 + which) * 8
        return DV[:, o:o + 8] if k is None else DV[:, o + k:o + k + 1]

    GM_M, SH_M, GG_M, GM_F, SH_F, GG_F = range(6)

    with ExitStack() as ph:
        scT, sc_t = sb("scT", [128, 8, 2], F32, ph)
        modT, mod_t = sb("modT", [128, 48, 2], F32, ph)
        wbuf = [sb("modw%d" % i, [128, 8, 1024], F32, ph) for i in range(2)]
        P.dma("sp", scT[:], cT, writes=[sc_t])
        ACT(scT[:], scT[:], AF.Silu, [sc_t], [sc_t])
        for l in (0, 1):
            ps, pst = P.psl[l]
            for j in range(6):
                wb, wbt = wbuf[j % 2]
                P.dma("sp", wb[:], modw[l][:, :, j * 1024:(j + 1) * 1024], writes=[wbt])
                for kc in range(8):
                    col = (j * 8 + kc) * 2
                    for k in range(8):
                        MM(ps[:, col:col + 2], wb[:, k, kc * 128:(kc + 1) * 128], scT[:, k, :], k == 0, k == 7, [wbt, sc_t], pst)
            mb = VO["l%d_mod_b" % l]
            for s in (0, 1):
                TTo(modT[:, :, s], ps[:, 0:96].rearrange("p (j t) -> p j t", t=2)[:, :, s], V[:, mb:mb + 48], ALU.add,
                    [pst, Vt], [mod_t])
                pre = "l%d_" % l
                for (which_gm, which_sh, which_gg, base, npre, npost) in (
                        (GM_M, SH_M, GG_M, 0, "mix_pre", "mix_post"), (GM_F, SH_F, GG_F, 24, "ffn_pre", "ffn_post")):
                    o_pre = VO[pre + npre]
                    o_post = VO[pre + npost]
                    STT(dv(l, s, which_gm), modT[:, base + 8:base + 16, s], 1.0, V[:, o_pre:o_pre + 8], ALU.add, ALU.mult,
                        [mod_t, Vt], [DVt])
                    CP(dv(l, s, which_sh), modT[:, base:base + 8, s], [mod_t], [DVt])
                    TTo(dv(l, s, which_gg), modT[:, base + 16:base + 24, s], V[:, o_post:o_post + 8], ALU.mult,
                        [mod_t, Vt], [DVt])
        P.barrier()

    def rms_rstd(src, src_t, n, sq, sq_t, rstd, rstd_t, bank, nch=8, scale=1.0 / 1024, npart=128):
        ACT(sq[:, :nch, :n], src, AF.Square, [src_t], [sq_t])
        ps, pst = P.psl[bank]
        for k in range(nch):
            MM(ps[:npart, :n], ones_bf[:, :npart], sq[:, k, :n], k == 0, k == nch - 1, [sq_t, ones_t], pst)
        ACT(rstd[:npart, :n], ps[:npart, :n], AF.Sqrt, [pst, eps_t], [rstd_t], scale=scale, bias=epsc[:npart, 0:1])
        P.op("dve", lambda e: e.reciprocal(rstd[:npart, :n], rstd[:npart, :n]), reads=[rstd_t], writes=[rstd_t])

    def norm_mod(xT, x_t, n, l, s, which_gm, which_sh, h, h_t, sq, sq_t, rstd, rstd_t, tmp, tmp_t, bank):
        rms_rstd(xT[:, :, :n], x_t, n, sq, sq_t, rstd, rstd_t, bank)
        for k in range(8):
            TTo(tmp[:, k, :n], xT[:, k, :n], rstd[:, :n], ALU.mult, [x_t, rstd_t], [tmp_t])
        for k in range(8):
            ACT(h[:, k, :n], tmp[:, k, :n], AF.Identity, [tmp_t, DVt], [h_t],
                scale=dv(l, s, which_gm, k), bias=dv(l, s, which_sh, k))

    def post_res(y, y_t, n, l, s, which_gg, xres, xres_t, sq, sq_t, rstd, rstd_t, bank, out, out_t):
        rms_rstd(y[:, :, :n], y_t, n, sq, sq_t, rstd, rstd_t, bank)
        for k in range(8):
            TTo(y[:, k, :n], y[:, k, :n], rstd[:, :n], ALU.mult, [y_t, rstd_t], [y_t])
        for k in range(8):
            STT(out[:, k, :n], y[:, k, :n], dv(l, s, which_gg, k), xres[:, k, :n], ALU.mult, ALU.add,
                [y_t, xres_t, DVt], [out_t])

    if upto == "p0":
        return finish(nc, P, outT)

    with ExitStack() as mix:
        QT, _ = sb("QT", [64, 8, TT], BF16, mix)
        KT, _ = sb("KT", [64, 2, TT], BF16, mix)
        VT, _ = sb("VT", [128, NBLK, 128], BF16, mix)
        QTt = [Tr() for _ in range(NBLK)]
        KTt = [Tr() for _ in range(NBLK)]
        VTt = [Tr() for _ in range(NBLK)]
        POt = [Tr() for _ in range(NBLK)]
        UTt = Tr()
        xin_t = Tr()

        with ExitStack() as ph:
            w_in, w_in_t = sb("w_in", [128, 8, 1280], BF16, ph)
            rotf, rotf_t = sb("rotf", [64, 64], F32, ph)
            rot, rot_t = sb("rot", [64, 64], BF16, ph)
            for k in range(8):
                P.dma("pool", w_in[:, k, :], w0_in[:, k, :], writes=[w_in_t])
            P.dma("sp", rotf[:], rotT, writes=[rotf_t])
            CP(rot[:], rotf[:], [rotf_t], [rot_t])
            xTb = [sb("xT%d" % i, [128, 8, 512], F32, ph) for i in range(2)]
            cosb = [sb("cos%d" % i, [64, 512], F32, ph) for i in range(2)]
            sinb = [sb("sin%d" % i, [64, 512], F32, ph) for i in range(2)]
            sq, sq_t = sb("sq", [128, 8, 512], BF16, ph)
            rstd, rstd_t = sb("rstd", [128, 512], F32, ph)
            tmp, tmp_t = sb("tmp", [128, 8, 512], F32, ph)
            h, h_t = sb("h", [128, 8, 512], BF16, ph)
            ub = [sb("ub%d" % i, [128, 512], F32, ph) for i in range(2)]
            qb = [sb("qb%d" % i, [64, 512], BF16, ph) for i in range(2)]
            t1 = [sb("t1_%d" % i, [64, 512], F32, ph) for i in range(2)]
            t2 = [sb("t2_%d" % i, [64, 512], F32, ph) for i in range(2)]
            c_proj, c_rot, c_v, c_misc = [0], [0], [0], [0]

            def loadA(i):
                t0, n = TILES512[i]
                xT, xt = xTb[i % 2]
                for k in range(8):
                    P.dma("sp", xT[:, k, :n], xin[k, :, t0:t0 + n], reads=[xin_t], writes=[xt])
                if i > 0:
                    l0 = t0 - CTX
                    P.dma("sp", cosb[i % 2][0][:, :n], rope_cos[:, l0:l0 + n], writes=[cosb[i % 2][1]])
                    P.dma("sp", sinb[i % 2][0][:, :n], rope_sin[:, l0:l0 + n], writes=[sinb[i % 2][1]])

            loadA(0)
            NTA = int(os.environ.get('NTA', len(TILES512)))
            for i, (t0, n) in enumerate(TILES512[:NTA]):
                if i + 1 < NTA:
                    loadA(i + 1)
                s = 1 if i == 0 else 0
                blks = list(range(t0 // 128, (t0 + n) // 128))
                xT, xt = xTb[i % 2]
                norm_mod(xT, xt, n, 0, s, GM_M, SH_M, h, h_t, sq, sq_t, rstd, rstd_t, tmp, tmp_t, 0)
                for m in range(0 if os.environ.get('NOU') else 4):
                    ps, pst = P.ps([1, 2, 3], c_proj)
                    for k in range(8):
                        MM(ps[:, :n], w_in[:, k, m * 128:(m + 1) * 128], h[:, k, :n], k == 0, k == 7, [w_in_t, h_t], pst)
                    u, ut = ub[c_misc[0] % 2]
                    c_misc[0] += 1
                    ACT(u[:, :n], ps[:, :n], AF.Copy, [pst], [ut])
                    P.dma("sp", UT[m, :, t0:t0 + n], u[:, :n], reads=[ut], writes=[UTt])
                for hh in range(int(os.environ.get('NHH', 10))):
                    ps, pst = P.ps([1, 2, 3], c_proj)
                    c0 = 512 + hh * 64
                    for k in range(8):
                        MM(ps[:64, :n], w_in[:, k, c0:c0 + 64], h[:, k, :n], k == 0, k == 7, [w_in_t, h_t], pst)
                    dst = QT[:, hh, t0:t0 + n] if hh < 8 else KT[:, hh - 8, t0:t0 + n]
                    dst_t = [QTt[b] for b in blks] if hh < 8 else [KTt[b] for b in blks]
                    if i == 0 or os.environ.get('NOROPE'):
                        ACT(dst, ps[:64, :n], AF.Copy, [pst], dst_t)
                    else:
                        q, qt = qb[c_misc[0] % 2]
                        a1, a1t = t1[c_misc[0] % 2]
                        a2, a2t = t2[c_misc[0] % 2]
                        c_misc[0] += 1
                        cs, cst = cosb[i % 2]
                        sn, snt = sinb[i % 2]
                        ACT(q[:, :n], ps[:64, :n], AF.Copy, [pst], [qt])
                        pr, prt = P.ps([4, 5], c_rot)
                        MM(pr[:64, :n], rot[:], q[:, :n], True, True, [rot_t, qt], prt)
                        if os.environ.get('ROPEV') == '1':
                            TTo(a1[:, :n], q[:, :n], cs[:, :n], ALU.mult, [qt, cst], [a1t])
                        else:
                            TTo(a1[:, :n], ps[:64, :n], cs[:, :n], ALU.mult, [pst, cst], [a1t])
                        TTo(a2[:, :n], pr[:64, :n], sn[:, :n], ALU.mult, [prt, snt], [a2t])
                        TTo(dst, a1[:, :n], a2[:, :n], ALU.add, [a1t, a2t], dst_t)
                for b in range(0 if os.environ.get('NOV') else n // 128):
                    ps, pst = P.ps([6, 7], c_v)
                    for k in range(8):
                        MM(ps[:, :128], h[:, k, b * 128:(b + 1) * 128], w_in[:, k, 1152:1280], k == 0, k == 7, [w_in_t, h_t], pst)
                    ACT(VT[:, blks[b], :], ps[:, :128], AF.Copy, [pst], [VTt[blks[b]]])
            P.barrier()
        if upto == "A":
            dq = dscr("dQT", [64, 8, TT], BF16)
            dk = dscr("dKT", [64, 2, TT], BF16)
            dvv = dscr("dVT", [128, NBLK, 128], BF16)
            P.dma("sp", dq, QT[:], reads=QTt)
            P.dma("sp", dk, KT[:], reads=KTt)
            P.dma("sp", dvv, VT[:], reads=VTt)
            return finish(nc, P, outT)

        with ExitStack() as ph:
            msk, msk_t = sb("msk", [128, 2, 512], BF16, ph)
            mskf, mskf_t = sb("mskf", [128, 2, 512], F32, ph)
            P.dma("sp", mskf[:], masks, writes=[mskf_t])
            CP(msk[:], mskf[:], [mskf_t], [msk_t])
            esk, esk_t = sb("esk", [64, 2, 512], F32, ph)
            so = VO["sinks"]
            for hh in range(8):
                ACT(esk[:, hh // 4, (hh % 4) * 128:(hh % 4 + 1) * 128], ones_f[:64, :], AF.Exp, [onesf_t, Vt], [esk_t],
                    scale=V[:64, so + hh:so + hh + 1])
            pts = [sb("pt%d" % i, [128, 512], BF16, ph) for i in range(6)]
            dens = [sb("den%d" % i, [64, 512], F32, ph) for i in range(2)]
            c_s, c_pt, c_nd = [0], [0], [0]
            for tb in range(NBLK):
                if tb < 2:
                    kbs = [(0, None), (1, None)]
                else:
                    kbs = [(0, None), (1, None)]
                    if tb > 2:
                        kbs.append((tb - 1, 0))
                    kbs.append((tb, None))
                    if tb < NBLK - 1:
                        kbs.append((tb + 1, 1))
                for j in range(2):
                    ptl = []
                    for (kb, mi) in kbs:
                        ps, pst = P.ps([0, 1, 2, 3, 4, 5], c_s)
                        MM(ps[:, :].rearrange("p (h q) -> p h q", h=4), KT[:, j, kb * 128:(kb + 1) * 128],
                           QT[:, 4 * j:4 * j + 4, tb * 128:(tb + 1) * 128], True, True, [KTt[kb], QTt[tb]], pst)
                        pt, ptt = pts[c_pt[0] % 6]
                        c_pt[0] += 1
                        ACT(pt[:], ps[:], AF.Exp, [pst], [ptt], scale=0.125)
                        if mi is not None:
                            TTo(pt[:], pt[:], msk[:, mi, :], ALU.mult, [ptt, msk_t], [ptt])
                        ptl.append((pt, ptt, kb))
                    psn, psnt = P.psl[6]
                    psd, psdt = P.psl[7]
                    for ii, (pt, ptt, kb) in enumerate(ptl):
                        MM(psn[:64, :], VT[:, kb, j * 64:(j + 1) * 64], pt[:], ii == 0, ii == len(ptl) - 1, [VTt[kb], ptt], psnt)
                    for ii, (pt, ptt, kb) in enumerate(ptl):
                        MM(psd[:64, :], ones_bf[:, :64], pt[:], ii == 0, ii == len(ptl) - 1, [ones_t, ptt], psdt)
                    dn, dnt = dens[c_nd[0] % 2]
                    c_nd[0] += 1
                    TTo(dn[:], psd[:64, :], esk[:, j, :], ALU.add, [psdt, esk_t], [dnt])
                    P.op("dve", lambda e: e.reciprocal(dn[:], dn[:]), reads=[dnt], writes=[dnt])
                    TTo(QT[:, 4 * j:4 * j + 4, tb * 128:(tb + 1) * 128], psn[:64, :].rearrange("p (h q) -> p h q", h=4),
                        dn[:].rearrange("p (h q) -> p h q", h=4), ALU.mult, [psnt, dnt], [QTt[tb]])
            P.barrier()
        if upto == "B":
            dq = dscr("dAO", [64, 8, TT], BF16)
            P.dma("sp", dq, QT[:], reads=QTt)
            return finish(nc, P, outT)

        PO, _ = sb("PO", [128, 4, TT], BF16, mix)
        with ExitStack() as ph:
            pw, pw_t = sb("pw", [128, 4, 128], BF16, ph)
            P.dma("pool", pw[:], pool_w, writes=[pw_t])
            LP = LAT + 16
            U0, U0t = sb("U0", [128, LP], F32, ph)
            Ba, Bat = sb("Ba", [128, LP], F32, ph)
            Bb, Bbt = sb("Bb", [128, LP], F32, ph)
            ivc, ivct = sb("ivc", [128, LAT], F32, ph)
            dl, dlt = sb("dl", [128, LAT], BF16, ph)
            c_p = [0]
            pso = VO["pool_scale"]
            for g, w in enumerate((2, 4, 8, 16)):
                for (s0, L, ivsrc) in ((0, CTX, invc_ctx), (CTX, LAT, invc_lat)):
                    Lt = L + 16
                    P.op("dve", lambda e: e.memset(U0[:, 0:8], 0.0), writes=[U0t])
                    P.op("dve", lambda e: e.memset(U0[:, 8 + L:16 + L], 0.0), writes=[U0t])
                    P.dma("sp", U0[:, 8:8 + L], UT[g, :, s0:s0 + L], reads=[UTt], writes=[U0t])
                    P.dma("sp", ivc[:, :L], ivsrc[:, g, :], writes=[ivct])
                    src, srct = U0, U0t
                    sh = 1
                    ln = Lt
                    bufs = [(Ba, Bat), (Bb, Bbt)]
                    bi = 0
                    while sh < w:
                        dst, dstt = bufs[bi % 2]
                        bi += 1
                        ln = ln - sh
                        TTo(dst[:, :ln], src[:, :ln], src[:, sh:sh + ln], ALU.add, [srct], [dstt])
                        src, srct = dst, dstt
                        sh *= 2
                    o = 8 - w // 2
                    dst, dstt = bufs[bi % 2]
                    TTo(dst[:, :L], src[:, o:o + L], ivc[:, :L], ALU.mult, [srct, ivct], [dstt])
                    TTo(dl[:, :L], dst[:, :L], U0[:, 8:8 + L], ALU.subtract, [dstt, U0t], [dlt])
                    for c0 in range(0, L, 512):
                        n = min(512, L - c0)
                        ps, pst = P.ps([0, 1, 2, 3], c_p)
                        MM(ps[:, :n], pw[:, g, :], dl[:, c0:c0 + n], True, True, [pw_t, dlt], pst)
                        blks = list(range((s0 + c0) // 128, (s0 + c0 + n) // 128))
                        ACT(PO[:, g, s0 + c0:s0 + c0 + n], ps[:, :n], AF.Copy, [pst, Vt], [POt[b] for b in blks],
                            scale=V[:, pso + g:pso + g + 1])
            P.barrier()
        if upto == "C":
            dq = dscr("dPO", [128, 4, TT], BF16)
            P.dma("sp", dq, PO[:], reads=POt)
            return finish(nc, P, outT)

        with ExitStack() as ph:
            wop, wop_t = sb("wop", [128, 4, 1024], BF16, ph)
            woa, woa_t = sb("woa", [64, 8, 1024], BF16, ph)
            for k in range(4):
                P.dma("pool", wop[:, k, :], w0_outp[:, k, :], writes=[wop_t])
            for k in range(8):
                P.dma("pool", woa[:, k, :], w0_outa[:, k, :], writes=[woa_t])
            xTb = [sb("xTd%d" % i, [128, 8, 512], F32, ph) for i in range(1)]
            yb, yb_t = sb("yb", [128, 8, 512], F32, ph)
            sq, sq_t = sb("sqd", [128, 8, 512], BF16, ph)
            rstd, rstd_t = sb("rstdd", [128, 512], F32, ph)
            XMt = Tr()
            c_y = [0]

            def loadD(i):
                t0, n = TILES512[i]
                xT, xt = xTb[0]
                for k in range(8):
                    P.dma("sp", xT[:, k, :n], xin[k, :, t0:t0 + n], reads=[xin_t], writes=[xt])

            for i, (t0, n) in enumerate(TILES512):
                loadD(i)
                s = 1 if i == 0 else 0
                blks = list(range(t0 // 128, (t0 + n) // 128))
                xT, xt = xTb[0]
                rd = [POt[b] for b in blks] + [QTt[b] for b in blks]
                for m in range(8):
                    ps, pst = P.ps([1, 2, 3, 4], c_y)
                    for g in range(4):
                        MM(ps[:, :n], wop[:, g, m * 128:(m + 1) * 128], PO[:, g, t0:t0 + n], g == 0, False, [wop_t] + rd, pst)
                    for hh in range(8):
                        MM(ps[:, :n], woa[:, hh, m * 128:(m + 1) * 128], QT[:, hh, t0:t0 + n], False, hh == 7, [woa_t] + rd, pst)
                    ACT(yb[:, m, :n], ps[:, :n], AF.Copy, [pst], [yb_t])
                xout, xout_t = yb, yb_t
                post_res(yb, yb_t, n, 0, s, GG_M, xT, xt, sq, sq_t, rstd, rstd_t, 0, xout, xout_t)
                for k in range(8):
                    P.dma("sp", XM[k, :, t0:t0 + n], xout[:, k, :n], reads=[xout_t], writes=[XMt])
            P.barrier()
    if upto == "D":
        return finish(nc, P, outT)

    XMt, X1t = Tr(), Tr()
    with ExitStack() as ph:
        w1, w1_t = sb("w1", [128, 8, 2816], BF16, ph)
        w3, w3_t = sb("w3", [128, 8, 2816], BF16, ph)
        for k in range(8):
            P.dma("pool", w1[:, k, :], ffn_w1[:, k, :], writes=[w1_t])
            P.dma("pool", w3[:, k, :], ffn_w3[:, k, :], writes=[w3_t])
        w2b = [sb("w2b%d" % i, [128, 22, 128], BF16, ph) for i in range(2)]
        xTb = [sb("xTe%d" % i, [128, 8, 256], F32, ph) for i in range(2)]
        sq, sq_t = sb("sqe", [128, 8, 256], BF16, ph)
        rstd, rstd_t = sb("rstde", [128, 256], F32, ph)
        tmp, tmp_t = sb("tmpe", [128, 8, 256], F32, ph)
        h2, h2_t = sb("h2e", [128, 8, 256], BF16, ph)
        gg, gg_t = sb("gge", [128, 22, 256], BF16, ph)
        slb = [sb("sle%d" % i, [128, 256], F32, ph) for i in range(2)]
        xout, xout_t = sb("xoe", [128, 8, 256], F32, ph)
        c_a, c_b, c_w, c_s = [0], [0], [0], [0]

        def loadE(i):
            t0, n = TILES256[i]
            xT, xt = xTb[i % 2]
            for k in range(8):
                P.dma("sp", xT[:, k, :n], XM[k, :, t0:t0 + n], reads=[XMt], writes=[xt])

        loadE(0)
        for i, (t0, n) in enumerate(TILES256):
            if i + 1 < len(TILES256):
                loadE(i + 1)
            s = 1 if i == 0 else 0
            xT, xt = xTb[i % 2]
            norm_mod(xT, xt, n, 0, s, GM_F, SH_F, h2, h2_t, sq, sq_t, rstd, rstd_t, tmp, tmp_t, 0)
            for c in range(22):
                ps1, ps1t = P.ps([1, 2, 3, 4], c_a)
                for k in range(8):
                    MM(ps1[:, :n], w1[:, k, c * 128:(c + 1) * 128], h2[:, k, :n], k == 0, k == 7, [w1_t, h2_t], ps1t)
                ps3, ps3t = P.ps([1, 2, 3, 4], c_a)
                for k in range(8):
                    MM(ps3[:, :n], w3[:, k, c * 128:(c + 1) * 128], h2[:, k, :n], k == 0, k == 7, [w3_t, h2_t], ps3t)
                sl, slt = slb[c_s[0] % 2]
                c_s[0] += 1
                ACT(sl[:, :n], ps1[:, :n], AF.Silu, [ps1t], [slt])
                TTo(gg[:, c, :n], sl[:, :n], ps3[:, :n], ALU.mult, [slt, ps3t], [gg_t])
            for m in range(8):
                wb, wbt = w2b[c_w[0] % 2]
                c_w[0] += 1
                P.dma("pool", wb[:], ffn_w2[:, :, m * 128:(m + 1) * 128], writes=[wbt])
                ps, pst = P.ps([5, 6, 7], c_b)
                for c in range(22):
                    MM(ps[:, :n], wb[:, c, :], gg[:, c, :n], c == 0, c == 21, [wbt, gg_t], pst)
                ACT(tmp[:, m, :n], ps[:, :n], AF.Copy, [pst], [tmp_t])
            post_res(tmp, tmp_t, n, 0, s, GG_F, xT, xt, sq, sq_t, rstd, rstd_t, 0, xout, xout_t)
            for k in range(8):
                P.dma("sp", X1[k, :, t0:t0 + n], xout[:, k, :n], reads=[xout_t], writes=[X1t])
        P.barrier()
    if upto == "E":
        return finish(nc, P, outT)


    w1_in = din("w1_in", [128, 8, 4128])
    w1_out = din("w1_out", [128, 8, 1024])
    selc = din("selc", [16, 16, 128])
    gmask = din("gmask", [64, 4, 64])
    tri = din("tri", [64, 2, 64])
    router = din("router", [128, 8, 8])
    moe_w1 = [din("moe_w1_%d" % e, [1024, 3584]) for e in range(8)]
    moe_w3 = [din("moe_w3_%d" % e, [1024, 3584]) for e in range(8)]
    moe_w2 = [din("moe_w2_%d" % e, [3584, 1024]) for e in range(8)]
    QKV = dscr("QKV", [24, 128, TT], BF16)
    Z1 = dscr("Z1", [8, 128, TT])
    OD = dscr("OD", [2, 8, 128, TT])
    QKVt, Z1t, ODt = Tr(), Tr(), Tr()
    NCH = TT // 64

    def norm_mod_v(xs, x_t, n, l, s, which_gm, which_sh, hs, h_t, sq, sq_t, rstd, rstd_t, tmp, tmp_t, bank):
        rms_rstd(xs, x_t, n, sq, sq_t, rstd, rstd_t, bank)
        for k in range(8):
            TTo(tmp[:, k, :n], xs[:, k, :], rstd[:, :n], ALU.mult, [x_t, rstd_t], [tmp_t])
        for k in range(8):
            ACT(hs[:, k, :], tmp[:, k, :n], AF.Identity, [tmp_t, DVt], [h_t],
                scale=dv(l, s, which_gm, k), bias=dv(l, s, which_sh, k))

    with ExitStack() as l1:
        GT, GT_t = sb("GT", [64, NCH, 16], F32, l1)
        BT, BT_t = sb("BT", [64, NCH, 16], F32, l1)
        with ExitStack() as ph:
            w_in, w_in_t = sb("w1in", [128, 8, 4128], BF16, ph)
            for k in range(8):
                P.dma("pool", w_in[:, k, :], w1_in[:, k, :], writes=[w_in_t])
            nea, nea_t = sb("nea", [64, 16], F32, ph)
            alo, dbo, cwo = VO["a_log"], VO["dt_bias"], VO["conv_w"]
            ACT(nea[:], V[:64, alo:alo + 16], AF.Exp, [Vt], [nea_t])
            ACT(nea[:], nea[:], AF.Copy, [nea_t], [nea_t], scale=-1.0)
            eps128, eps128_t = sb("eps128", [128, 1], F32, ph)
            P.op("dve", lambda e: e.memset(eps128[:], 128.0 * EPS), writes=[eps128_t])
            W = 260
            xTb = [sb("xTf%d" % i, [128, 8, W], F32, ph) for i in range(2)]
            sq, sq_t = sb("sqf", [128, 8, W], BF16, ph)
            rstd, rstd_t = sb("rstdf", [128, W], F32, ph)
            tmp, tmp_t = sb("tmpf", [128, 8, W], F32, ph)
            h, h_t = sb("hf", [128, 8, W], BF16, ph)
            pcb = [sb("pc%d" % i, [128, W], F32, ph) for i in range(2)]
            accb = [sb("acc%d" % i, [128, 256], F32, ph) for i in range(2)]
            silb = [sb("sil%d" % i, [128, 256], F32, ph) for i in range(2)]
            sq2b = [sb("sq2%d" % i, [128, 256], BF16, ph) for i in range(2)]
            rs2b = [sb("rs2%d" % i, [128, 256], F32, ph) for i in range(2)]
            obb = [sb("ob%d" % i, [128, 256], BF16, ph) for i in range(3)]
            zbb = [sb("zb%d" % i, [128, 256], F32, ph) for i in range(2)]
            ta, ta_t = sb("ta", [64, 4, 16], F32, ph)
            te, te_t = sb("te", [64, 4, 16], F32, ph)
            c_a, c_n, c_m, c_o, c_z = [0], [0], [0], [0], [0]

            def rngF(i):
                t0 = 256 * i
                lo = 2 if i in (0, 1) else 0
                hi = 258 if i in (0, 16) else 260
                return t0, lo, hi

            def loadF(i):
                t0, lo, hi = rngF(i)
                xT, xt = xTb[i % 2]
                for k in range(8):
                    P.dma("sp", xT[:, k, lo:hi], X1[k, :, t0 - 2 + lo:t0 - 2 + hi], reads=[X1t], writes=[xt])

            NTF = int(os.environ.get('NTF', 17))
            loadF(0)
            for i in range(NTF):
                if i + 1 < NTF:
                    loadF(i + 1)
                t0, lo, hi = rngF(i)
                nv = hi - lo
                s = 1 if i == 0 else 0
                xT, xt = xTb[i % 2]
                norm_mod_v(xT[:, :, lo:hi], xt, nv, 1, s, GM_M, SH_M, h[:, :, lo:hi], h_t, sq, sq_t, rstd, rstd_t, tmp, tmp_t, 0)
                for m in range(32):
                    ps, pst = P.ps([1, 2, 3], c_a)
                    for k in range(8):
                        MM(ps[:, :nv], w_in[:, k, m * 128:(m + 1) * 128], h[:, k, lo:hi], k == 0, k == 7, [w_in_t, h_t], pst)
                    if m >= 24:
                        zb, zbt = zbb[c_z[0] % 2]
                        c_z[0] += 1
                        ACT(zb[:], ps[:, 2 - lo:258 - lo], AF.Copy, [pst], [zbt])
                        P.dma("sp", Z1[m - 24, :, t0:t0 + 256], zb[:], reads=[zbt], writes=[Z1t])
                        continue
                    pc, pct = pcb[c_m[0] % 2]
                    acc, acct = accb[c_m[0] % 2]
                    sil, silt = silb[c_m[0] % 2]
                    sq2, sq2t = sq2b[c_m[0] % 2]
                    rs2, rs2t = rs2b[c_m[0] % 2]
                    c_m[0] += 1
                    if lo > 0:
                        P.op("dve", lambda e: e.memset(pc[:, 0:2], 0.0), writes=[pct])
                    if hi < W:
                        P.op("dve", lambda e: e.memset(pc[:, 258:260], 0.0), writes=[pct])
                    ACT(pc[:, lo:hi], ps[:, :nv], AF.Copy, [pst], [pct])
                    ACT(acc[:], pc[:, 0:256], AF.Copy, [pct, Vt], [acct], scale=V[:, cwo + m:cwo + m + 1])
                    for j in range(1, 5):
                        STT(acc[:], pc[:, j:j + 256], V[:, cwo + j * 24 + m:cwo + j * 24 + m + 1], acc[:], ALU.mult, ALU.add,
                            [pct, Vt, acct], [acct])
                    ACT(sil[:], acc[:], AF.Silu, [acct], [silt])
                    ob, obt = obb[c_o[0] % 3]
                    c_o[0] += 1
                    if m < 16:
                        ACT(sq2[:], sil[:], AF.Square, [silt], [sq2t])
                        pn, pnt = P.ps([4, 5], c_n)
                        MM(pn[:, :256], ones_bf[:], sq2[:], True, True, [ones_t, sq2t], pnt)
                        if m < 8:
                            ACT(rs2[:], pn[:, :256], AF.Sqrt, [pnt, eps128_t], [rs2t], scale=128.0, bias=eps128[:, 0:1])
                        else:
                            ACT(rs2[:], pn[:, :256], AF.Sqrt, [pnt, eps_t], [rs2t], scale=1.0, bias=epsc[:, 0:1])
                        P.op("dve", lambda e: e.reciprocal(rs2[:], rs2[:]), reads=[rs2t], writes=[rs2t])
                        TTo(ob[:], sil[:], rs2[:], ALU.mult, [silt, rs2t], [obt])
                    else:
                        CP(ob[:], sil[:], [silt], [obt])
                    P.dma("sp", QKV[m, :, t0:t0 + 256], ob[:], reads=[obt], writes=[QKVt])
                pab, pabt = P.psl[6]
                for cc in range(4):
                    for k in range(8):
                        MM(pab[:64, cc * 32:(cc + 1) * 32], h[:, k, 2 + cc * 64:2 + (cc + 1) * 64], w_in[:, k, 4096:4128],
                           k == 0, k == 7, [w_in_t, h_t], pabt)
                for cc in range(4):
                    pv = pab[:64, cc * 32:(cc + 1) * 32].rearrange("p (d ab h) -> p d ab h", d=2, ab=2)
                    TTo(ta[:, cc, :].rearrange("p (d h) -> p d h", d=2), pv[:, :, 0, :],
                        V[:64, dbo:dbo + 16].rearrange("p (d h) -> p d h", d=2), ALU.add, [pabt, Vt], [ta_t])
                ACT(te[:], ta[:], AF.Exp, [ta_t], [te_t])
                ACT(ta[:], te[:], AF.Ln, [te_t, onesf_t], [ta_t], bias=ones_f[:64, 0:1])
                for cc in range(4):
                    c = t0 // 64 + cc
                    pv = pab[:64, cc * 32:(cc + 1) * 32].rearrange("p (d ab h) -> p d ab h", d=2, ab=2)
                    TTo(GT[:, c, :], ta[:, cc, :], nea[:], ALU.mult, [ta_t, nea_t], [GT_t])
                    ACT(BT[:, c, :].rearrange("p (d h) -> p d h", d=2), pv[:, :, 1, :], AF.Sigmoid, [pabt], [BT_t])
            P.barrier()
        if upto == "F":
            dg = dscr("dGT", [64, NCH, 16])
            db = dscr("dBT", [64, NCH, 16])
            P.dma("sp", dg, GT[:], reads=[GT_t])
            P.dma("sp", db, BT[:], reads=[BT_t])
            return finish(nc, P, outT)

        with ExitStack() as ph:
            GAMc, GAMc_t = sb("GAMc", [64, NCH, 16], F32, ph)
            GLb, GLb_t = sb("GLb", [128, NCH, 16], F32, ph)
            CD, CD_t = sb("CD", [128, NCH, 16], F32, ph)
            CKD, CKD_t = sb("CKD", [64, NCH, 16], F32, ph)
            CKB, CKB_t = sb("CKB", [64, NCH, 16], F32, ph)
            GAMr, GAMr_t = sb("GAMr", [16, TT], F32, ph)
            NGAMr, NGAMr_t = sb("NGAMr", [16, TT], F32, ph)
            Sel, Sel_t = sb("Sel", [16, 16, 128], F32, ph)
            gm, gm_t = sb("gm", [64, 4, 64], F32, ph)
            trt, trt_t = sb("trt", [64, 2, 64], F32, ph)
            P.dma("sp", Sel[:], selc, writes=[Sel_t])
            P.dma("sp", gm[:], gmask, writes=[gm_t])
            P.dma("sp", trt[:], tri, writes=[trt_t])
            for c0 in range(0, NCH, 32):
                c1 = min(NCH, c0 + 32)
                n = (c1 - c0) * 16
                rhs = GT[:, c0:c1, :]
                psF, psFt = P.psl[0]
                psB, psBt = P.psl[1]
                psL, psLt = P.psl[2]
                MM(psF[:64, :n].rearrange("p (c x) -> p c x", x=16), trt[:, 0, :], rhs, True, True, [trt_t, GT_t], psFt)
                MM(psB[:64, :n].rearrange("p (c x) -> p c x", x=16), trt[:, 1, :], rhs, True, True, [trt_t, GT_t], psBt)
                MM(psL[:, :n].rearrange("p (c x) -> p c x", x=16), ones_f[:64, :], rhs, True, True, [onesf_t, GT_t], psLt)
                ACT(GAMc[:, c0:c1, 0:8], psF[:64, :n].rearrange("p (c x) -> p c x", x=16)[:, :, 0:8], AF.Copy, [psFt], [GAMc_t])
                ACT(GAMc[:, c0:c1, 8:16], psB[:64, :n].rearrange("p (c x) -> p c x", x=16)[:, :, 8:16], AF.Copy, [psBt], [GAMc_t])
                CP(GLb[:, c0:c1, :], psL[:, :n].rearrange("p (c x) -> p c x", x=16), [psLt], [GLb_t])
            ACT(CD[:], GLb[:], AF.Exp, [GLb_t], [CD_t])
            TTo(CKD[:], GLb[:64], GAMc[:], ALU.subtract, [GLb_t, GAMc_t], [CKD_t])
            ACT(CKD[:], CKD[:], AF.Exp, [CKD_t], [CKD_t])
            ACT(CKB[:], GAMc[:], AF.Exp, [GAMc_t], [CKB_t])
            TTo(CKB[:], CKB[:], BT[:], ALU.mult, [CKB_t, BT_t], [CKB_t])
            for c in range(NCH):
                psr, psrt = P.psl[3 + (c // 8) % 2]
                MM(psr[:16, (c % 8) * 64:(c % 8 + 1) * 64], GAMc[:, c, :], identF[:64, :64], True, True, [GAMc_t, identF_t], psrt)
                if c % 8 == 7 or c == NCH - 1:
                    cb = (c // 8) * 8
                    n = (c + 1 - cb) * 64
                    ACT(GAMr[:, cb * 64:cb * 64 + n], psr[:16, :n], AF.Copy, [psrt], [GAMr_t])
                    ACT(NGAMr[:, cb * 64:cb * 64 + n], psr[:16, :n], AF.Copy, [psrt], [NGAMr_t], scale=-1.0)
            P.barrier()

            cur = {"bank": 0, "used": 0}

            def psg(w=64):
                if cur["used"] + w > 256:
                    cur["bank"] = (cur["bank"] + 1) % 8
                    cur["used"] = 0
                b, o = cur["bank"], cur["used"]
                cur["used"] += w
                t, tr = P.psl[b]
                return t[:, o:o + w], tr

            CHN = []
            for hh in range(8):
                c = {}
                for nm, shp, dt in [("Dm", [64, 64], F32), ("E", [64, 64], F32), ("ELs", [64, 64], F32), ("ELi", [64, 64], F32),
                                    ("M", [64, 64], BF16), ("Y", [64, 64], BF16), ("QKm", [64, 64], BF16), ("QKmT", [64, 64], BF16),
                                    ("Xa", [64, 64], BF16), ("Xb", [64, 64], BF16), ("Ya", [64, 64], BF16), ("Yb", [64, 64], BF16),
                                    ("Pa", [64, 64], BF16), ("Pb", [64, 64], BF16),
                                    ("Kbg", [64, 128], BF16), ("Kd", [64, 128], BF16), ("Vb", [64, 128], BF16), ("vn", [64, 128], BF16),
                                    ("nWT", [128, 64], BF16), ("EGe", [128, 64], F32), ("qg", [128, 64], BF16),
                                    ("S", [128, 128], F32), ("Sb", [128, 128], BF16), ("OB", [128, 256], F32)]:
                    c[nm] = sb("%s%d" % (nm, hh), shp, dt, ph)
                CHN.append(c)
            stg = [sb("stg%d" % i, [128, 24, 256], BF16, ph) for i in range(2)]
            idB = identB[:64, :64]
            idF = identF[:64, :64]
            NEU = 5
            for d in range(2):
                order = list(range(NCH)) if d == 0 else [3, 2, 1, 0] + list(range(NCH - 1, 3, -1))
                order = order[:int(os.environ.get('NSTEP', len(order)))]
                groups = []
                for c in order:
                    if not groups or groups[-1] != c // 4:
                        groups.append(c // 4)
                gpos = {}

                def loadG(gi):
                    g4 = groups[gi]
                    st, stt = stg[gi % 2]
                    for m in range(24):
                        P.dma("sp", st[:, m, :], QKV[m, :, g4 * 256:(g4 + 1) * 256], reads=[QKVt], writes=[stt])
                    gpos[g4] = gi

                for hh in range(8):
                    S, St = CHN[hh]["S"]
                    Sb, Sbt = CHN[hh]["Sb"]
                    P.op("dve", lambda e: e.memset(S[:], 0.0), writes=[St])
                    P.op("dve", lambda e: e.memset(Sb[:], 0.0), writes=[Sbt])
                loadG(0)
                for c in order:
                    g4 = c // 4
                    gi = gpos[g4]
                    if c == order[0] or (c // 4 != prev_c // 4):
                        if gi + 1 < len(groups):
                            loadG(gi + 1)
                    prev_c = c
                    st, stt = stg[gi % 2]
                    o64 = (c % 4) * 64
                    tk = slice(c * 64, (c + 1) * 64)
                    is_lat = c >= 4
                    mS, mI = (0, 1) if d == 0 else (2, 3)
                    T = [dict() for _ in range(8)]
                    for hh in range(8):
                        dh = d * 8 + hh
                        T[hh]["qT"] = st[:, hh, o64:o64 + 64]
                        T[hh]["kT"] = st[:, 8 + hh, o64:o64 + 64]
                        T[hh]["vT"] = st[:, 16 + hh, o64:o64 + 64]
                        ps, pst = psg()
                        MM(ps[:64, :64], GAMr[:, tk], Sel[:, dh, :64], True, False, [GAMr_t, Sel_t], pst)
                        MM(ps[:64, :64], Sel[:, dh, :64], NGAMr[:, tk], False, True, [NGAMr_t, Sel_t], pst)
                        T[hh]["psD"] = (ps, pst)
                    for hh in range(8):
                        ps, pst = T[hh]["psD"]
                        Dm, Dmt = CHN[hh]["Dm"]
                        TS(Dm[:], ps[:64, :64], 0.0, 0.0, ALU.min, ALU.add, [pst], [Dmt])
                    for hh in range(8):
                        Dm, Dmt = CHN[hh]["Dm"]
                        E, Et = CHN[hh]["E"]
                        ACT(E[:], Dm[:], AF.Exp, [Dmt], [Et])
                    for hh in range(8):
                        E, Et = CHN[hh]["E"]
                        ELs, ELst = CHN[hh]["ELs"]
                        ELi, ELit = CHN[hh]["ELi"]
                        TTo(ELs[:], E[:], gm[:, mS, :], ALU.mult, [Et, gm_t], [ELst])
                        TTo(ELi[:], E[:], gm[:, mI, :], ALU.mult, [Et, gm_t], [ELit])
                    for hh in range(8):
                        ps, pst = psg()
                        MM(ps[:64, :64], T[hh]["kT"], T[hh]["kT"], True, True, [stt], pst)
                        T[hh]["psG"] = (ps, pst)
                        ps, pst = psg()
                        MM(ps[:64, :64], T[hh]["qT"], T[hh]["kT"], True, True, [stt], pst)
                        T[hh]["psQ"] = (ps, pst)
                    for hh in range(8):
                        dh = d * 8 + hh
                        ps, pst = T[hh]["psG"]
                        M, Mt = CHN[hh]["M"]
                        ELs, ELst = CHN[hh]["ELs"]
                        STT(M[:], ps[:64, :64], BT[:, c, dh:dh + 1], ELs[:], ALU.mult, ALU.mult, [pst, BT_t, ELst], [Mt])
                        ps, pst = T[hh]["psQ"]
                        QKm, QKmt = CHN[hh]["QKm"]
                        ELi, ELit = CHN[hh]["ELi"]
                        TTo(QKm[:], ps[:64, :64], ELi[:], ALU.mult, [pst, ELit], [QKmt])
                    for hh in range(8):
                        M, Mt = CHN[hh]["M"]
                        QKm, QKmt = CHN[hh]["QKm"]
                        ps, pst = psg()
                        MM(ps[:64, :64], M[:], idB, True, True, [Mt, identB_t], pst)
                        T[hh]["psY"] = (ps, pst)
                        ps, pst = psg()
                        MM(ps[:64, :64], QKm[:], idB, True, True, [QKmt, identB_t], pst)
                        T[hh]["psQT"] = (ps, pst)
                    for hh in range(8):
                        ps, pst = T[hh]["psY"]
                        Y, Yt = CHN[hh]["Y"]
                        Pa, Pat = CHN[hh]["Pa"]
                        ACT(Y[:], ps[:64, :64], AF.Copy, [pst], [Yt])
                        STT(Pa[:], ps[:64, :64], -1.0, idF, ALU.mult, ALU.add, [pst, identF_t], [Pat])
                        ps, pst = T[hh]["psQT"]
                        QKmT, QKmTt = CHN[hh]["QKmT"]
                        ACT(QKmT[:], ps[:64, :64], AF.Copy, [pst], [QKmTt])
                        T[hh]["X"] = CHN[hh]["M"]
                        T[hh]["Yc"] = CHN[hh]["Y"]
                        T[hh]["P"] = CHN[hh]["Pa"]
                    for r in range(NEU):
                        xn, yn, pn = ("Xa", "Ya", "Pb") if r % 2 == 0 else ("Xb", "Yb", "Pa")
                        for hh in range(8):
                            X, Xt = T[hh]["X"]
                            Yc, Yct = T[hh]["Yc"]
                            ps, pst = psg()
                            MM(ps[:64, :64], Yc[:], X[:], True, True, [Xt, Yct], pst)
                            T[hh]["psX"] = (ps, pst)
                            if r < NEU - 1:
                                ps, pst = psg()
                                MM(ps[:64, :64], X[:], Yc[:], True, True, [Xt, Yct], pst)
                                T[hh]["psYn"] = (ps, pst)
                        for hh in range(8):
                            Xn, Xnt = CHN[hh][xn]
                            ps, pst = T[hh]["psX"]
                            ACT(Xn[:], ps[:64, :64], AF.Copy, [pst], [Xnt])
                            if r < NEU - 1:
                                Yn, Ynt = CHN[hh][yn]
                                ps, pst = T[hh]["psYn"]
                                CP(Yn[:], ps[:64, :64], [pst], [Ynt])
                                T[hh]["Yc"] = CHN[hh][yn]
                            T[hh]["X"] = CHN[hh][xn]
                        for hh in range(8):
                            X, Xt = T[hh]["X"]
                            Pc, Pct = T[hh]["P"]
                            ps, pst = psg()
                            MM(ps[:64, :64], X[:], Pc[:], True, True, [Xt, Pct], pst)
                            T[hh]["psP"] = (ps, pst)
                        for hh in range(8):
                            Pc, Pct = T[hh]["P"]
                            Pn, Pnt = CHN[hh][pn]
                            ps, pst = T[hh]["psP"]
                            TTo(Pn[:], ps[:64, :64], Pc[:], ALU.add, [pst, Pct], [Pnt])
                            T[hh]["P"] = CHN[hh][pn]
                    for hh in range(8):
                        ps, pst = psg(128)
                        MM(ps[:64, :128], T[hh]["kT"], identB[:], True, True, [stt, identB_t], pst)
                        T[hh]["pskt"] = (ps, pst)
                        ps, pst = psg(128)
                        MM(ps[:64, :128], T[hh]["vT"], identB[:], True, True, [stt, identB_t], pst)
                        T[hh]["psvt"] = (ps, pst)
                    for hh in range(8):
                        dh = d * 8 + hh
                        ps, pst = T[hh]["pskt"]
                        Kbg, Kbgt = CHN[hh]["Kbg"]
                        Kd, Kdt = CHN[hh]["Kd"]
                        ACT(Kbg[:], ps[:64, :128], AF.Copy, [pst, CKB_t], [Kbgt], scale=CKB[:, c, dh:dh + 1])
                        TS(Kd[:], ps[:64, :128], CKD[:, c, dh:dh + 1], 0.0, ALU.mult, ALU.add, [pst, CKD_t], [Kdt])
                        ps, pst = T[hh]["psvt"]
                        Vb, Vbt = CHN[hh]["Vb"]
                        ACT(Vb[:], ps[:64, :128], AF.Copy, [pst, BT_t], [Vbt], scale=BT[:, c, dh:dh + 1])
                    for hh in range(8):
                        dh = d * 8 + hh
                        Kbg, Kbgt = CHN[hh]["Kbg"]
                        Pc, Pct = T[hh]["P"]
                        ps, pst = psg()
                        MM(ps[:, :64], Kbg[:], Pc[:], True, True, [Kbgt, Pct], pst)
                        T[hh]["psW"] = (ps, pst)
                        ps, pst = psg()
                        MM(ps[:, :64], Sel[:, dh, :], GAMr[:, tk], True, True, [Sel_t, GAMr_t], pst)
                        T[hh]["pse"] = (ps, pst)
                    for hh in range(8):
                        ps, pst = T[hh]["psW"]
                        nWT, nWTt = CHN[hh]["nWT"]
                        ACT(nWT[:], ps[:, :64], AF.Copy, [pst], [nWTt], scale=-1.0)
                        ps, pst = T[hh]["pse"]
                        EGe, EGet = CHN[hh]["EGe"]
                        qg, qgt = CHN[hh]["qg"]
                        ACT(EGe[:], ps[:, :64], AF.Exp, [pst], [EGet])
                        TTo(qg[:], T[hh]["qT"], EGe[:], ALU.mult, [stt, EGet], [qgt])
                    for hh in range(8):
                        Pc, Pct = T[hh]["P"]
                        Vb, Vbt = CHN[hh]["Vb"]
                        nWT, nWTt = CHN[hh]["nWT"]
                        Sb, Sbt = CHN[hh]["Sb"]
                        ps, pst = psg(128)
                        MM(ps[:64, :128], Pc[:], Vb[:], True, False, [Pct, Vbt], pst)
                        MM(ps[:64, :128], nWT[:], Sb[:], False, True, [nWTt, Sbt], pst)
                        T[hh]["psv"] = (ps, pst)
                    for hh in range(8):
                        ps, pst = T[hh]["psv"]
                        vn, vnt = CHN[hh]["vn"]
                        ACT(vn[:], ps[:64, :128], AF.Copy, [pst], [vnt])
                    for hh in range(8):
                        vn, vnt = CHN[hh]["vn"]
                        Sb, Sbt = CHN[hh]["Sb"]
                        qg, qgt = CHN[hh]["qg"]
                        QKmT, QKmTt = CHN[hh]["QKmT"]
                        Kd, Kdt = CHN[hh]["Kd"]
                        if is_lat:
                            ps, pst = psg()
                            MM(ps[:, :64], Sb[:], qg[:], True, False, [Sbt, qgt], pst)
                            MM(ps[:, :64], vn[:], QKmT[:], False, True, [vnt, QKmTt], pst)
                            T[hh]["pso"] = (ps, pst)
                        ps, pst = psg(128)
                        MM(ps[:, :128], Kd[:], vn[:], True, True, [Kdt, vnt], pst)
                        T[hh]["psS"] = (ps, pst)
                    for hh in range(8):
                        dh = d * 8 + hh
                        S, St = CHN[hh]["S"]
                        Sb, Sbt = CHN[hh]["Sb"]
                        OB, OBt = CHN[hh]["OB"]
                        if is_lat:
                            ps, pst = T[hh]["pso"]
                            CP(OB[:, o64:o64 + 64], ps[:, :64], [pst], [OBt])
                        ps, pst = T[hh]["psS"]
                        STT(S[:], S[:], CD[:, c, dh:dh + 1], ps[:, :128], ALU.mult, ALU.add, [St, CD_t, pst], [St])
                        ACT(Sb[:], S[:], AF.Copy, [St], [Sbt])
                        last_in_group = (c % 4 == 3) if d == 0 else (c % 4 == 0)
                        if is_lat and last_in_group:
                            P.dma("sp", OD[d, hh, :, g4 * 256:(g4 + 1) * 256], OB[:], reads=[OBt], writes=[ODt])
            P.barrier()
    if upto == "G":
        return finish(nc, P, outT)


    LT512 = [(256 + 512 * i, 512) for i in range(8)]
    with ExitStack() as ph:
        wo, wo_t = sb("wo1", [128, 8, 1024], BF16, ph)
        for k in range(8):
            P.dma("pool", wo[:, k, :], w1_out[:, k, :], writes=[wo_t])
        ono = VO["out_norm"]
        xT, xt = sb("xTh", [128, 8, 512], F32, ph)
        o0b = [sb("o0b%d" % i, [128, 512], F32, ph) for i in range(2)]
        o1b = [sb("o1b%d" % i, [128, 512], F32, ph) for i in range(2)]
        zbh = [sb("zbh%d" % i, [128, 512], F32, ph) for i in range(2)]
        sqh = [sb("sqh%d" % i, [128, 512], BF16, ph) for i in range(2)]
        rsh = [sb("rsh%d" % i, [128, 512], F32, ph) for i in range(2)]
        yg, yg_t = sb("yg", [128, 8, 512], BF16, ph)
        yb, yb_t = sb("ybh", [128, 8, 512], F32, ph)
        sq, sq_t = sb("sqhh", [128, 8, 512], BF16, ph)
        rstd, rstd_t = sb("rstdh", [128, 512], F32, ph)
        c_h, c_n, c_y = [0], [0], [0]
        XM2t = Tr()
        for (t0, n) in LT512:
            for k in range(8):
                P.dma("sp", xT[:, k, :], X1[k, :, t0:t0 + n], reads=[X1t], writes=[xt])
            for hh in range(8):
                o0, o0t = o0b[c_h[0] % 2]
                o1, o1t = o1b[c_h[0] % 2]
                zb, zbt = zbh[c_h[0] % 2]
                sqq, sqqt = sqh[c_h[0] % 2]
                rs, rst = rsh[c_h[0] % 2]
                c_h[0] += 1
                P.dma("sp", o0[:], OD[0, hh, :, t0:t0 + n], reads=[ODt], writes=[o0t])
                P.dma("sp", o1[:], OD[1, hh, :, t0:t0 + n], reads=[ODt], writes=[o1t])
                P.dma("sp", zb[:], Z1[hh, :, t0:t0 + n], reads=[Z1t], writes=[zbt])
                TTo(o0[:], o0[:], o1[:], ALU.add, [o0t, o1t], [o0t])
                ACT(sqq[:], o0[:], AF.Square, [o0t], [sqqt])
                pn, pnt = P.ps([4, 5], c_n)
                MM(pn[:, :n], ones_bf[:], sqq[:], True, True, [ones_t, sqqt], pnt)
                ACT(rs[:], pn[:, :n], AF.Sqrt, [pnt, eps_t], [rst], scale=1.0 / 128, bias=epsc[:, 0:1])
                P.op("dve", lambda e: e.reciprocal(rs[:], rs[:]), reads=[rst], writes=[rst])
                ACT(zb[:], zb[:], AF.Silu, [zbt], [zbt])
                TTo(o0[:], o0[:], rs[:], ALU.mult, [o0t, rst], [o0t])
                STT(yg[:, hh, :], o0[:], V[:, ono:ono + 1], zb[:], ALU.mult, ALU.mult, [o0t, Vt, zbt], [yg_t])
            for m in range(8):
                ps, pst = P.ps([1, 2, 3], c_y)
                for hh in range(8):
                    MM(ps[:, :n], wo[:, hh, m * 128:(m + 1) * 128], yg[:, hh, :], hh == 0, hh == 7, [wo_t, yg_t], pst)
                ACT(yb[:, m, :], ps[:, :n], AF.Copy, [pst], [yb_t])
            post_res(yb, yb_t, n, 1, 0, GG_M, xT, xt, sq, sq_t, rstd, rstd_t, 0, yb, yb_t)
            for k in range(8):
                P.dma("sp", XM[k, :, t0:t0 + n], yb[:, k, :], reads=[yb_t], writes=[XM2t])
        P.barrier()
    if upto == "H":
        return finish(nc, P, outT)

    with ExitStack() as ph:
        rt, rt_t = sb("rt", [128, 8, 8], F32, ph)
        P.dma("sp", rt[:], router, writes=[rt_t])
        Sel, Sel_t = sb("Sel2", [16, 16, 128], F32, ph)
        P.dma("sp", Sel[:], selc, writes=[Sel_t])
        xT, xt = sb("xTi", [128, 8, 512], F32, ph)
        tmp, tmp_t = sb("tmpi", [128, 8, 512], F32, ph)
        h2, h2_t = sb("h2i", [128, 8, 512], BF16, ph)
        sq, sq_t = sb("sqi", [128, 8, 512], BF16, ph)
        rstd, rstd_t = sb("rstdi", [128, 512], F32, ph)
        gg, gg_t = sb("ggi", [128, 28, 512], BF16, ph)
        yacc, yacc_t = sb("yacc", [128, 8, 512], F32, ph)
        cwb, cwb_t = sb("cwb", [128, 8, 512], F32, ph)
        w1bb = [sb("w1b%d" % i, [128, 8, 512], BF16, ph) for i in range(2)]
        w3bb = [sb("w3b%d" % i, [128, 8, 512], BF16, ph) for i in range(2)]
        w2bb = [sb("w2bi%d" % i, [128, 28, 128], BF16, ph) for i in range(2)]
        slb = [sb("sli%d" % i, [128, 512], F32, ph) for i in range(2)]
        tb_ = [sb("tbi%d" % i, [128, 512], F32, ph) for i in range(2)]
        lg, lg_t = sb("lg", [128, 4, 8], F32, ph)
        l2, l2_t = sb("l2", [128, 4, 8], F32, ph)
        mk1, mk1_t = sb("mk1", [128, 4, 8], F32, ph)
        mk2, mk2_t = sb("mk2", [128, 4, 8], F32, ph)
        comb, comb_t = sb("comb", [128, 4, 8], F32, ph)
        m1, m1_t = sb("m1", [128, 4], F32, ph)
        m2, m2_t = sb("m2", [128, 4], F32, ph)
        g1, g1_t = sb("g1", [128, 4], F32, ph)
        g2, g2_t = sb("g2", [128, 4], F32, ph)
        cT, cT_t = sb("cTm", [8, 512], F32, ph)
        c_a, c_b, c_w, c_w2, c_s = [0], [0], [0], [0], [0]
        OUTt = Tr()
        NEXP = int(os.environ.get('NEXP', 8))
        for (t0, n) in LT512[:int(os.environ.get('NTI', 8))]:
            for k in range(8):
                P.dma("sp", xT[:, k, :], XM[k, :, t0:t0 + n], reads=[XM2t], writes=[xt])
            rms_rstd(xT[:, :, :], xt, n, sq, sq_t, rstd, rstd_t, 0)
            for k in range(8):
                TTo(tmp[:, k, :], xT[:, k, :], rstd[:, :], ALU.mult, [xt, rstd_t], [tmp_t])
            for k in range(8):
                ACT(tmp[:, k, :], tmp[:, k, :], AF.Identity, [tmp_t, DVt], [tmp_t], scale=dv(1, 0, GM_F, k), bias=dv(1, 0, SH_F, k))
            for k in range(8):
                CP(h2[:, k, :], tmp[:, k, :], [tmp_t], [h2_t])
            pr, prt = P.psl[6]
            for b in range(4):
                for k in range(8):
                    MM(pr[:, b * 8:(b + 1) * 8], tmp[:, k, b * 128:(b + 1) * 128], rt[:, k, :], k == 0, k == 7, [tmp_t, rt_t], prt)
            ACT(lg[:].rearrange("p b e -> p (b e)"), pr[:, :32], AF.Copy, [prt], [lg_t])
            P.op("dve", lambda e: e.tensor_reduce(out=m1[:], in_=lg[:], axis=AX.X, op=ALU.max), reads=[lg_t], writes=[m1_t])
            for b in range(4):
                TS(mk1[:, b, :], lg[:, b, :], m1[:, b:b + 1], 0.0, ALU.is_equal, ALU.add, [lg_t, m1_t], [mk1_t])
            STT(l2[:], mk1[:], -1e30, lg[:], ALU.mult, ALU.add, [mk1_t, lg_t], [l2_t])
            P.op("dve", lambda e: e.tensor_reduce(out=m2[:], in_=l2[:], axis=AX.X, op=ALU.max), reads=[l2_t], writes=[m2_t])
            for b in range(4):
                TS(mk2[:, b, :], l2[:, b, :], m2[:, b:b + 1], 0.0, ALU.is_equal, ALU.add, [l2_t, m2_t], [mk2_t])
            TTo(g1[:], m1[:], m2[:], ALU.subtract, [m1_t, m2_t], [g1_t])
            ACT(g1[:], g1[:], AF.Sigmoid, [g1_t], [g1_t])
            TS(g2[:], g1[:], -1.0, 1.0, ALU.mult, ALU.add, [g1_t], [g2_t])
            for b in range(4):
                TS(comb[:, b, :], mk1[:, b, :], g1[:, b:b + 1], 0.0, ALU.mult, ALU.add, [mk1_t, g1_t], [comb_t])
                STT(comb[:, b, :], mk2[:, b, :], g2[:, b:b + 1], comb[:, b, :], ALU.mult, ALU.add, [mk2_t, g2_t, comb_t], [comb_t])
            pt, ptt = P.psl[7]
            for b in range(4):
                MM(pt[:8, b * 128:(b + 1) * 128], comb[:, b, :], identF[:], True, True, [comb_t, identF_t], ptt)
            ACT(cT[:], pt[:8, :512], AF.Copy, [ptt], [cT_t])
            for e in range(8):
                ps, pst = P.ps([1, 2, 3, 4], c_a)
                MM(ps[:, :512], Sel[:8, e, :], cT[:], True, True, [Sel_t, cT_t], pst)
                ACT(cwb[:, e, :], ps[:, :512], AF.Copy, [pst], [cwb_t])
            for e in range(NEXP):
                w1v = moe_w1[e].rearrange("(k p) n -> p k n", p=128)
                w3v = moe_w3[e].rearrange("(k p) n -> p k n", p=128)
                w2v = moe_w2[e].rearrange("(c p) n -> p c n", p=128)
                for cb in range(7):
                    w1b, w1bt = w1bb[c_w[0] % 2]
                    w3b, w3bt = w3bb[c_w[0] % 2]
                    c_w[0] += 1
                    for k in range(8):
                        P.dma("pool", w1b[:, k, :], w1v[:, k, cb * 512:(cb + 1) * 512], writes=[w1bt])
                        P.dma("pool", w3b[:, k, :], w3v[:, k, cb * 512:(cb + 1) * 512], writes=[w3bt])
                    for c4 in range(4):
                        c = cb * 4 + c4
                        ps1, ps1t = P.ps([1, 2, 3, 4], c_a)
                        for k in range(8):
                            MM(ps1[:, :n], w1b[:, k, c4 * 128:(c4 + 1) * 128], h2[:, k, :], k == 0, k == 7, [w1bt, h2_t], ps1t)
                        ps3, ps3t = P.ps([1, 2, 3, 4], c_a)
                        for k in range(8):
                            MM(ps3[:, :n], w3b[:, k, c4 * 128:(c4 + 1) * 128], h2[:, k, :], k == 0, k == 7, [w3bt, h2_t], ps3t)
                        sl, slt = slb[c_s[0] % 2]
                        tb, tbt = tb_[c_s[0] % 2]
                        c_s[0] += 1
                        ACT(sl[:], ps1[:, :n], AF.Silu, [ps1t], [slt])
                        TTo(tb[:], sl[:], ps3[:, :n], ALU.mult, [slt, ps3t], [tbt])
                        TTo(gg[:, c, :], tb[:], cwb[:, e, :], ALU.mult, [tbt, cwb_t], [gg_t])
                for m in range(8):
                    w2b, w2bt = w2bb[c_w2[0] % 2]
                    c_w2[0] += 1
                    for c7 in range(0, 28, 7):
                        P.dma("pool", w2b[:, c7:c7 + 7, :], w2v[:, c7:c7 + 7, m * 128:(m + 1) * 128], writes=[w2bt])
                    ps, pst = P.ps([5, 6, 7], c_b)
                    for c in range(28):
                        MM(ps[:, :n], w2b[:, c, :], gg[:, c, :], c == 0, c == 27, [w2bt, gg_t], pst)
                    if e == 0:
                        ACT(yacc[:, m, :], ps[:, :n], AF.Copy, [pst], [yacc_t])
                    else:
                        TTo(yacc[:, m, :], yacc[:, m, :], ps[:, :n], ALU.add, [pst, yacc_t], [yacc_t])
            post_res(yacc, yacc_t, n, 1, 0, GG_F, xT, xt, sq, sq_t, rstd, rstd_t, 0, yacc, yacc_t)
            for k in range(8):
                P.dma("sp", outT[k, :, t0 - CTX:t0 - CTX + n], yacc[:, k, :], reads=[yacc_t], writes=[OUTt])
        P.barrier()
    return finish(nc, P, outT)


def finish(nc, P, outT):
    P.barrier()
    return nc, P


def _wk(w):
    w = np.asarray(w, np.float32)
    K, N = w.shape
    return np.ascontiguousarray(w.reshape(K // 128, 128, N).transpose(1, 0, 2))


def host_inputs(inp):
    shared = {}
    shared["vecs"] = vec_layout(inp).pack()
    for l in (0, 1):
        shared["l%d_mod_w" % l] = _wk(inp["l%d_mod_w" % l])
    shared["w0_in"] = _wk(inp["l0_w_in"])
    shared["pool_w"] = np.ascontiguousarray(np.asarray(inp["l0_pool_w"], np.float32).transpose(1, 0, 2))
    wo = np.asarray(inp["l0_w_out"], np.float32)
    shared["w0_outp"] = _wk(wo[:512])
    shared["w0_outa"] = np.ascontiguousarray(wo[512:].reshape(8, 64, 1024).transpose(1, 0, 2))
    shared["ffn_w1"] = _wk(inp["l0_ffn_w1"])
    shared["ffn_w3"] = _wk(inp["l0_ffn_w3"])
    shared["ffn_w2"] = _wk(inp["l0_ffn_w2"])
    cos, sin = _rope_tables()
    shared["rope_cos"], shared["rope_sin"] = np.ascontiguousarray(cos[:64]), np.ascontiguousarray(sin[:64])
    shared["rotT"] = np.ascontiguousarray(_rot_lhsT()[:64, :64])
    shared["identf"] = np.eye(128, dtype=np.float32)
    kk = np.arange(128)[:, None]
    qq = np.arange(128)[None, :]
    m = np.zeros((128, 2, 512), np.float32)
    m[:, 0, :] = np.tile((qq <= kk).astype(np.float32), (1, 4))
    m[:, 1, :] = np.tile((kk <= qq).astype(np.float32), (1, 4))
    shared["masks"] = m
    shared["invc_lat"] = np.ascontiguousarray(np.broadcast_to(_pool_invcnt(LAT)[None], (128, 4, LAT)))
    shared["invc_ctx"] = np.ascontiguousarray(np.broadcast_to(_pool_invcnt(CTX)[None], (128, 4, CTX)))
    shared["w1_in"] = _wk(inp["l1_w_in"])
    shared["w1_out"] = _wk(inp["l1_w_out"])
    sel = np.zeros((16, 16, 128), np.float32)
    for k0 in range(16):
        sel[k0, k0, :] = 1.0
    shared["selc"] = sel
    ii = np.arange(64)[:, None]
    jj = np.arange(64)[None, :]
    gmk = np.zeros((64, 4, 64), np.float32)
    gmk[:, 0, :] = ii > jj
    gmk[:, 1, :] = ii >= jj
    gmk[:, 2, :] = ii < jj
    gmk[:, 3, :] = ii <= jj
    shared["gmask"] = gmk
    trm = np.zeros((64, 2, 64), np.float32)
    trm[:, 0, :] = ii <= jj
    trm[:, 1, :] = ii >= jj
    shared["tri"] = trm
    shared["router"] = _wk(inp["l1_router"])
    for e in range(8):
        shared["moe_w1_%d" % e] = np.ascontiguousarray(np.asarray(inp["l1_moe_w1"][e], np.float32))
        shared["moe_w3_%d" % e] = np.ascontiguousarray(np.asarray(inp["l1_moe_w3"][e], np.float32))
        shared["moe_w2_%d" % e] = np.ascontiguousarray(np.asarray(inp["l1_moe_w2"][e], np.float32))
    maps = []
    x = np.asarray(inp["x"], np.float32)
    ctx = np.asarray(inp["ctx"], np.float32)
    c = np.asarray(inp["c"], np.float32)
    cc = np.asarray(inp["c_ctx"], np.float32)
    for b in range(NCORES):
        d = dict(shared)
        xt = np.concatenate([ctx[b], x[b]], axis=0).T
        d["xin"] = np.ascontiguousarray(xt.reshape(8, 128, TT))
        d["cT"] = np.ascontiguousarray(np.stack([_colvec(c[b]), _colvec(cc)], axis=-1))
        maps.append(d)
    return maps


_CACHE = {}


def kernel(**inputs):
    if "nc" not in _CACHE:
        _CACHE["nc"] = build()[0]
    nc = _CACHE["nc"]
    maps = host_inputs(inputs)
    res = run_bass_kernel_spmd(nc, maps, core_ids=list(range(NCORES)))
    out = np.stack([np.ascontiguousarray(r["outT"].reshape(1024, LAT).T) for r in res.results], axis=0)
    return out.astype(np.float32)
```

```python
import os
import numpy as np
from contextlib import ExitStack
import concourse.bass as bass
import concourse.mybir as mybir
from concourse.bass_utils import run_bass_kernel_spmd

F32 = mybir.dt.float32
BF16 = mybir.dt.bfloat16
AF = mybir.ActivationFunctionType
ALU = mybir.AluOpType
AX = mybir.AxisListType

D = 1024
LAT = 4096
CTX = 256
TT = LAT + CTX
EPS = 1e-6
NCORES = 8


class Tr:
    __slots__ = ("w", "r", "x")

    def __init__(self, x=False):
        self.w = None
        self.r = {}
        self.x = x


class Prog:
    NDMA = 24

    def __init__(self, nc):
        self.nc = nc
        self.es = ExitStack()
        self.eng = {"pe": nc.tensor, "act": nc.scalar, "dve": nc.vector, "pool": nc.gpsimd, "sp": nc.sync}
        self.semh = {}
        for k in self.eng:
            self.semh[k] = self.es.enter_context(nc.semaphore("s_" + k))
        self.cnt = {k: 0 for k in self.eng}
        self.epoch = {k: 0 for k in self.eng}
        self.ekey = {k: k for k in self.eng}
        self.hist = []
        self.waited = {k: {} for k in self.eng}
        self.dcnt = [0] * self.NDMA
        self.dnext = 0
        for i in range(self.NDMA):
            self.semh["d%d" % i] = self.es.enter_context(nc.semaphore("sd%d" % i))
        self.nps = 0
        self.psl = []
        for i in range(8):
            t = self.es.enter_context(nc.psum_tensor("ps%d" % i, [128, 512], F32))
            self.psl.append((t, Tr(True)))
        self.n_ins = 0

    def ps(self, banks, ctr):
        t = self.psl[banks[ctr[0] % len(banks)]]
        ctr[0] += 1
        return t

    def _wait(self, e, deps):
        best = {}
        for d in deps:
            if d is None:
                continue
            k, v = d
            if v > best.get(k, 0):
                best[k] = v
        for k, v in best.items():
            if e == "pe" and (k == "pe" or k.startswith("pe#")):
                continue
            if self.waited[e].get(k, 0) >= v:
                continue
            self.eng[e].wait_ge(self.semh[k], v)
            self.waited[e][k] = v

    @staticmethod
    def _deps(reads, writes):
        deps = []
        for t in reads:
            if t.w is not None:
                deps.append(t.w)
            if t.x:
                deps.extend(t.r.items())
        for t in writes:
            if t.w is not None:
                deps.append(t.w)
            deps.extend(t.r.items())
        return deps

    @staticmethod
    def _mark(me, reads, writes):
        k, v = me
        for t in reads:
            if t.r.get(k, 0) < v:
                t.r[k] = v
        for t in writes:
            t.w = me
            t.r = {}

    EPOCH = 12000

    def op(self, e, fn, reads=(), writes=()):
        if self.cnt[e] >= self.EPOCH:
            self.epoch[e] += 1
            self.ekey[e] = "%s#%d" % (e, self.epoch[e])
            self.semh[self.ekey[e]] = self.es.enter_context(self.nc.semaphore("s_%s_%d" % (e, self.epoch[e])))
            self.cnt[e] = 0
        self._wait(e, self._deps(reads, writes))
        ins = fn(self.eng[e])
        self.cnt[e] += 1
        key = self.ekey[e]
        ins.then_inc(self.semh[key], 1)
        self._mark((key, self.cnt[e]), reads, writes)
        self.n_ins += 1
        return ins

    def dma(self, q, out, in_, reads=(), writes=()):
        slot = self.dnext
        self.dnext = (slot + 1) % self.NDMA
        deps = self._deps(reads, writes)
        key = "d%d" % slot
        if self.dcnt[slot] > 0:
            deps.append((key, 16 * self.dcnt[slot]))
        self._wait(q, deps)
        ins = self.eng[q].dma_start(out=out, in_=in_)
        self.dcnt[slot] += 1
        ins.then_inc(self.semh[key], 16)
        self._mark((key, 16 * self.dcnt[slot]), reads, writes)
        self.n_ins += 1
        return ins

    def barrier(self):
        deps = [(self.ekey[k], self.cnt[k]) for k in self.eng if self.cnt[k] > 0]
        deps += [("d%d" % i, 16 * self.dcnt[i]) for i in range(self.NDMA) if self.dcnt[i] > 0]
        for e in self.eng:
            best = {}
            for k, v in deps:
                best[k] = v
            for k, v in best.items():
                if self.waited[e].get(k, 0) >= v:
                    continue
                self.eng[e].wait_ge(self.semh[k], v)
                self.waited[e][k] = v
        for (t, tr) in self.psl:
            tr.w = None
            tr.r = {}


def _rope_tables():
    half = 32
    inv = 10000.0 ** (-np.arange(0, half, 2, dtype=np.float32) / half)
    t = np.arange(LAT)
    rows = (t // 64).astype(np.float32)
    cols = (t % 64).astype(np.float32)
    cos = np.zeros((64, LAT), np.float32)
    sin = np.zeros((64, LAT), np.float32)
    for i in range(64):
        pos = rows if i < 32 else cols
        ang = pos * inv[i % 16]
        cos[i] = np.cos(ang)
        sin[i] = np.sin(ang)
    cos = np.concatenate([cos, cos], 0)
    sin = np.concatenate([sin, sin], 0)
    return cos, sin


def _rot_lhsT():
    R = np.zeros((128, 128), np.float32)
    for hb in (0, 64):
        for blk in (0, 32):
            for i in range(16):
                R[hb + blk + i, hb + blk + i + 16] = -1.0
                R[hb + blk + i + 16, hb + blk + i] = 1.0
    return np.ascontiguousarray(R.T)


def _pool_invcnt(L):
    t = np.arange(L)
    out = np.zeros((4, L), np.float32)
    for gi, w in enumerate((2, 4, 8, 16)):
        lo = np.clip(t - w // 2, 0, L)
        hi = np.clip(t + w // 2, 0, L)
        out[gi] = 1.0 / (hi - lo).astype(np.float32)
    return out


def _colvec(v):
    return np.ascontiguousarray(np.asarray(v, np.float32).reshape(-1, 128).T)


class VecPack:
    def __init__(self):
        self.cols = []
        self.off = {}
        self.n = 0

    def add(self, name, arr):
        arr = np.asarray(arr, np.float32)
        assert arr.shape[0] == 128
        self.off[name] = self.n
        self.cols.append(arr)
        self.n += arr.shape[1]

    def pack(self):
        return np.ascontiguousarray(np.concatenate(self.cols, axis=1))


_VEC_INPUTS = ("l0_mix_pre", "l0_mix_post", "l0_ffn_pre", "l0_ffn_post", "l0_mod_b", "l0_mod_w",
               "l1_mix_pre", "l1_mix_post", "l1_ffn_pre", "l1_ffn_post", "l1_mod_b", "l1_mod_w")


def vec_layout(inp=None):
    z = lambda *s: np.zeros(s, np.float32)
    g = (lambda k, shp: np.asarray(inp[k], np.float32)) if inp is not None else (lambda k, shp: z(*shp))
    vp = VecPack()
    for l in (0, 1):
        for nm in ("mix_pre", "mix_post", "ffn_pre", "ffn_post"):
            vp.add("l%d_%s" % (l, nm), _colvec(g("l%d_%s" % (l, nm), (1024,))))
        vp.add("l%d_mod_b" % l, _colvec(g("l%d_mod_b" % l, (6144,))))
    vp.add("pool_scale", _colvec(g("l0_pool_scale", (512,))))
    sk = np.asarray(g("l0_sinks", (8,)), np.float32).reshape(1, 8)
    vp.add("sinks", np.broadcast_to(sk, (128, 8)))
    cw = g("l1_conv_w", (5, 3072))
    vp.add("conv_w", np.concatenate([_colvec(cw[j]) for j in range(5)], axis=1))
    vp.add("out_norm", np.asarray(g("l1_out_norm", (128,)), np.float32).reshape(128, 1))
    al = g("l1_a_log", (2, 8)).reshape(1, 16)
    db = g("l1_dt_bias", (2, 8)).reshape(1, 16)
    vp.add("a_log", np.broadcast_to(al, (128, 16)))
    vp.add("dt_bias", np.broadcast_to(db, (128, 16)))
    return vp


TILES512 = [(0, 256)] + [(256 + 512 * i, 512) for i in range(8)]
TILES256 = [(256 * i, 256) for i in range(17)]
NBLK = TT // 128


def build(upto="all", dbg=()):
    nc = bass.Bass("TRN2", target_bir_lowering=False)
    P = Prog(nc)
    es = P.es
    VO = vec_layout(None).off
    NV = vec_layout(None).n

    def din(name, shape, dt=F32):
        return nc.dram_tensor(name, list(shape), dt, kind="ExternalInput").ap()

    def dscr(name, shape, dt=F32):
        kind = "ExternalOutput" if name in dbg else "Internal"
        return nc.dram_tensor(name, list(shape), dt, kind=kind).ap()

    xin = din("xin", [8, 128, TT])
    cT = din("cT", [128, 8, 2])
    vecs = din("vecs", [128, NV])
    modw = [din("l%d_mod_w" % l, [128, 8, 6144]) for l in (0, 1)]
    w0_in = din("w0_in", [128, 8, 1280])
    pool_w = din("pool_w", [128, 4, 128])
    w0_outp = din("w0_outp", [128, 4, 1024])
    w0_outa = din("w0_outa", [64, 8, 1024])
    ffn_w1 = din("ffn_w1", [128, 8, 2816])
    ffn_w3 = din("ffn_w3", [128, 8, 2816])
    ffn_w2 = din("ffn_w2", [128, 22, 1024])
    rope_cos = din("rope_cos", [64, LAT])
    rope_sin = din("rope_sin", [64, LAT])
    rotT = din("rotT", [64, 64])
    masks = din("masks", [128, 2, 512])
    invc_lat = din("invc_lat", [128, 4, LAT])
    invc_ctx = din("invc_ctx", [128, 4, CTX])
    identf = din("identf", [128, 128])
    outT = nc.dram_tensor("outT", [8, 128, LAT], F32, kind="ExternalOutput").ap()

    UT = dscr("UT", [4, 128, TT])
    XM = dscr("XM", [8, 128, TT])
    X1 = dscr("X1", [8, 128, TT])

    def sb(name, shape, dt=F32, stack=None):
        t = (stack or es).enter_context(nc.sbuf_tensor(name, list(shape), dt))
        return t, Tr()

    def ACT(out, in_, func, reads, writes, **kw):
        return P.op("act", lambda e: e.activation(out=out, in_=in_, func=func, **kw), reads, writes)

    def TTo(out, a, b, op, reads, writes, eng="dve"):
        return P.op(eng, lambda e: e.tensor_tensor(out=out, in0=a, in1=b, op=op), reads, writes)

    def STT(out, in0, scalar, in1, op0, op1, reads, writes):
        return P.op("dve", lambda e: e.scalar_tensor_tensor(out=out, in0=in0, scalar=scalar, in1=in1, op0=op0, op1=op1),
                    reads, writes)

    def TS(out, in0, s1, s2, op0, op1, reads, writes):
        return P.op("dve", lambda e: e.tensor_scalar(out=out, in0=in0, scalar1=s1, scalar2=s2, op0=op0, op1=op1),
                    reads, writes)

    def CP(out, in_, reads, writes, eng="dve"):
        return P.op(eng, lambda e: e.tensor_copy(out=out, in_=in_), reads, writes)

    def MM(ps, lhsT, rhs, start, stop, reads, pst):
        return P.op("pe", lambda e: e.matmul(ps, lhsT, rhs, start=start, stop=stop), reads, [pst])

    V, Vt = sb("V", [128, NV])
    DV, DVt = sb("DV", [128, 2 * 2 * 48])
    ones_bf, ones_t = sb("ones_bf", [128, 128], BF16)
    ones_f, onesf_t = sb("ones_f", [128, 128], F32)
    epsc, eps_t = sb("epsc", [128, 1])
    identF, identF_t = sb("identF", [128, 128], F32)
    identB, identB_t = sb("identB", [128, 128], BF16)
    P.op("dve", lambda e: e.memset(ones_bf[:], 1.0), writes=[ones_t])
    P.op("dve", lambda e: e.memset(ones_f[:], 1.0), writes=[onesf_t])
    P.op("dve", lambda e: e.memset(epsc[:], EPS), writes=[eps_t])
    P.dma("sp", V[:], vecs, writes=[Vt])
    P.dma("sp", identF[:], identf, writes=[identF_t])
    CP(identB[:], identF[:], [identF_t], [identB_t])

    def dv(l, s, which, k=None):
        o = ((l * 2 + s) * 6 + which) * 8
        return DV[:, o:o + 8] if k is None else DV[:, o + k:o + k + 1]

    GM_M, SH_M, GG_M, GM_F, SH_F, GG_F = range(6)

    with ExitStack() as ph:
        scT, sc_t = sb("scT", [128, 8, 2], F32, ph)
        modT, mod_t = sb("modT", [128, 48, 2], F32, ph)
        wbuf = [sb("modw%d" % i, [128, 8, 1024], F32, ph) for i in range(2)]
        P.dma("sp", scT[:], cT, writes=[sc_t])
        ACT(scT[:], scT[:], AF.Silu, [sc_t], [sc_t])
        for l in (0, 1):
            ps, pst = P.psl[l]
            for j in range(6):
                wb, wbt = wbuf[j % 2]
                P.dma("sp", wb[:], modw[l][:, :, j * 1024:(j + 1) * 1024], writes=[wbt])
                for kc in range(8):
                    col = (j * 8 + kc) * 2
                    for k in range(8):
                        MM(ps[:, col:col + 2], wb[:, k, kc * 128:(kc + 1) * 128], scT[:, k, :], k == 0, k == 7, [wbt, sc_t], pst)
            mb = VO["l%d_mod_b" % l]
            for s in (0, 1):
                TTo(modT[:, :, s], ps[:, 0:96].rearrange("p (j t) -> p j t", t=2)[:, :, s], V[:, mb:mb + 48], ALU.add,
                    [pst, Vt], [mod_t])
                pre = "l%d_" % l
                for (which_gm, which_sh, which_gg, base, npre, npost) in (
                        (GM_M, SH_M, GG_M, 0, "mix_pre", "mix_post"), (GM_F, SH_F, GG_F, 24, "ffn_pre", "ffn_post")):
                    o_pre = VO[pre + npre]
                    o_post = VO[pre + npost]
                    STT(dv(l, s, which_gm), modT[:, base + 8:base + 16, s], 1.0, V[:, o_pre:o_pre + 8], ALU.add, ALU.mult,
                        [mod_t, Vt], [DVt])
                    CP(dv(l, s, which_sh), modT[:, base:base + 8, s], [mod_t], [DVt])
                    TTo(dv(l, s, which_gg), modT[:, base + 16:base + 24, s], V[:, o_post:o_post + 8], ALU.mult,
                        [mod_t, Vt], [DVt])
        P.barrier()

    def rms_rstd(src, src_t, n, sq, sq_t, rstd, rstd_t, bank, nch=8, scale=1.0 / 1024, npart=128):
        ACT(sq[:, :nch, :n], src, AF.Square, [src_t], [sq_t])
        ps, pst = P.psl[bank]
        for k in range(nch):
            MM(ps[:npart, :n], ones_bf[:, :npart], sq[:, k, :n], k == 0, k == nch - 1, [sq_t, ones_t], pst)
        ACT(rstd[:npart, :n], ps[:npart, :n], AF.Sqrt, [pst, eps_t], [rstd_t], scale=scale, bias=epsc[:npart, 0:1])
        P.op("dve", lambda e: e.reciprocal(rstd[:npart, :n], rstd[:npart, :n]), reads=[rstd_t], writes=[rstd_t])

    def norm_mod(xT, x_t, n, l, s, which_gm, which_sh, h, h_t, sq, sq_t, rstd, rstd_t, tmp, tmp_t, bank):
        rms_rstd(xT[:, :, :n], x_t, n, sq, sq_t, rstd, rstd_t, bank)
        for k in range(8):
            TTo(tmp[:, k, :n], xT[:, k, :n], rstd[:, :n], ALU.mult, [x_t, rstd_t], [tmp_t])
        for k in range(8):
            ACT(h[:, k, :n], tmp[:, k, :n], AF.Identity, [tmp_t, DVt], [h_t],
                scale=dv(l, s, which_gm, k), bias=dv(l, s, which_sh, k))

    def post_res(y, y_t, n, l, s, which_gg, xres, xres_t, sq, sq_t, rstd, rstd_t, bank, out, out_t):
        rms_rstd(y[:, :, :n], y_t, n, sq, sq_t, rstd, rstd_t, bank)
        for k in range(8):
            TTo(y[:, k, :n], y[:, k, :n], rstd[:, :n], ALU.mult, [y_t, rstd_t], [y_t])
        for k in range(8):
            STT(out[:, k, :n], y[:, k, :n], dv(l, s, which_gg, k), xres[:, k, :n], ALU.mult, ALU.add,
                [y_t, xres_t, DVt], [out_t])

    if upto == "p0":
        return finish(nc, P, outT)

    with ExitStack() as mix:
        QT, _ = sb("QT", [64, 8, TT], BF16, mix)
        KT, _ = sb("KT", [64, 2, TT], BF16, mix)
        VT, _ = sb("VT", [128, NBLK, 128], BF16, mix)
        QTt = [Tr() for _ in range(NBLK)]
        KTt = [Tr() for _ in range(NBLK)]
        VTt = [Tr() for _ in range(NBLK)]
        POt = [Tr() for _ in range(NBLK)]
        UTt = Tr()
        xin_t = Tr()

        with ExitStack() as ph:
            w_in, w_in_t = sb("w_in", [128, 8, 1280], BF16, ph)
            rotf, rotf_t = sb("rotf", [64, 64], F32, ph)
            rot, rot_t = sb("rot", [64, 64], BF16, ph)
            for k in range(8):
                P.dma("pool", w_in[:, k, :], w0_in[:, k, :], writes=[w_in_t])
            P.dma("sp", rotf[:], rotT, writes=[rotf_t])
            CP(rot[:], rotf[:], [rotf_t], [rot_t])
            xTb = [sb("xT%d" % i, [128, 8, 512], F32, ph) for i in range(2)]
            cosb = [sb("cos%d" % i, [64, 512], F32, ph) for i in range(2)]
            sinb = [sb("sin%d" % i, [64, 512], F32, ph) for i in range(2)]
            sq, sq_t = sb("sq", [128, 8, 512], BF16, ph)
            rstd, rstd_t = sb("rstd", [128, 512], F32, ph)
            tmp, tmp_t = sb("tmp", [128, 8, 512], F32, ph)
            h, h_t = sb("h", [128, 8, 512], BF16, ph)
            ub = [sb("ub%d" % i, [128, 512], F32, ph) for i in range(2)]
            qb = [sb("qb%d" % i, [64, 512], BF16, ph) for i in range(2)]
            t1 = [sb("t1_%d" % i, [64, 512], F32, ph) for i in range(2)]
            t2 = [sb("t2_%d" % i, [64, 512], F32, ph) for i in range(2)]
            c_proj, c_rot, c_v, c_misc = [0], [0], [0], [0]

            def loadA(i):
                t0, n = TILES512[i]
                xT, xt = xTb[i % 2]
                for k in range(8):
                    P.dma("sp", xT[:, k, :n], xin[k, :, t0:t0 + n], reads=[xin_t], writes=[xt])
                if i > 0:
                    l0 = t0 - CTX
                    P.dma("sp", cosb[i % 2][0][:, :n], rope_cos[:, l0:l0 + n], writes=[cosb[i % 2][1]])
                    P.dma("sp", sinb[i % 2][0][:, :n], rope_sin[:, l0:l0 + n], writes=[sinb[i % 2][1]])

            loadA(0)
            NTA = int(os.environ.get('NTA', len(TILES512)))
            for i, (t0, n) in enumerate(TILES512[:NTA]):
                if i + 1 < NTA:
                    loadA(i + 1)
                s = 1 if i == 0 else 0
                blks = list(range(t0 // 128, (t0 + n) // 128))
                xT, xt = xTb[i % 2]
                norm_mod(xT, xt, n, 0, s, GM_M, SH_M, h, h_t, sq, sq_t, rstd, rstd_t, tmp, tmp_t, 0)
                for m in range(0 if os.environ.get('NOU') else 4):
                    ps, pst = P.ps([1, 2, 3], c_proj)
                    for k in range(8):
                        MM(ps[:, :n], w_in[:, k, m * 128:(m + 1) * 128], h[:, k, :n], k == 0, k == 7, [w_in_t, h_t], pst)
                    u, ut = ub[c_misc[0] % 2]
                    c_misc[0] += 1
                    ACT(u[:, :n], ps[:, :n], AF.Copy, [pst], [ut])
                    P.dma("sp", UT[m, :, t0:t0 + n], u[:, :n], reads=[ut], writes=[UTt])
                for hh in range(int(os.environ.get('NHH', 10))):
                    ps, pst = P.ps([1, 2, 3], c_proj)
                    c0 = 512 + hh * 64
                    for k in range(8):
                        MM(ps[:64, :n], w_in[:, k, c0:c0 + 64], h[:, k, :n], k == 0, k == 7, [w_in_t, h_t], pst)
                    dst = QT[:, hh, t0:t0 + n] if hh < 8 else KT[:, hh - 8, t0:t0 + n]
                    dst_t = [QTt[b] for b in blks] if hh < 8 else [KTt[b] for b in blks]
                    if i == 0 or os.environ.get('NOROPE'):
                        ACT(dst, ps[:64, :n], AF.Copy, [pst], dst_t)
                    else:
                        q, qt = qb[c_misc[0] % 2]
                        a1, a1t = t1[c_misc[0] % 2]
                        a2, a2t = t2[c_misc[0] % 2]
                        c_misc[0] += 1
                        cs, cst = cosb[i % 2]
                        sn, snt = sinb[i % 2]
                        ACT(q[:, :n], ps[:64, :n], AF.Copy, [pst], [qt])
                        pr, prt = P.ps([4, 5], c_rot)
                        MM(pr[:64, :n], rot[:], q[:, :n], True, True, [rot_t, qt], prt)
                        if os.environ.get('ROPEV') == '1':
                            TTo(a1[:, :n], q[:, :n], cs[:, :n], ALU.mult, [qt, cst], [a1t])
                        else:
                            TTo(a1[:, :n], ps[:64, :n], cs[:, :n], ALU.mult, [pst, cst], [a1t])
                        TTo(a2[:, :n], pr[:64, :n], sn[:, :n], ALU.mult, [prt, snt], [a2t])
                        TTo(dst, a1[:, :n], a2[:, :n], ALU.add, [a1t, a2t], dst_t)
                for b in range(0 if os.environ.get('NOV') else n // 128):
                    ps, pst = P.ps([6, 7], c_v)
                    for k in range(8):
                        MM(ps[:, :128], h[:, k, b * 128:(b + 1) * 128], w_in[:, k, 1152:1280], k == 0, k == 7, [w_in_t, h_t], pst)
                    ACT(VT[:, blks[b], :], ps[:, :128], AF.Copy, [pst], [VTt[blks[b]]])
            P.barrier()
        if upto == "A":
            dq = dscr("dQT", [64, 8, TT], BF16)
            dk = dscr("dKT", [64, 2, TT], BF16)
            dvv = dscr("dVT", [128, NBLK, 128], BF16)
            P.dma("sp", dq, QT[:], reads=QTt)
            P.dma("sp", dk, KT[:], reads=KTt)
            P.dma("sp", dvv, VT[:], reads=VTt)
            return finish(nc, P, outT)

        with ExitStack() as ph:
            msk, msk_t = sb("msk", [128, 2, 512], BF16, ph)
            mskf, mskf_t = sb("mskf", [128, 2, 512], F32, ph)
            P.dma("sp", mskf[:], masks, writes=[mskf_t])
            CP(msk[:], mskf[:], [mskf_t], [msk_t])
            esk, esk_t = sb("esk", [64, 2, 512], F32, ph)
            so = VO["sinks"]
            for hh in range(8):
                ACT(esk[:, hh // 4, (hh % 4) * 128:(hh % 4 + 1) * 128], ones_f[:64, :], AF.Exp, [onesf_t, Vt], [esk_t],
                    scale=V[:64, so + hh:so + hh + 1])
            pts = [sb("pt%d" % i, [128, 512], BF16, ph) for i in range(6)]
            dens = [sb("den%d" % i, [64, 512], F32, ph) for i in range(2)]
            c_s, c_pt, c_nd = [0], [0], [0]
            for tb in range(NBLK):
                if tb < 2:
                    kbs = [(0, None), (1, None)]
                else:
                    kbs = [(0, None), (1, None)]
                    if tb > 2:
                        kbs.append((tb - 1, 0))
                    kbs.append((tb, None))
                    if tb < NBLK - 1:
                        kbs.append((tb + 1, 1))
                for j in range(2):
                    ptl = []
                    for (kb, mi) in kbs:
                        ps, pst = P.ps([0, 1, 2, 3, 4, 5], c_s)
                        MM(ps[:, :].rearrange("p (h q) -> p h q", h=4), KT[:, j, kb * 128:(kb + 1) * 128],
                           QT[:, 4 * j:4 * j + 4, tb * 128:(tb + 1) * 128], True, True, [KTt[kb], QTt[tb]], pst)
                        pt, ptt = pts[c_pt[0] % 6]
                        c_pt[0] += 1
                        ACT(pt[:], ps[:], AF.Exp, [pst], [ptt], scale=0.125)
                        if mi is not None:
                            TTo(pt[:], pt[:], msk[:, mi, :], ALU.mult, [ptt, msk_t], [ptt])
                        ptl.append((pt, ptt, kb))
                    psn, psnt = P.psl[6]
                    psd, psdt = P.psl[7]
                    for ii, (pt, ptt, kb) in enumerate(ptl):
                        MM(psn[:64, :], VT[:, kb, j * 64:(j + 1) * 64], pt[:], ii == 0, ii == len(ptl) - 1, [VTt[kb], ptt], psnt)
                    for ii, (pt, ptt, kb) in enumerate(ptl):
                        MM(psd[:64, :], ones_bf[:, :64], pt[:], ii == 0, ii == len(ptl) - 1, [ones_t, ptt], psdt)
                    dn, dnt = dens[c_nd[0] % 2]
                    c_nd[0] += 1
                    TTo(dn[:], psd[:64, :], esk[:, j, :], ALU.add, [psdt, esk_t], [dnt])
                    P.op("dve", lambda e: e.reciprocal(dn[:], dn[:]), reads=[dnt], writes=[dnt])
                    TTo(QT[:, 4 * j:4 * j + 4, tb * 128:(tb + 1) * 128], psn[:64, :].rearrange("p (h q) -> p h q", h=4),
                        dn[:].rearrange("p (h q) -> p h q", h=4), ALU.mult, [psnt, dnt], [QTt[tb]])
            P.barrier()
        if upto == "B":
            dq = dscr("dAO", [64, 8, TT], BF16)
            P.dma("sp", dq, QT[:], reads=QTt)
            return finish(nc, P, outT)

        PO, _ = sb("PO", [128, 4, TT], BF16, mix)
        with ExitStack() as ph:
            pw, pw_t = sb("pw", [128, 4, 128], BF16, ph)
            P.dma("pool", pw[:], pool_w, writes=[pw_t])
            LP = LAT + 16
            U0, U0t = sb("U0", [128, LP], F32, ph)
            Ba, Bat = sb("Ba", [128, LP], F32, ph)
            Bb, Bbt = sb("Bb", [128, LP], F32, ph)
            ivc, ivct = sb("ivc", [128, LAT], F32, ph)
            dl, dlt = sb("dl", [128, LAT], BF16, ph)
            c_p = [0]
            pso = VO["pool_scale"]
            for g, w in enumerate((2, 4, 8, 16)):
                for (s0, L, ivsrc) in ((0, CTX, invc_ctx), (CTX, LAT, invc_lat)):
                    Lt = L + 16
                    P.op("dve", lambda e: e.memset(U0[:, 0:8], 0.0), writes=[U0t])
                    P.op("dve", lambda e: e.memset(U0[:, 8 + L:16 + L], 0.0), writes=[U0t])
                    P.dma("sp", U0[:, 8:8 + L], UT[g, :, s0:s0 + L], reads=[UTt], writes=[U0t])
                    P.dma("sp", ivc[:, :L], ivsrc[:, g, :], writes=[ivct])
                    src, srct = U0, U0t
                    sh = 1
                    ln = Lt
                    bufs = [(Ba, Bat), (Bb, Bbt)]
                    bi = 0
                    while sh < w:
                        dst, dstt = bufs[bi % 2]
                        bi += 1
                        ln = ln - sh
                        TTo(dst[:, :ln], src[:, :ln], src[:, sh:sh + ln], ALU.add, [srct], [dstt])
                        src, srct = dst, dstt
                        sh *= 2
                    o = 8 - w // 2
                    dst, dstt = bufs[bi % 2]
                    TTo(dst[:, :L], src[:, o:o + L], ivc[:, :L], ALU.mult, [srct, ivct], [dstt])
                    TTo(dl[:, :L], dst[:, :L], U0[:, 8:8 + L], ALU.subtract, [dstt, U0t], [dlt])
                    for c0 in range(0, L, 512):
                        n = min(512, L - c0)
                        ps, pst = P.ps([0, 1, 2, 3], c_p)
                        MM(ps[:, :n], pw[:, g, :], dl[:, c0:c0 + n], True, True, [pw_t, dlt], pst)
                        blks = list(range((s0 + c0) // 128, (s0 + c0 + n) // 128))
                        ACT(PO[:, g, s0 + c0:s0 + c0 + n], ps[:, :n], AF.Copy, [pst, Vt], [POt[b] for b in blks],
                            scale=V[:, pso + g:pso + g + 1])
            P.barrier()
        if upto == "C":
            dq = dscr("dPO", [128, 4, TT], BF16)
            P.dma("sp", dq, PO[:], reads=POt)
            return finish(nc, P, outT)

        with ExitStack() as ph:
            wop, wop_t = sb("wop", [128, 4, 1024], BF16, ph)
            woa, woa_t = sb("woa", [64, 8, 1024], BF16, ph)
            for k in range(4):
                P.dma("pool", wop[:, k, :], w0_outp[:, k, :], writes=[wop_t])
            for k in range(8):
                P.dma("pool", woa[:, k, :], w0_outa[:, k, :], writes=[woa_t])
            xTb = [sb("xTd%d" % i, [128, 8, 512], F32, ph) for i in range(1)]
            yb, yb_t = sb("yb", [128, 8, 512], F32, ph)
            sq, sq_t = sb("sqd", [128, 8, 512], BF16, ph)
            rstd, rstd_t = sb("rstdd", [128, 512], F32, ph)
            XMt = Tr()
            c_y = [0]

            def loadD(i):
                t0, n = TILES512[i]
                xT, xt = xTb[0]
                for k in range(8):
                    P.dma("sp", xT[:, k, :n], xin[k, :, t0:t0 + n], reads=[xin_t], writes=[xt])

            for i, (t0, n) in enumerate(TILES512):
                loadD(i)
                s = 1 if i == 0 else 0
                blks = list(range(t0 // 128, (t0 + n) // 128))
                xT, xt = xTb[0]
                rd = [POt[b] for b in blks] + [QTt[b] for b in blks]
                for m in range(8):
                    ps, pst = P.ps([1, 2, 3, 4], c_y)
                    for g in range(4):
                        MM(ps[:, :n], wop[:, g, m * 128:(m + 1) * 128], PO[:, g, t0:t0 + n], g == 0, False, [wop_t] + rd, pst)
                    for hh in range(8):
                        MM(ps[:, :n], woa[:, hh, m * 128:(m + 1) * 128], QT[:, hh, t0:t0 + n], False, hh == 7, [woa_t] + rd, pst)
                    ACT(yb[:, m, :n], ps[:, :n], AF.Copy, [pst], [yb_t])
                xout, xout_t = yb, yb_t
                post_res(yb, yb_t, n, 0, s, GG_M, xT, xt, sq, sq_t, rstd, rstd_t, 0, xout, xout_t)
                for k in range(8):
                    P.dma("sp", XM[k, :, t0:t0 + n], xout[:, k, :n], reads=[xout_t], writes=[XMt])
            P.barrier()
    if upto == "D":
        return finish(nc, P, outT)

    XMt, X1t = Tr(), Tr()
    with ExitStack() as ph:
        w1, w1_t = sb("w1", [128, 8, 2816], BF16, ph)
        w3, w3_t = sb("w3", [128, 8, 2816], BF16, ph)
        for k in range(8):
            P.dma("pool", w1[:, k, :], ffn_w1[:, k, :], writes=[w1_t])
            P.dma("pool", w3[:, k, :], ffn_w3[:, k, :], writes=[w3_t])
        w2b = [sb("w2b%d" % i, [128, 22, 128], BF16, ph) for i in range(2)]
        xTb = [sb("xTe%d" % i, [128, 8, 256], F32, ph) for i in range(2)]
        sq, sq_t = sb("sqe", [128, 8, 256], BF16, ph)
        rstd, rstd_t = sb("rstde", [128, 256], F32, ph)
        tmp, tmp_t = sb("tmpe", [128, 8, 256], F32, ph)
        h2, h2_t = sb("h2e", [128, 8, 256], BF16, ph)
        gg, gg_t = sb("gge", [128, 22, 256], BF16, ph)
        slb = [sb("sle%d" % i, [128, 256], F32, ph) for i in range(2)]
        xout, xout_t = sb("xoe", [128, 8, 256], F32, ph)
        c_a, c_b, c_w, c_s = [0], [0], [0], [0]

        def loadE(i):
            t0, n = TILES256[i]
            xT, xt = xTb[i % 2]
            for k in range(8):
                P.dma("sp", xT[:, k, :n], XM[k, :, t0:t0 + n], reads=[XMt], writes=[xt])

        loadE(0)
        for i, (t0, n) in enumerate(TILES256):
            if i + 1 < len(TILES256):
                loadE(i + 1)
            s = 1 if i == 0 else 0
            xT, xt = xTb[i % 2]
            norm_mod(xT, xt, n, 0, s, GM_F, SH_F, h2, h2_t, sq, sq_t, rstd, rstd_t, tmp, tmp_t, 0)
            for c in range(22):
                ps1, ps1t = P.ps([1, 2, 3, 4], c_a)
                for k in range(8):
                    MM(ps1[:, :n], w1[:, k, c * 128:(c + 1) * 128], h2[:, k, :n], k == 0, k == 7, [w1_t, h2_t], ps1t)
                ps3, ps3t = P.ps([1, 2, 3, 4], c_a)
                for k in range(8):
                    MM(ps3[:, :n], w3[:, k, c * 128:(c + 1) * 128], h2[:, k, :n], k == 0, k == 7, [w3_t, h2_t], ps3t)
                sl, slt = slb[c_s[0] % 2]
                c_s[0] += 1
                ACT(sl[:, :n], ps1[:, :n], AF.Silu, [ps1t], [slt])
                TTo(gg[:, c, :n], sl[:, :n], ps3[:, :n], ALU.mult, [slt, ps3t], [gg_t])
            for m in range(8):
                wb, wbt = w2b[c_w[0] % 2]
                c_w[0] += 1
                P.dma("pool", wb[:], ffn_w2[:, :, m * 128:(m + 1) * 128], writes=[wbt])
                ps, pst = P.ps([5, 6, 7], c_b)
                for c in range(22):
                    MM(ps[:, :n], wb[:, c, :], gg[:, c, :n], c == 0, c == 21, [wbt, gg_t], pst)
                ACT(tmp[:, m, :n], ps[:, :n], AF.Copy, [pst], [tmp_t])
            post_res(tmp, tmp_t, n, 0, s, GG_F, xT, xt, sq, sq_t, rstd, rstd_t, 0, xout, xout_t)
            for k in range(8):
                P.dma("sp", X1[k, :, t0:t0 + n], xout[:, k, :n], reads=[xout_t], writes=[X1t])
        P.barrier()
    if upto == "E":
        return finish(nc, P, outT)


    w1_in = din("w1_in", [128, 8, 4128])
    w1_out = din("w1_out", [128, 8, 1024])
    selc = din("selc", [16, 16, 128])
    gmask = din("gmask", [64, 4, 64])
    tri = din("tri", [64, 2, 64])
    router = din("router", [128, 8, 8])
    moe_w1 = [din("moe_w1_%d" % e, [1024, 3584]) for e in range(8)]
    moe_w3 = [din("moe_w3_%d" % e, [1024, 3584]) for e in range(8)]
    moe_w2 = [din("moe_w2_%d" % e, [3584, 1024]) for e in range(8)]
    QKV = dscr("QKV", [24, 128, TT], BF16)
    Z1 = dscr("Z1", [8, 128, TT])
    OD = dscr("OD", [2, 8, 128, TT])
    QKVt, Z1t, ODt = Tr(), Tr(), Tr()
    NCH = TT // 64

    def norm_mod_v(xs, x_t, n, l, s, which_gm, which_sh, hs, h_t, sq, sq_t, rstd, rstd_t, tmp, tmp_t, bank):
        rms_rstd(xs, x_t, n, sq, sq_t, rstd, rstd_t, bank)
        for k in range(8):
            TTo(tmp[:, k, :n], xs[:, k, :], rstd[:, :n], ALU.mult, [x_t, rstd_t], [tmp_t])
        for k in range(8):
            ACT(hs[:, k, :], tmp[:, k, :n], AF.Identity, [tmp_t, DVt], [h_t],
                scale=dv(l, s, which_gm, k), bias=dv(l, s, which_sh, k))

    with ExitStack() as l1:
        GT, GT_t = sb("GT", [64, NCH, 16], F32, l1)
        BT, BT_t = sb("BT", [64, NCH, 16], F32, l1)
        with ExitStack() as ph:
            w_in, w_in_t = sb("w1in", [128, 8, 4128], BF16, ph)
            for k in range(8):
                P.dma("pool", w_in[:, k, :], w1_in[:, k, :], writes=[w_in_t])
            nea, nea_t = sb("nea", [64, 16], F32, ph)
            alo, dbo, cwo = VO["a_log"], VO["dt_bias"], VO["conv_w"]
            ACT(nea[:], V[:64, alo:alo + 16], AF.Exp, [Vt], [nea_t])
            ACT(nea[:], nea[:], AF.Copy, [nea_t], [nea_t], scale=-1.0)
            eps128, eps128_t = sb("eps128", [128, 1], F32, ph)
            P.op("dve", lambda e: e.memset(eps128[:], 128.0 * EPS), writes=[eps128_t])
            W = 260
            xTb = [sb("xTf%d" % i, [128, 8, W], F32, ph) for i in range(2)]
            sq, sq_t = sb("sqf", [128, 8, W], BF16, ph)
            rstd, rstd_t = sb("rstdf", [128, W], F32, ph)
            tmp, tmp_t = sb("tmpf", [128, 8, W], F32, ph)
            h, h_t = sb("hf", [128, 8, W], BF16, ph)
            pcb = [sb("pc%d" % i, [128, W], F32, ph) for i in range(2)]
            accb = [sb("acc%d" % i, [128, 256], F32, ph) for i in range(2)]
            silb = [sb("sil%d" % i, [128, 256], F32, ph) for i in range(2)]
            sq2b = [sb("sq2%d" % i, [128, 256], BF16, ph) for i in range(2)]
            rs2b = [sb("rs2%d" % i, [128, 256], F32, ph) for i in range(2)]
            obb = [sb("ob%d" % i, [128, 256], BF16, ph) for i in range(3)]
            zbb = [sb("zb%d" % i, [128, 256], F32, ph) for i in range(2)]
            ta, ta_t = sb("ta", [64, 4, 16], F32, ph)
            te, te_t = sb("te", [64, 4, 16], F32, ph)
            c_a, c_n, c_m, c_o, c_z = [0], [0], [0], [0], [0]

            def rngF(i):
                t0 = 256 * i
                lo = 2 if i in (0, 1) else 0
                hi = 258 if i in (0, 16) else 260
                return t0, lo, hi

            def loadF(i):
                t0, lo, hi = rngF(i)
                xT, xt = xTb[i % 2]
                for k in range(8):
                    P.dma("sp", xT[:, k, lo:hi], X1[k, :, t0 - 2 + lo:t0 - 2 + hi], reads=[X1t], writes=[xt])

            NTF = int(os.environ.get('NTF', 17))
            loadF(0)
            for i in range(NTF):
                if i + 1 < NTF:
                    loadF(i + 1)
                t0, lo, hi = rngF(i)
                nv = hi - lo
                s = 1 if i == 0 else 0
                xT, xt = xTb[i % 2]
                norm_mod_v(xT[:, :, lo:hi], xt, nv, 1, s, GM_M, SH_M, h[:, :, lo:hi], h_t, sq, sq_t, rstd, rstd_t, tmp, tmp_t, 0)
                for m in range(32):
                    ps, pst = P.ps([1, 2, 3], c_a)
                    for k in range(8):
                        MM(ps[:, :nv], w_in[:, k, m * 128:(m + 1) * 128], h[:, k, lo:hi], k == 0, k == 7, [w_in_t, h_t], pst)
                    if m >= 24:
                        zb, zbt = zbb[c_z[0] % 2]
                        c_z[0] += 1
                        ACT(zb[:], ps[:, 2 - lo:258 - lo], AF.Copy, [pst], [zbt])
                        P.dma("sp", Z1[m - 24, :, t0:t0 + 256], zb[:], reads=[zbt], writes=[Z1t])
                        continue
                    pc, pct = pcb[c_m[0] % 2]
                    acc, acct = accb[c_m[0] % 2]
                    sil, silt = silb[c_m[0] % 2]
                    sq2, sq2t = sq2b[c_m[0] % 2]
                    rs2, rs2t = rs2b[c_m[0] % 2]
                    c_m[0] += 1
                    if lo > 0:
                        P.op("dve", lambda e: e.memset(pc[:, 0:2], 0.0), writes=[pct])
                    if hi < W:
                        P.op("dve", lambda e: e.memset(pc[:, 258:260], 0.0), writes=[pct])
                    ACT(pc[:, lo:hi], ps[:, :nv], AF.Copy, [pst], [pct])
                    ACT(acc[:], pc[:, 0:256], AF.Copy, [pct, Vt], [acct], scale=V[:, cwo + m:cwo + m + 1])
                    for j in range(1, 5):
                        STT(acc[:], pc[:, j:j + 256], V[:, cwo + j * 24 + m:cwo + j * 24 + m + 1], acc[:], ALU.mult, ALU.add,
                            [pct, Vt, acct], [acct])
                    ACT(sil[:], acc[:], AF.Silu, [acct], [silt])
                    ob, obt = obb[c_o[0] % 3]
                    c_o[0] += 1
                    if m < 16:
                        ACT(sq2[:], sil[:], AF.Square, [silt], [sq2t])
                        pn, pnt = P.ps([4, 5], c_n)
                        MM(pn[:, :256], ones_bf[:], sq2[:], True, True, [ones_t, sq2t], pnt)
                        if m < 8:
                            ACT(rs2[:], pn[:, :256], AF.Sqrt, [pnt, eps128_t], [rs2t], scale=128.0, bias=eps128[:, 0:1])
                        else:
                            ACT(rs2[:], pn[:, :256], AF.Sqrt, [pnt, eps_t], [rs2t], scale=1.0, bias=epsc[:, 0:1])
                        P.op("dve", lambda e: e.reciprocal(rs2[:], rs2[:]), reads=[rs2t], writes=[rs2t])
                        TTo(ob[:], sil[:], rs2[:], ALU.mult, [silt, rs2t], [obt])
                    else:
                        CP(ob[:], sil[:], [silt], [obt])
                    P.dma("sp", QKV[m, :, t0:t0 + 256], ob[:], reads=[obt], writes=[QKVt])
                pab, pabt = P.psl[6]
                for cc in range(4):
                    for k in range(8):
                        MM(pab[:64, cc * 32:(cc + 1) * 32], h[:, k, 2 + cc * 64:2 + (cc + 1) * 64], w_in[:, k, 4096:4128],
                           k == 0, k == 7, [w_in_t, h_t], pabt)
                for cc in range(4):
                    pv = pab[:64, cc * 32:(cc + 1) * 32].rearrange("p (d ab h) -> p d ab h", d=2, ab=2)
                    TTo(ta[:, cc, :].rearrange("p (d h) -> p d h", d=2), pv[:, :, 0, :],
                        V[:64, dbo:dbo + 16].rearrange("p (d h) -> p d h", d=2), ALU.add, [pabt, Vt], [ta_t])
                ACT(te[:], ta[:], AF.Exp, [ta_t], [te_t])
                ACT(ta[:], te[:], AF.Ln, [te_t, onesf_t], [ta_t], bias=ones_f[:64, 0:1])
                for cc in range(4):
                    c = t0 // 64 + cc
                    pv = pab[:64, cc * 32:(cc + 1) * 32].rearrange("p (d ab h) -> p d ab h", d=2, ab=2)
                    TTo(GT[:, c, :], ta[:, cc, :], nea[:], ALU.mult, [ta_t, nea_t], [GT_t])
                    ACT(BT[:, c, :].rearrange("p (d h) -> p d h", d=2), pv[:, :, 1, :], AF.Sigmoid, [pabt], [BT_t])
            P.barrier()
        if upto == "F":
            dg = dscr("dGT", [64, NCH, 16])
            db = dscr("dBT", [64, NCH, 16])
            P.dma("sp", dg, GT[:], reads=[GT_t])
            P.dma("sp", db, BT[:], reads=[BT_t])
            return finish(nc, P, outT)

        with ExitStack() as ph:
            GAMc, GAMc_t = sb("GAMc", [64, NCH, 16], F32, ph)
            GLb, GLb_t = sb("GLb", [128, NCH, 16], F32, ph)
            CD, CD_t = sb("CD", [128, NCH, 16], F32, ph)
            CKD, CKD_t = sb("CKD", [64, NCH, 16], F32, ph)
            CKB, CKB_t = sb("CKB", [64, NCH, 16], F32, ph)
            GAMr, GAMr_t = sb("GAMr", [16, TT], F32, ph)
            NGAMr, NGAMr_t = sb("NGAMr", [16, TT], F32, ph)
            Sel, Sel_t = sb("Sel", [16, 16, 128], F32, ph)
            gm, gm_t = sb("gm", [64, 4, 64], F32, ph)
            trt, trt_t = sb("trt", [64, 2, 64], F32, ph)
            P.dma("sp", Sel[:], selc, writes=[Sel_t])
            P.dma("sp", gm[:], gmask, writes=[gm_t])
            P.dma("sp", trt[:], tri, writes=[trt_t])
            for c0 in range(0, NCH, 32):
                c1 = min(NCH, c0 + 32)
                n = (c1 - c0) * 16
                rhs = GT[:, c0:c1, :]
                psF, psFt = P.psl[0]
                psB, psBt = P.psl[1]
                psL, psLt = P.psl[2]
                MM(psF[:64, :n].rearrange("p (c x) -> p c x", x=16), trt[:, 0, :], rhs, True, True, [trt_t, GT_t], psFt)
                MM(psB[:64, :n].rearrange("p (c x) -> p c x", x=16), trt[:, 1, :], rhs, True, True, [trt_t, GT_t], psBt)
                MM(psL[:, :n].rearrange("p (c x) -> p c x", x=16), ones_f[:64, :], rhs, True, True, [onesf_t, GT_t], psLt)
                ACT(GAMc[:, c0:c1, 0:8], psF[:64, :n].rearrange("p (c x) -> p c x", x=16)[:, :, 0:8], AF.Copy, [psFt], [GAMc_t])
                ACT(GAMc[:, c0:c1, 8:16], psB[:64, :n].rearrange("p (c x) -> p c x", x=16)[:, :, 8:16], AF.Copy, [psBt], [GAMc_t])
                CP(GLb[:, c0:c1, :], psL[:, :n].rearrange("p (c x) -> p c x", x=16), [psLt], [GLb_t])
            ACT(CD[:], GLb[:], AF.Exp, [GLb_t], [CD_t])
            TTo(CKD[:], GLb[:64], GAMc[:], ALU.subtract, [GLb_t, GAMc_t], [CKD_t])
            ACT(CKD[:], CKD[:], AF.Exp, [CKD_t], [CKD_t])
            ACT(CKB[:], GAMc[:], AF.Exp, [GAMc_t], [CKB_t])
            TTo(CKB[:], CKB[:], BT[:], ALU.mult, [CKB_t, BT_t], [CKB_t])
            for c in range(NCH):
                psr, psrt = P.psl[3 + (c // 8) % 2]
                MM(psr[:16, (c % 8) * 64:(c % 8 + 1) * 64], GAMc[:, c, :], identF[:64, :64], True, True, [GAMc_t, identF_t], psrt)
                if c % 8 == 7 or c == NCH - 1:
                    cb = (c // 8) * 8
                    n = (c + 1 - cb) * 64
                    ACT(GAMr[:, cb * 64:cb * 64 + n], psr[:16, :n], AF.Copy, [psrt], [GAMr_t])
                    ACT(NGAMr[:, cb * 64:cb * 64 + n], psr[:16, :n], AF.Copy, [psrt], [NGAMr_t], scale=-1.0)
            P.barrier()

            cur = {"bank": 0, "used": 0}

            def psg(w=64):
                if cur["used"] + w > 256:
                    cur["bank"] = (cur["bank"] + 1) % 8
                    cur["used"] = 0
                b, o = cur["bank"], cur["used"]
                cur["used"] += w
                t, tr = P.psl[b]
                return t[:, o:o + w], tr

            CHN = []
            for hh in range(8):
                c = {}
                for nm, shp, dt in [("Dm", [64, 64], F32), ("E", [64, 64], F32), ("ELs", [64, 64], F32), ("ELi", [64, 64], F32),
                                    ("M", [64, 64], BF16), ("Y", [64, 64], BF16), ("QKm", [64, 64], BF16), ("QKmT", [64, 64], BF16),
                                    ("Xa", [64, 64], BF16), ("Xb", [64, 64], BF16), ("Ya", [64, 64], BF16), ("Yb", [64, 64], BF16),
                                    ("Pa", [64, 64], BF16), ("Pb", [64, 64], BF16),
                                    ("Kbg", [64, 128], BF16), ("Kd", [64, 128], BF16), ("Vb", [64, 128], BF16), ("vn", [64, 128], BF16),
                                    ("nWT", [128, 64], BF16), ("EGe", [128, 64], F32), ("qg", [128, 64], BF16),
                                    ("S", [128, 128], F32), ("Sb", [128, 128], BF16), ("OB", [128, 256], F32)]:
                    c[nm] = sb("%s%d" % (nm, hh), shp, dt, ph)
                CHN.append(c)
            stg = [sb("stg%d" % i, [128, 24, 256], BF16, ph) for i in range(2)]
            idB = identB[:64, :64]
            idF = identF[:64, :64]
            NEU = 5
            for d in range(2):
                order = list(range(NCH)) if d == 0 else [3, 2, 1, 0] + list(range(NCH - 1, 3, -1))
                order = order[:int(os.environ.get('NSTEP', len(order)))]
                groups = []
                for c in order:
                    if not groups or groups[-1] != c // 4:
                        groups.append(c // 4)
                gpos = {}

                def loadG(gi):
                    g4 = groups[gi]
                    st, stt = stg[gi % 2]
                    for m in range(24):
                        P.dma("sp", st[:, m, :], QKV[m, :, g4 * 256:(g4 + 1) * 256], reads=[QKVt], writes=[stt])
                    gpos[g4] = gi

                for hh in range(8):
                    S, St = CHN[hh]["S"]
                    Sb, Sbt = CHN[hh]["Sb"]
                    P.op("dve", lambda e: e.memset(S[:], 0.0), writes=[St])
                    P.op("dve", lambda e: e.memset(Sb[:], 0.0), writes=[Sbt])
                loadG(0)
                for c in order:
                    g4 = c // 4
                    gi = gpos[g4]
                    if c == order[0] or (c // 4 != prev_c // 4):
                        if gi + 1 < len(groups):
                            loadG(gi + 1)
                    prev_c = c
                    st, stt = stg[gi % 2]
                    o64 = (c % 4) * 64
                    tk = slice(c * 64, (c + 1) * 64)
                    is_lat = c >= 4
                    mS, mI = (0, 1) if d == 0 else (2, 3)
                    T = [dict() for _ in range(8)]
                    for hh in range(8):
                        dh = d * 8 + hh
                        T[hh]["qT"] = st[:, hh, o64:o64 + 64]
                        T[hh]["kT"] = st[:, 8 + hh, o64:o64 + 64]
                        T[hh]["vT"] = st[:, 16 + hh, o64:o64 + 64]
                        ps, pst = psg()
                        MM(ps[:64, :64], GAMr[:, tk], Sel[:, dh, :64], True, False, [GAMr_t, Sel_t], pst)
                        MM(ps[:64, :64], Sel[:, dh, :64], NGAMr[:, tk], False, True, [NGAMr_t, Sel_t], pst)
                        T[hh]["psD"] = (ps, pst)
                    for hh in range(8):
                        ps, pst = T[hh]["psD"]
                        Dm, Dmt = CHN[hh]["Dm"]
                        TS(Dm[:], ps[:64, :64], 0.0, 0.0, ALU.min, ALU.add, [pst], [Dmt])
                    for hh in range(8):
                        Dm, Dmt = CHN[hh]["Dm"]
                        E, Et = CHN[hh]["E"]
                        ACT(E[:], Dm[:], AF.Exp, [Dmt], [Et])
                    for hh in range(8):
                        E, Et = CHN[hh]["E"]
                        ELs, ELst = CHN[hh]["ELs"]
                        ELi, ELit = CHN[hh]["ELi"]
                        TTo(ELs[:], E[:], gm[:, mS, :], ALU.mult, [Et, gm_t], [ELst])
                        TTo(ELi[:], E[:], gm[:, mI, :], ALU.mult, [Et, gm_t], [ELit])
                    for hh in range(8):
                        ps, pst = psg()
                        MM(ps[:64, :64], T[hh]["kT"], T[hh]["kT"], True, True, [stt], pst)
                        T[hh]["psG"] = (ps, pst)
                        ps, pst = psg()
                        MM(ps[:64, :64], T[hh]["qT"], T[hh]["kT"], True, True, [stt], pst)
                        T[hh]["psQ"] = (ps, pst)
                    for hh in range(8):
                        dh = d * 8 + hh
                        ps, pst = T[hh]["psG"]
                        M, Mt = CHN[hh]["M"]
                        ELs, ELst = CHN[hh]["ELs"]
                        STT(M[:], ps[:64, :64], BT[:, c, dh:dh + 1], ELs[:], ALU.mult, ALU.mult, [pst, BT_t, ELst], [Mt])
                        ps, pst = T[hh]["psQ"]
                        QKm, QKmt = CHN[hh]["QKm"]
                        ELi, ELit = CHN[hh]["ELi"]
                        TTo(QKm[:], ps[:64, :64], ELi[:], ALU.mult, [pst, ELit], [QKmt])
                    for hh in range(8):
                        M, Mt = CHN[hh]["M"]
                        QKm, QKmt = CHN[hh]["QKm"]
                        ps, pst = psg()
                        MM(ps[:64, :64], M[:], idB, True, True, [Mt, identB_t], pst)
                        T[hh]["psY"] = (ps, pst)
                        ps, pst = psg()
                        MM(ps[:64, :64], QKm[:], idB, True, True, [QKmt, identB_t], pst)
                        T[hh]["psQT"] = (ps, pst)
                    for hh in range(8):
                        ps, pst = T[hh]["psY"]
                        Y, Yt = CHN[hh]["Y"]
                        Pa, Pat = CHN[hh]["Pa"]
                        ACT(Y[:], ps[:64, :64], AF.Copy, [pst], [Yt])
                        STT(Pa[:], ps[:64, :64], -1.0, idF, ALU.mult, ALU.add, [pst, identF_t], [Pat])
                        ps, pst = T[hh]["psQT"]
                        QKmT, QKmTt = CHN[hh]["QKmT"]
                        ACT(QKmT[:], ps[:64, :64], AF.Copy, [pst], [QKmTt])
                        T[hh]["X"] = CHN[hh]["M"]
                        T[hh]["Yc"] = CHN[hh]["Y"]
                        T[hh]["P"] = CHN[hh]["Pa"]
                    for r in range(NEU):
                        xn, yn, pn = ("Xa", "Ya", "Pb") if r % 2 == 0 else ("Xb", "Yb", "Pa")
                        for hh in range(8):
                            X, Xt = T[hh]["X"]
                            Yc, Yct = T[hh]["Yc"]
                            ps, pst = psg()
                            MM(ps[:64, :64], Yc[:], X[:], True, True, [Xt, Yct], pst)
                            T[hh]["psX"] = (ps, pst)
                            if r < NEU - 1:
                                ps, pst = psg()
                                MM(ps[:64, :64], X[:], Yc[:], True, True, [Xt, Yct], pst)
                                T[hh]["psYn"] = (ps, pst)
                        for hh in range(8):
                            Xn, Xnt = CHN[hh][xn]
                            ps, pst = T[hh]["psX"]
                            ACT(Xn[:], ps[:64, :64], AF.Copy, [pst], [Xnt])
                            if r < NEU - 1:
                                Yn, Ynt = CHN[hh][yn]
                                ps, pst = T[hh]["psYn"]
                                CP(Yn[:], ps[:64, :64], [pst], [Ynt])
                                T[hh]["Yc"] = CHN[hh][yn]
                            T[hh]["X"] = CHN[hh][xn]
                        for hh in range(8):
                            X, Xt = T[hh]["X"]
                            Pc, Pct = T[hh]["P"]
                            ps, pst = psg()
                            MM(ps[:64, :64], X[:], Pc[:], True, True, [Xt, Pct], pst)
                            T[hh]["psP"] = (ps, pst)
                        for hh in range(8):
                            Pc, Pct = T[hh]["P"]
                            Pn, Pnt = CHN[hh][pn]
                            ps, pst = T[hh]["psP"]
                            TTo(Pn[:], ps[:64, :64], Pc[:], ALU.add, [pst, Pct], [Pnt])
                            T[hh]["P"] = CHN[hh][pn]
                    for hh in range(8):
                        ps, pst = psg(128)
                        MM(ps[:64, :128], T[hh]["kT"], identB[:], True, True, [stt, identB_t], pst)
                        T[hh]["pskt"] = (ps, pst)
                        ps, pst = psg(128)
                        MM(ps[:64, :128], T[hh]["vT"], identB[:], True, True, [stt, identB_t], pst)
                        T[hh]["psvt"] = (ps, pst)
                    for hh in range(8):
                        dh = d * 8 + hh
                        ps, pst = T[hh]["pskt"]
                        Kbg, Kbgt = CHN[hh]["Kbg"]
                        Kd, Kdt = CHN[hh]["Kd"]
                        ACT(Kbg[:], ps[:64, :128], AF.Copy, [pst, CKB_t], [Kbgt], scale=CKB[:, c, dh:dh + 1])
                        TS(Kd[:], ps[:64, :128], CKD[:, c, dh:dh + 1], 0.0, ALU.mult, ALU.add, [pst, CKD_t], [Kdt])
                        ps, pst = T[hh]["psvt"]
                        Vb, Vbt = CHN[hh]["Vb"]
                        ACT(Vb[:], ps[:64, :128], AF.Copy, [pst, BT_t], [Vbt], scale=BT[:, c, dh:dh + 1])
                    for hh in range(8):
                        dh = d * 8 + hh
                        Kbg, Kbgt = CHN[hh]["Kbg"]
                        Pc, Pct = T[hh]["P"]
                        ps, pst = psg()
                        MM(ps[:, :64], Kbg[:], Pc[:], True, True, [Kbgt, Pct], pst)
                        T[hh]["psW"] = (ps, pst)
                        ps, pst = psg()
                        MM(ps[:, :64], Sel[:, dh, :], GAMr[:, tk], True, True, [Sel_t, GAMr_t], pst)
                        T[hh]["pse"] = (ps, pst)
                    for hh in range(8):
                        ps, pst = T[hh]["psW"]
                        nWT, nWTt = CHN[hh]["nWT"]
                        ACT(nWT[:], ps[:, :64], AF.Copy, [pst], [nWTt], scale=-1.0)
                        ps, pst = T[hh]["pse"]
                        EGe, EGet = CHN[hh]["EGe"]
                        qg, qgt = CHN[hh]["qg"]
                        ACT(EGe[:], ps[:, :64], AF.Exp, [pst], [EGet])
                        TTo(qg[:], T[hh]["qT"], EGe[:], ALU.mult, [stt, EGet], [qgt])
                    for hh in range(8):
                        Pc, Pct = T[hh]["P"]
                        Vb, Vbt = CHN[hh]["Vb"]
                        nWT, nWTt = CHN[hh]["nWT"]
                        Sb, Sbt = CHN[hh]["Sb"]
                        ps, pst = psg(128)
                        MM(ps[:64, :128], Pc[:], Vb[:], True, False, [Pct, Vbt], pst)
                        MM(ps[:64, :128], nWT[:], Sb[:], False, True, [nWTt, Sbt], pst)
                        T[hh]["psv"] = (ps, pst)
                    for hh in range(8):
                        ps, pst = T[hh]["psv"]
                        vn, vnt = CHN[hh]["vn"]
                        ACT(vn[:], ps[:64, :128], AF.Copy, [pst], [vnt])
                    for hh in range(8):
                        vn, vnt = CHN[hh]["vn"]
                        Sb, Sbt = CHN[hh]["Sb"]
                        qg, qgt = CHN[hh]["qg"]
                        QKmT, QKmTt = CHN[hh]["QKmT"]
                        Kd, Kdt = CHN[hh]["Kd"]
                        if is_lat:
                            ps, pst = psg()
                            MM(ps[:, :64], Sb[:], qg[:], True, False, [Sbt, qgt], pst)
                            MM(ps[:, :64], vn[:], QKmT[:], False, True, [vnt, QKmTt], pst)
                            T[hh]["pso"] = (ps, pst)
                        ps, pst = psg(128)
                        MM(ps[:, :128], Kd[:], vn[:], True, True, [Kdt, vnt], pst)
                        T[hh]["psS"] = (ps, pst)
                    for hh in range(8):
                        dh = d * 8 + hh
                        S, St = CHN[hh]["S"]
                        Sb, Sbt = CHN[hh]["Sb"]
                        OB, OBt = CHN[hh]["OB"]
                        if is_lat:
                            ps, pst = T[hh]["pso"]
                            CP(OB[:, o64:o64 + 64], ps[:, :64], [pst], [OBt])
                        ps, pst = T[hh]["psS"]
                        STT(S[:], S[:], CD[:, c, dh:dh + 1], ps[:, :128], ALU.mult, ALU.add, [St, CD_t, pst], [St])
                        ACT(Sb[:], S[:], AF.Copy, [St], [Sbt])
                        last_in_group = (c % 4 == 3) if d == 0 else (c % 4 == 0)
                        if is_lat and last_in_group:
                            P.dma("sp", OD[d, hh, :, g4 * 256:(g4 + 1) * 256], OB[:], reads=[OBt], writes=[ODt])
            P.barrier()
    if upto == "G":
        return finish(nc, P, outT)


    LT512 = [(256 + 512 * i, 512) for i in range(8)]
    with ExitStack() as ph:
        wo, wo_t = sb("wo1", [128, 8, 1024], BF16, ph)
        for k in range(8):
            P.dma("pool", wo[:, k, :], w1_out[:, k, :], writes=[wo_t])
        ono = VO["out_norm"]
        xT, xt = sb("xTh", [128, 8, 512], F32, ph)
        o0b = [sb("o0b%d" % i, [128, 512], F32, ph) for i in range(2)]
        o1b = [sb("o1b%d" % i, [128, 512], F32, ph) for i in range(2)]
        zbh = [sb("zbh%d" % i, [128, 512], F32, ph) for i in range(2)]
        sqh = [sb("sqh%d" % i, [128, 512], BF16, ph) for i in range(2)]
        rsh = [sb("rsh%d" % i, [128, 512], F32, ph) for i in range(2)]
        yg, yg_t = sb("yg", [128, 8, 512], BF16, ph)
        yb, yb_t = sb("ybh", [128, 8, 512], F32, ph)
        sq, sq_t = sb("sqhh", [128, 8, 512], BF16, ph)
        rstd, rstd_t = sb("rstdh", [128, 512], F32, ph)
        c_h, c_n, c_y = [0], [0], [0]
        XM2t = Tr()
        for (t0, n) in LT512:
            for k in range(8):
                P.dma("sp", xT[:, k, :], X1[k, :, t0:t0 + n], reads=[X1t], writes=[xt])
            for hh in range(8):
                o0, o0t = o0b[c_h[0] % 2]
                o1, o1t = o1b[c_h[0] % 2]
                zb, zbt = zbh[c_h[0] % 2]
                sqq, sqqt = sqh[c_h[0] % 2]
                rs, rst = rsh[c_h[0] % 2]
                c_h[0] += 1
                P.dma("sp", o0[:], OD[0, hh, :, t0:t0 + n], reads=[ODt], writes=[o0t])
                P.dma("sp", o1[:], OD[1, hh, :, t0:t0 + n], reads=[ODt], writes=[o1t])
                P.dma("sp", zb[:], Z1[hh, :, t0:t0 + n], reads=[Z1t], writes=[zbt])
                TTo(o0[:], o0[:], o1[:], ALU.add, [o0t, o1t], [o0t])
                ACT(sqq[:], o0[:], AF.Square, [o0t], [sqqt])
                pn, pnt = P.ps([4, 5], c_n)
                MM(pn[:, :n], ones_bf[:], sqq[:], True, True, [ones_t, sqqt], pnt)
                ACT(rs[:], pn[:, :n], AF.Sqrt, [pnt, eps_t], [rst], scale=1.0 / 128, bias=epsc[:, 0:1])
                P.op("dve", lambda e: e.reciprocal(rs[:], rs[:]), reads=[rst], writes=[rst])
                ACT(zb[:], zb[:], AF.Silu, [zbt], [zbt])
                TTo(o0[:], o0[:], rs[:], ALU.mult, [o0t, rst], [o0t])
                STT(yg[:, hh, :], o0[:], V[:, ono:ono + 1], zb[:], ALU.mult, ALU.mult, [o0t, Vt, zbt], [yg_t])
            for m in range(8):
                ps, pst = P.ps([1, 2, 3], c_y)
                for hh in range(8):
                    MM(ps[:, :n], wo[:, hh, m * 128:(m + 1) * 128], yg[:, hh, :], hh == 0, hh == 7, [wo_t, yg_t], pst)
                ACT(yb[:, m, :], ps[:, :n], AF.Copy, [pst], [yb_t])
            post_res(yb, yb_t, n, 1, 0, GG_M, xT, xt, sq, sq_t, rstd, rstd_t, 0, yb, yb_t)
            for k in range(8):
                P.dma("sp", XM[k, :, t0:t0 + n], yb[:, k, :], reads=[yb_t], writes=[XM2t])
        P.barrier()
    if upto == "H":
        return finish(nc, P, outT)

    with ExitStack() as ph:
        rt, rt_t = sb("rt", [128, 8, 8], F32, ph)
        P.dma("sp", rt[:], router, writes=[rt_t])
        Sel, Sel_t = sb("Sel2", [16, 16, 128], F32, ph)
        P.dma("sp", Sel[:], selc, writes=[Sel_t])
        xT, xt = sb("xTi", [128, 8, 512], F32, ph)
        tmp, tmp_t = sb("tmpi", [128, 8, 512], F32, ph)
        h2, h2_t = sb("h2i", [128, 8, 512], BF16, ph)
        sq, sq_t = sb("sqi", [128, 8, 512], BF16, ph)
        rstd, rstd_t = sb("rstdi", [128, 512], F32, ph)
        gg, gg_t = sb("ggi", [128, 28, 512], BF16, ph)
        yacc, yacc_t = sb("yacc", [128, 8, 512], F32, ph)
        cwb, cwb_t = sb("cwb", [128, 8, 512], F32, ph)
        w1bb = [sb("w1b%d" % i, [128, 8, 512], BF16, ph) for i in range(3)]
        w3bb = [sb("w3b%d" % i, [128, 8, 512], BF16, ph) for i in range(3)]
        w2bb = [sb("w2bi%d" % i, [128, 28, 128], BF16, ph) for i in range(3)]
        slb = [sb("sli%d" % i, [128, 512], F32, ph) for i in range(2)]
        tb_ = [sb("tbi%d" % i, [128, 512], F32, ph) for i in range(2)]
        lg, lg_t = sb("lg", [128, 4, 8], F32, ph)
        l2, l2_t = sb("l2", [128, 4, 8], F32, ph)
        mk1, mk1_t = sb("mk1", [128, 4, 8], F32, ph)
        mk2, mk2_t = sb("mk2", [128, 4, 8], F32, ph)
        comb, comb_t = sb("comb", [128, 4, 8], F32, ph)
        m1, m1_t = sb("m1", [128, 4], F32, ph)
        m2, m2_t = sb("m2", [128, 4], F32, ph)
        g1, g1_t = sb("g1", [128, 4], F32, ph)
        g2, g2_t = sb("g2", [128, 4], F32, ph)
        cT, cT_t = sb("cTm", [8, 512], F32, ph)
        c_a, c_b, c_w, c_w2, c_s = [0], [0], [0], [0], [0]
        OUTt = Tr()
        NEXP = int(os.environ.get('NEXP', 8))
        for (t0, n) in LT512[:int(os.environ.get('NTI', 8))]:
            for k in range(8):
                P.dma("sp", xT[:, k, :], XM[k, :, t0:t0 + n], reads=[XM2t], writes=[xt])
            rms_rstd(xT[:, :, :], xt, n, sq, sq_t, rstd, rstd_t, 0)
            for k in range(8):
                TTo(tmp[:, k, :], xT[:, k, :], rstd[:, :], ALU.mult, [xt, rstd_t], [tmp_t])
            for k in range(8):
                ACT(tmp[:, k, :], tmp[:, k, :], AF.Identity, [tmp_t, DVt], [tmp_t], scale=dv(1, 0, GM_F, k), bias=dv(1, 0, SH_F, k))
            for k in range(8):
                CP(h2[:, k, :], tmp[:, k, :], [tmp_t], [h2_t])
            pr, prt = P.psl[6]
            for b in range(4):
                for k in range(8):
                    MM(pr[:, b * 8:(b + 1) * 8], tmp[:, k, b * 128:(b + 1) * 128], rt[:, k, :], k == 0, k == 7, [tmp_t, rt_t], prt)
            ACT(lg[:].rearrange("p b e -> p (b e)"), pr[:, :32], AF.Copy, [prt], [lg_t])
            P.op("dve", lambda e: e.tensor_reduce(out=m1[:], in_=lg[:], axis=AX.X, op=ALU.max), reads=[lg_t], writes=[m1_t])
            for b in range(4):
                TS(mk1[:, b, :], lg[:, b, :], m1[:, b:b + 1], 0.0, ALU.is_equal, ALU.add, [lg_t, m1_t], [mk1_t])
            STT(l2[:], mk1[:], -1e30, lg[:], ALU.mult, ALU.add, [mk1_t, lg_t], [l2_t])
            P.op("dve", lambda e: e.tensor_reduce(out=m2[:], in_=l2[:], axis=AX.X, op=ALU.max), reads=[l2_t], writes=[m2_t])
            for b in range(4):
                TS(mk2[:, b, :], l2[:, b, :], m2[:, b:b + 1], 0.0, ALU.is_equal, ALU.add, [l2_t, m2_t], [mk2_t])
            TTo(g1[:], m1[:], m2[:], ALU.subtract, [m1_t, m2_t], [g1_t])
            ACT(g1[:], g1[:], AF.Sigmoid, [g1_t], [g1_t])
            TS(g2[:], g1[:], -1.0, 1.0, ALU.mult, ALU.add, [g1_t], [g2_t])
            for b in range(4):
                TS(comb[:, b, :], mk1[:, b, :], g1[:, b:b + 1], 0.0, ALU.mult, ALU.add, [mk1_t, g1_t], [comb_t])
                STT(comb[:, b, :], mk2[:, b, :], g2[:, b:b + 1], comb[:, b, :], ALU.mult, ALU.add, [mk2_t, g2_t, comb_t], [comb_t])
            pt, ptt = P.psl[7]
            for b in range(4):
                MM(pt[:8, b * 128:(b + 1) * 128], comb[:, b, :], identF[:], True, True, [comb_t, identF_t], ptt)
            ACT(cT[:], pt[:8, :512], AF.Copy, [ptt], [cT_t])
            for e in range(8):
                ps, pst = P.ps([1, 2, 3, 4], c_a)
                MM(ps[:, :512], Sel[:8, e, :], cT[:], True, True, [Sel_t, cT_t], pst)
                ACT(cwb[:, e, :], ps[:, :512], AF.Copy, [pst], [cwb_t])
            for e in range(NEXP):
                w1v = moe_w1[e].rearrange("(k p) n -> p k n", p=128)
                w3v = moe_w3[e].rearrange("(k p) n -> p k n", p=128)
                w2v = moe_w2[e].rearrange("(c p) n -> p c n", p=128)
                for cb in range(7):
                    w1b, w1bt = w1bb[c_w[0] % 3]
                    w3b, w3bt = w3bb[c_w[0] % 3]
                    c_w[0] += 1
                    for k in range(8):
                        P.dma("pool", w1b[:, k, :], w1v[:, k, cb * 512:(cb + 1) * 512], writes=[w1bt])
                        P.dma("pool", w3b[:, k, :], w3v[:, k, cb * 512:(cb + 1) * 512], writes=[w3bt])
                    for c4 in range(4):
                        c = cb * 4 + c4
                        ps1, ps1t = P.ps([1, 2, 3, 4], c_a)
                        for k in range(8):
                            MM(ps1[:, :n], w1b[:, k, c4 * 128:(c4 + 1) * 128], h2[:, k, :], k == 0, k == 7, [w1bt, h2_t], ps1t)
                        ps3, ps3t = P.ps([1, 2, 3, 4], c_a)
                        for k in range(8):
                            MM(ps3[:, :n], w3b[:, k, c4 * 128:(c4 + 1) * 128], h2[:, k, :], k == 0, k == 7, [w3bt, h2_t], ps3t)
                        sl, slt = slb[c_s[0] % 2]
                        tb, tbt = tb_[c_s[0] % 2]
                        c_s[0] += 1
                        ACT(sl[:], ps1[:, :n], AF.Silu, [ps1t], [slt])
                        TTo(tb[:], sl[:], ps3[:, :n], ALU.mult, [slt, ps3t], [tbt])
                        TTo(gg[:, c, :], tb[:], cwb[:, e, :], ALU.mult, [tbt, cwb_t], [gg_t])
                for m in range(8):
                    w2b, w2bt = w2bb[c_w2[0] % 3]
                    c_w2[0] += 1
                    for c7 in range(0, 28, 7):
                        P.dma("pool", w2b[:, c7:c7 + 7, :], w2v[:, c7:c7 + 7, m * 128:(m + 1) * 128], writes=[w2bt])
                    ps, pst = P.ps([5, 6, 7], c_b)
                    for c in range(28):
                        MM(ps[:, :n], w2b[:, c, :], gg[:, c, :], c == 0, c == 27, [w2bt, gg_t], pst)
                    if e == 0:
                        ACT(yacc[:, m, :], ps[:, :n], AF.Copy, [pst], [yacc_t])
                    else:
                        TTo(yacc[:, m, :], yacc[:, m, :], ps[:, :n], ALU.add, [pst, yacc_t], [yacc_t])
            post_res(yacc, yacc_t, n, 1, 0, GG_F, xT, xt, sq, sq_t, rstd, rstd_t, 0, yacc, yacc_t)
            for k in range(8):
                P.dma("sp", outT[k, :, t0 - CTX:t0 - CTX + n], yacc[:, k, :], reads=[yacc_t], writes=[OUTt])
        P.barrier()
    return finish(nc, P, outT)


def finish(nc, P, outT):
    P.barrier()
    return nc, P


def _wk(w):
    w = np.asarray(w, np.float32)
    K, N = w.shape
    return np.ascontiguousarray(w.reshape(K // 128, 128, N).transpose(1, 0, 2))


def host_inputs(inp):
    shared = {}
    shared["vecs"] = vec_layout(inp).pack()
    for l in (0, 1):
        shared["l%d_mod_w" % l] = _wk(inp["l%d_mod_w" % l])
    shared["w0_in"] = _wk(inp["l0_w_in"])
    shared["pool_w"] = np.ascontiguousarray(np.asarray(inp["l0_pool_w"], np.float32).transpose(1, 0, 2))
    wo = np.asarray(inp["l0_w_out"], np.float32)
    shared["w0_outp"] = _wk(wo[:512])
    shared["w0_outa"] = np.ascontiguousarray(wo[512:].reshape(8, 64, 1024).transpose(1, 0, 2))
    shared["ffn_w1"] = _wk(inp["l0_ffn_w1"])
    shared["ffn_w3"] = _wk(inp["l0_ffn_w3"])
    shared["ffn_w2"] = _wk(inp["l0_ffn_w2"])
    cos, sin = _rope_tables()
    shared["rope_cos"], shared["rope_sin"] = np.ascontiguousarray(cos[:64]), np.ascontiguousarray(sin[:64])
    shared["rotT"] = np.ascontiguousarray(_rot_lhsT()[:64, :64])
    shared["identf"] = np.eye(128, dtype=np.float32)
    kk = np.arange(128)[:, None]
    qq = np.arange(128)[None, :]
    m = np.zeros((128, 2, 512), np.float32)
    m[:, 0, :] = np.tile((qq <= kk).astype(np.float32), (1, 4))
    m[:, 1, :] = np.tile((kk <= qq).astype(np.float32), (1, 4))
    shared["masks"] = m
    shared["invc_lat"] = np.ascontiguousarray(np.broadcast_to(_pool_invcnt(LAT)[None], (128, 4, LAT)))
    shared["invc_ctx"] = np.ascontiguousarray(np.broadcast_to(_pool_invcnt(CTX)[None], (128, 4, CTX)))
    shared["w1_in"] = _wk(inp["l1_w_in"])
    shared["w1_out"] = _wk(inp["l1_w_out"])
    sel = np.zeros((16, 16, 128), np.float32)
    for k0 in range(16):
        sel[k0, k0, :] = 1.0
    shared["selc"] = sel
    ii = np.arange(64)[:, None]
    jj = np.arange(64)[None, :]
    gmk = np.zeros((64, 4, 64), np.float32)
    gmk[:, 0, :] = ii > jj
    gmk[:, 1, :] = ii >= jj
    gmk[:, 2, :] = ii < jj
    gmk[:, 3, :] = ii <= jj
    shared["gmask"] = gmk
    trm = np.zeros((64, 2, 64), np.float32)
    trm[:, 0, :] = ii <= jj
    trm[:, 1, :] = ii >= jj
    shared["tri"] = trm
    shared["router"] = _wk(inp["l1_router"])
    for e in range(8):
        shared["moe_w1_%d" % e] = np.ascontiguousarray(np.asarray(inp["l1_moe_w1"][e], np.float32))
        shared["moe_w3_%d" % e] = np.ascontiguousarray(np.asarray(inp["l1_moe_w3"][e], np.float32))
        shared["moe_w2_%d" % e] = np.ascontiguousarray(np.asarray(inp["l1_moe_w2"][e], np.float32))
    maps = []
    x = np.asarray(inp["x"], np.float32)
    ctx = np.asarray(inp["ctx"], np.float32)
    c = np.asarray(inp["c"], np.float32)
    cc = np.asarray(inp["c_ctx"], np.float32)
    for b in range(NCORES):
        d = dict(shared)
        xt = np.concatenate([ctx[b], x[b]], axis=0).T
        d["xin"] = np.ascontiguousarray(xt.reshape(8, 128, TT))
        d["cT"] = np.ascontiguousarray(np.stack([_colvec(c[b]), _colvec(cc)], axis=-1))
        maps.append(d)
    return maps


_CACHE = {}


def kernel(**inputs):
    if "nc" not in _CACHE:
        _CACHE["nc"] = build()[0]
    nc = _CACHE["nc"]
    maps = host_inputs(inputs)
    res = run_bass_kernel_spmd(nc, maps, core_ids=list(range(NCORES)))
    out = np.stack([np.ascontiguousarray(r["outT"].reshape(1024, LAT).T) for r in res.results], axis=0)
    return out.astype(np.float32)
```

```python
import os
import numpy as np
from contextlib import ExitStack
import concourse.bass as bass
import concourse.mybir as mybir
from concourse.bass_utils import run_bass_kernel_spmd

F32 = mybir.dt.float32
BF16 = mybir.dt.bfloat16
AF = mybir.ActivationFunctionType
ALU = mybir.AluOpType
AX = mybir.AxisListType

D = 1024
LAT = 4096
CTX = 256
TT = LAT + CTX
EPS = 1e-6
NCORES = 8


class Tr:
    __slots__ = ("w", "r", "x")

    def __init__(self, x=False):
        self.w = None
        self.r = {}
        self.x = x


class Prog:
    NDMA = 24

    def __init__(self, nc):
        self.nc = nc
        self.es = ExitStack()
        self.eng = {"pe": nc.tensor, "act": nc.scalar, "dve": nc.vector, "pool": nc.gpsimd, "sp": nc.sync}
        self.semh = {}
        for k in self.eng:
            self.semh[k] = self.es.enter_context(nc.semaphore("s_" + k))
        self.cnt = {k: 0 for k in self.eng}
        self.epoch = {k: 0 for k in self.eng}
        self.ekey = {k: k for k in self.eng}
        self.hist = []
        self.waited = {k: {} for k in self.eng}
        self.dcnt = [0] * self.NDMA
        self.dnext = 0
        for i in range(self.NDMA):
            self.semh["d%d" % i] = self.es.enter_context(nc.semaphore("sd%d" % i))
        self.nps = 0
        self.psl = []
        for i in range(8):
            t = self.es.enter_context(nc.psum_tensor("ps%d" % i, [128, 512], F32))
            self.psl.append((t, Tr(True)))
        self.n_ins = 0

    def ps(self, banks, ctr):
        t = self.psl[banks[ctr[0] % len(banks)]]
        ctr[0] += 1
        return t

    def _wait(self, e, deps):
        best = {}
        for d in deps:
            if d is None:
                continue
            k, v = d
            if v > best.get(k, 0):
                best[k] = v
        for k, v in best.items():
            if e == "pe" and (k == "pe" or k.startswith("pe#")):
                continue
            if self.waited[e].get(k, 0) >= v:
                continue
            self.eng[e].wait_ge(self.semh[k], v)
            self.waited[e][k] = v

    @staticmethod
    def _deps(reads, writes):
        deps = []
        for t in reads:
            if t.w is not None:
                deps.append(t.w)
            if t.x:
                deps.extend(t.r.items())
        for t in writes:
            if t.w is not None:
                deps.append(t.w)
            deps.extend(t.r.items())
        return deps

    @staticmethod
    def _mark(me, reads, writes):
        k, v = me
        for t in reads:
            if t.r.get(k, 0) < v:
                t.r[k] = v
        for t in writes:
            t.w = me
            t.r = {}

    EPOCH = 12000

    def op(self, e, fn, reads=(), writes=()):
        if self.cnt[e] >= self.EPOCH:
            self.epoch[e] += 1
            self.ekey[e] = "%s#%d" % (e, self.epoch[e])
            self.semh[self.ekey[e]] = self.es.enter_context(self.nc.semaphore("s_%s_%d" % (e, self.epoch[e])))
            self.cnt[e] = 0
        self._wait(e, self._deps(reads, writes))
        ins = fn(self.eng[e])
        self.cnt[e] += 1
        key = self.ekey[e]
        ins.then_inc(self.semh[key], 1)
        self._mark((key, self.cnt[e]), reads, writes)
        self.n_ins += 1
        return ins

    def dma(self, q, out, in_, reads=(), writes=()):
        slot = self.dnext
        self.dnext = (slot + 1) % self.NDMA
        deps = self._deps(reads, writes)
        key = "d%d" % slot
        if self.dcnt[slot] > 0:
            deps.append((key, 16 * self.dcnt[slot]))
        self._wait(q, deps)
        ins = self.eng[q].dma_start(out=out, in_=in_)
        self.dcnt[slot] += 1
        ins.then_inc(self.semh[key], 16)
        self._mark((key, 16 * self.dcnt[slot]), reads, writes)
        self.n_ins += 1
        return ins

    def barrier(self):
        deps = [(self.ekey[k], self.cnt[k]) for k in self.eng if self.cnt[k] > 0]
        deps += [("d%d" % i, 16 * self.dcnt[i]) for i in range(self.NDMA) if self.dcnt[i] > 0]
        for e in self.eng:
            best = {}
            for k, v in deps:
                best[k] = v
            for k, v in best.items():
                if self.waited[e].get(k, 0) >= v:
                    continue
                self.eng[e].wait_ge(self.semh[k], v)
                self.waited[e][k] = v
        for (t, tr) in self.psl:
            tr.w = None
            tr.r = {}


def _rope_tables():
    half = 32
    inv = 10000.0 ** (-np.arange(0, half, 2, dtype=np.float32) / half)
    t = np.arange(LAT)
    rows = (t // 64).astype(np.float32)
    cols = (t % 64).astype(np.float32)
    cos = np.zeros((64, LAT), np.float32)
    sin = np.zeros((64, LAT), np.float32)
    for i in range(64):
        pos = rows if i < 32 else cols
        ang = pos * inv[i % 16]
        cos[i] = np.cos(ang)
        sin[i] = np.sin(ang)
    cos = np.concatenate([cos, cos], 0)
    sin = np.concatenate([sin, sin], 0)
    return cos, sin


def _rot_lhsT():
    R = np.zeros((128, 128), np.float32)
    for hb in (0, 64):
        for blk in (0, 32):
            for i in range(16):
                R[hb + blk + i, hb + blk + i + 16] = -1.0
                R[hb + blk + i + 16, hb + blk + i] = 1.0
    return np.ascontiguousarray(R.T)


def _pool_invcnt(L):
    t = np.arange(L)
    out = np.zeros((4, L), np.float32)
    for gi, w in enumerate((2, 4, 8, 16)):
        lo = np.clip(t - w // 2, 0, L)
        hi = np.clip(t + w // 2, 0, L)
        out[gi] = 1.0 / (hi - lo).astype(np.float32)
    return out


def _colvec(v):
    return np.ascontiguousarray(np.asarray(v, np.float32).reshape(-1, 128).T)


class VecPack:
    def __init__(self):
        self.cols = []
        self.off = {}
        self.n = 0

    def add(self, name, arr):
        arr = np.asarray(arr, np.float32)
        assert arr.shape[0] == 128
        self.off[name] = self.n
        self.cols.append(arr)
        self.n += arr.shape[1]

    def pack(self):
        return np.ascontiguousarray(np.concatenate(self.cols, axis=1))


_VEC_INPUTS = ("l0_mix_pre", "l0_mix_post", "l0_ffn_pre", "l0_ffn_post", "l0_mod_b", "l0_mod_w",
               "l1_mix_pre", "l1_mix_post", "l1_ffn_pre", "l1_ffn_post", "l1_mod_b", "l1_mod_w")


def vec_layout(inp=None):
    z = lambda *s: np.zeros(s, np.float32)
    g = (lambda k, shp: np.asarray(inp[k], np.float32)) if inp is not None else (lambda k, shp: z(*shp))
    vp = VecPack()
    for l in (0, 1):
        for nm in ("mix_pre", "mix_post", "ffn_pre", "ffn_post"):
            vp.add("l%d_%s" % (l, nm), _colvec(g("l%d_%s" % (l, nm), (1024,))))
        vp.add("l%d_mod_b" % l, _colvec(g("l%d_mod_b" % l, (6144,))))
    vp.add("pool_scale", _colvec(g("l0_pool_scale", (512,))))
    sk = np.asarray(g("l0_sinks", (8,)), np.float32).reshape(1, 8)
    vp.add("sinks", np.broadcast_to(sk, (128, 8)))
    cw = g("l1_conv_w", (5, 3072))
    vp.add("conv_w", np.concatenate([_colvec(cw[j]) for j in range(5)], axis=1))
    vp.add("out_norm", np.asarray(g("l1_out_norm", (128,)), np.float32).reshape(128, 1))
    al = g("l1_a_log", (2, 8)).reshape(1, 16)
    db = g("l1_dt_bias", (2, 8)).reshape(1, 16)
    vp.add("a_log", np.broadcast_to(al, (128, 16)))
    vp.add("dt_bias", np.broadcast_to(db, (128, 16)))
    return vp


TILES512 = [(0, 256)] + [(256 + 512 * i, 512) for i in range(8)]
TILES256 = [(256 * i, 256) for i in range(17)]
NBLK = TT // 128


def build(upto="all", dbg=()):
    nc = bass.Bass("TRN2", target_bir_lowering=False)
    P = Prog(nc)
    es = P.es
    VO = vec_layout(None).off
    NV = vec_layout(None).n

    def din(name, shape, dt=F32):
        return nc.dram_tensor(name, list(shape), dt, kind="ExternalInput").ap()

    def dscr(name, shape, dt=F32):
        kind = "ExternalOutput" if name in dbg else "Internal"
        return nc.dram_tensor(name, list(shape), dt, kind=kind).ap()

    xin = din("xin", [8, 128, TT])
    cT = din("cT", [128, 8, 2])
    vecs = din("vecs", [128, NV])
    modw = [din("l%d_mod_w" % l, [128, 8, 6144]) for l in (0, 1)]
    w0_in = din("w0_in", [128, 8, 1280])
    pool_w = din("pool_w", [128, 4, 128])
    w0_outp = din("w0_outp", [128, 4, 1024])
    w0_outa = din("w0_outa", [64, 8, 1024])
    ffn_w1 = din("ffn_w1", [128, 8, 2816])
    ffn_w3 = din("ffn_w3", [128, 8, 2816])
    ffn_w2 = din("ffn_w2", [128, 22, 1024])
    rope_cos = din("rope_cos", [64, LAT])
    rope_sin = din("rope_sin", [64, LAT])
    rotT = din("rotT", [64, 64])
    masks = din("masks", [128, 2, 512])
    invc_lat = din("invc_lat", [128, 4, LAT])
    invc_ctx = din("invc_ctx", [128, 4, CTX])
    identf = din("identf", [128, 128])
    outT = nc.dram_tensor("outT", [8, 128, LAT], F32, kind="ExternalOutput").ap()

    UT = dscr("UT", [4, 128, TT])
    XM = dscr("XM", [8, 128, TT])
    X1 = dscr("X1", [8, 128, TT])

    def sb(name, shape, dt=F32, stack=None):
        t = (stack or es).enter_context(nc.sbuf_tensor(name, list(shape), dt))
        return t, Tr()

    def ACT(out, in_, func, reads, writes, **kw):
        return P.op("act", lambda e: e.activation(out=out, in_=in_, func=func, **kw), reads, writes)

    def TTo(out, a, b, op, reads, writes, eng="dve"):
        return P.op(eng, lambda e: e.tensor_tensor(out=out, in0=a, in1=b, op=op), reads, writes)

    def STT(out, in0, scalar, in1, op0, op1, reads, writes):
        return P.op("dve", lambda e: e.scalar_tensor_tensor(out=out, in0=in0, scalar=scalar, in1=in1, op0=op0, op1=op1),
                    reads, writes)

    def TS(out, in0, s1, s2, op0, op1, reads, writes):
        return P.op("dve", lambda e: e.tensor_scalar(out=out, in0=in0, scalar1=s1, scalar2=s2, op0=op0, op1=op1),
                    reads, writes)

    def CP(out, in_, reads, writes, eng="dve"):
        return P.op(eng, lambda e: e.tensor_copy(out=out, in_=in_), reads, writes)

    def MM(ps, lhsT, rhs, start, stop, reads, pst):
        return P.op("pe", lambda e: e.matmul(ps, lhsT, rhs, start=start, stop=stop), reads, [pst])

    V, Vt = sb("V", [128, NV])
    DV, DVt = sb("DV", [128, 2 * 2 * 48])
    ones_bf, ones_t = sb("ones_bf", [128, 128], BF16)
    ones_f, onesf_t = sb("ones_f", [128, 128], F32)
    epsc, eps_t = sb("epsc", [128, 1])
    identF, identF_t = sb("identF", [128, 128], F32)
    identB, identB_t = sb("identB", [128, 128], BF16)
    P.op("dve", lambda e: e.memset(ones_bf[:], 1.0), writes=[ones_t])
    P.op("dve", lambda e: e.memset(ones_f[:], 1.0), writes=[onesf_t])
    P.op("dve", lambda e: e.memset(epsc[:], EPS), writes=[eps_t])
    P.dma("sp", V[:], vecs, writes=[Vt])
    P.dma("sp", identF[:], identf, writes=[identF_t])
    CP(identB[:], identF[:], [identF_t], [identB_t])

    def dv(l, s, which, k=None):
        o = ((l * 2 + s) * 6 + which) * 8
        return DV[:, o:o + 8] if k is None else DV[:, o + k:o + k + 1]

    GM_M, SH_M, GG_M, GM_F, SH_F, GG_F = range(6)

    with ExitStack() as ph:
        scT, sc_t = sb("scT", [128, 8, 2], F32, ph)
        modT, mod_t = sb("modT", [128, 48, 2], F32, ph)
        wbuf = [sb("modw%d" % i, [128, 8, 1024], F32, ph) for i in range(2)]
        P.dma("sp", scT[:], cT, writes=[sc_t])
        ACT(scT[:], scT[:], AF.Silu, [sc_t], [sc_t])
        for l in (0, 1):
            ps, pst = P.psl[l]
            for j in range(6):
                wb, wbt = wbuf[j % 2]
                P.dma("sp", wb[:], modw[l][:, :, j * 1024:(j + 1) * 1024], writes=[wbt])
                for kc in range(8):
                    col = (j * 8 + kc) * 2
                    for k in range(8):
                        MM(ps[:, col:col + 2], wb[:, k, kc * 128:(kc + 1) * 128], scT[:, k, :], k == 0, k == 7, [wbt, sc_t], pst)
            mb = VO["l%d_mod_b" % l]
            for s in (0, 1):
                TTo(modT[:, :, s], ps[:, 0:96].rearrange("p (j t) -> p j t", t=2)[:, :, s], V[:, mb:mb + 48], ALU.add,
                    [pst, Vt], [mod_t])
                pre = "l%d_" % l
                for (which_gm, which_sh, which_gg, base, npre, npost) in (
                        (GM_M, SH_M, GG_M, 0, "mix_pre", "mix_post"), (GM_F, SH_F, GG_F, 24, "ffn_pre", "ffn_post")):
                    o_pre = VO[pre + npre]
                    o_post = VO[pre + npost]
                    STT(dv(l, s, which_gm), modT[:, base + 8:base + 16, s], 1.0, V[:, o_pre:o_pre + 8], ALU.add, ALU.mult,
                        [mod_t, Vt], [DVt])
                    CP(dv(l, s, which_sh), modT[:, base:base + 8, s], [mod_t], [DVt])
                    TTo(dv(l, s, which_gg), modT[:, base + 16:base + 24, s], V[:, o_post:o_post + 8], ALU.mult,
                        [mod_t, Vt], [DVt])
        P.barrier()

    def rms_rstd(src, src_t, n, sq, sq_t, rstd, rstd_t, bank, nch=8, scale=1.0 / 1024, npart=128):
        ACT(sq[:, :nch, :n], src, AF.Square, [src_t], [sq_t])
        ps, pst = P.psl[bank]
        for k in range(nch):
            MM(ps[:npart, :n], ones_bf[:, :npart], sq[:, k, :n], k == 0, k == nch - 1, [sq_t, ones_t], pst)
        ACT(rstd[:npart, :n], ps[:npart, :n], AF.Sqrt, [pst, eps_t], [rstd_t], scale=scale, bias=epsc[:npart, 0:1])
        P.op("dve", lambda e: e.reciprocal(rstd[:npart, :n], rstd[:npart, :n]), reads=[rstd_t], writes=[rstd_t])

    def norm_mod(xT, x_t, n, l, s, which_gm, which_sh, h, h_t, sq, sq_t, rstd, rstd_t, tmp, tmp_t, bank):
        rms_rstd(xT[:, :, :n], x_t, n, sq, sq_t, rstd, rstd_t, bank)
        for k in range(8):
            TTo(tmp[:, k, :n], xT[:, k, :n], rstd[:, :n], ALU.mult, [x_t, rstd_t], [tmp_t])
        for k in range(8):
            ACT(h[:, k, :n], tmp[:, k, :n], AF.Identity, [tmp_t, DVt], [h_t],
                scale=dv(l, s, which_gm, k), bias=dv(l, s, which_sh, k))

    def post_res(y, y_t, n, l, s, which_gg, xres, xres_t, sq, sq_t, rstd, rstd_t, bank, out, out_t):
        rms_rstd(y[:, :, :n], y_t, n, sq, sq_t, rstd, rstd_t, bank)
        for k in range(8):
            TTo(y[:, k, :n], y[:, k, :n], rstd[:, :n], ALU.mult, [y_t, rstd_t], [y_t])
        for k in range(8):
            STT(out[:, k, :n], y[:, k, :n], dv(l, s, which_gg, k), xres[:, k, :n], ALU.mult, ALU.add,
                [y_t, xres_t, DVt], [out_t])

    if upto == "p0":
        return finish(nc, P, outT)

    with ExitStack() as mix:
        QT, _ = sb("QT", [64, 8, TT], BF16, mix)
        KT, _ = sb("KT", [64, 2, TT], BF16, mix)
        VT, _ = sb("VT", [128, NBLK, 128], BF16, mix)
        QTt = [Tr() for _ in range(NBLK)]
        KTt = [Tr() for _ in range(NBLK)]
        VTt = [Tr() for _ in range(NBLK)]
        POt = [Tr() for _ in range(NBLK)]
        UTt = Tr()
        xin_t = Tr()

        with ExitStack() as ph:
            w_in, w_in_t = sb("w_in", [128, 8, 1280], BF16, ph)
            rotf, rotf_t = sb("rotf", [64, 64], F32, ph)
            rot, rot_t = sb("rot", [64, 64], BF16, ph)
            for k in range(8):
                P.dma("pool", w_in[:, k, :], w0_in[:, k, :], writes=[w_in_t])
            P.dma("sp", rotf[:], rotT, writes=[rotf_t])
            CP(rot[:], rotf[:], [rotf_t], [rot_t])
            xTb = [sb("xT%d" % i, [128, 8, 512], F32, ph) for i in range(2)]
            cosb = [sb("cos%d" % i, [64, 512], F32, ph) for i in range(2)]
            sinb = [sb("sin%d" % i, [64, 512], F32, ph) for i in range(2)]
            sq, sq_t = sb("sq", [128, 8, 512], BF16, ph)
            rstd, rstd_t = sb("rstd", [128, 512], F32, ph)
            tmp, tmp_t = sb("tmp", [128, 8, 512], F32, ph)
            h, h_t = sb("h", [128, 8, 512], BF16, ph)
            ub = [sb("ub%d" % i, [128, 512], F32, ph) for i in range(2)]
            qb = [sb("qb%d" % i, [64, 512], BF16, ph) for i in range(2)]
            t1 = [sb("t1_%d" % i, [64, 512], F32, ph) for i in range(2)]
            t2 = [sb("t2_%d" % i, [64, 512], F32, ph) for i in range(2)]
            c_proj, c_rot, c_v, c_misc = [0], [0], [0], [0]

            def loadA(i):
                t0, n = TILES512[i]
                xT, xt = xTb[i % 2]
                for k in range(8):
                    P.dma("sp", xT[:, k, :n], xin[k, :, t0:t0 + n], reads=[xin_t], writes=[xt])
                if i > 0:
                    l0 = t0 - CTX
                    P.dma("sp", cosb[i % 2][0][:, :n], rope_cos[:, l0:l0 + n], writes=[cosb[i % 2][1]])
                    P.dma("sp", sinb[i % 2][0][:, :n], rope_sin[:, l0:l0 + n], writes=[sinb[i % 2][1]])

            loadA(0)
            NTA = int(os.environ.get('NTA', len(TILES512)))
            for i, (t0, n) in enumerate(TILES512[:NTA]):
                if i + 1 < NTA:
                    loadA(i + 1)
                s = 1 if i == 0 else 0
                blks = list(range(t0 // 128, (t0 + n) // 128))
                xT, xt = xTb[i % 2]
                norm_mod(xT, xt, n, 0, s, GM_M, SH_M, h, h_t, sq, sq_t, rstd, rstd_t, tmp, tmp_t, 0)
                for m in range(0 if os.environ.get('NOU') else 4):
                    ps, pst = P.ps([1, 2, 3], c_proj)
                    for k in range(8):
                        MM(ps[:, :n], w_in[:, k, m * 128:(m + 1) * 128], h[:, k, :n], k == 0, k == 7, [w_in_t, h_t], pst)
                    u, ut = ub[c_misc[0] % 2]
                    c_misc[0] += 1
                    ACT(u[:, :n], ps[:, :n], AF.Copy, [pst], [ut])
                    P.dma("sp", UT[m, :, t0:t0 + n], u[:, :n], reads=[ut], writes=[UTt])
                for hh in range(int(os.environ.get('NHH', 10))):
                    ps, pst = P.ps([1, 2, 3], c_proj)
                    c0 = 512 + hh * 64
                    for k in range(8):
                        MM(ps[:64, :n], w_in[:, k, c0:c0 + 64], h[:, k, :n], k == 0, k == 7, [w_in_t, h_t], pst)
                    dst = QT[:, hh, t0:t0 + n] if hh < 8 else KT[:, hh - 8, t0:t0 + n]
                    dst_t = [QTt[b] for b in blks] if hh < 8 else [KTt[b] for b in blks]
                    if i == 0 or os.environ.get('NOROPE'):
                        ACT(dst, ps[:64, :n], AF.Copy, [pst], dst_t)
                    else:
                        q, qt = qb[c_misc[0] % 2]
                        a1, a1t = t1[c_misc[0] % 2]
                        a2, a2t = t2[c_misc[0] % 2]
                        c_misc[0] += 1
                        cs, cst = cosb[i % 2]
                        sn, snt = sinb[i % 2]
                        ACT(q[:, :n], ps[:64, :n], AF.Copy, [pst], [qt])
                        pr, prt = P.ps([4, 5], c_rot)
                        MM(pr[:64, :n], rot[:], q[:, :n], True, True, [rot_t, qt], prt)
                        if os.environ.get('ROPEV') == '1':
                            TTo(a1[:, :n], q[:, :n], cs[:, :n], ALU.mult, [qt, cst], [a1t])
                        else:
                            TTo(a1[:, :n], ps[:64, :n], cs[:, :n], ALU.mult, [pst, cst], [a1t])
                        TTo(a2[:, :n], pr[:64, :n], sn[:, :n], ALU.mult, [prt, snt], [a2t])
                        TTo(dst, a1[:, :n], a2[:, :n], ALU.add, [a1t, a2t], dst_t)
                for b in range(0 if os.environ.get('NOV') else n // 128):
                    ps, pst = P.ps([6, 7], c_v)
                    for k in range(8):
                        MM(ps[:, :128], h[:, k, b * 128:(b + 1) * 128], w_in[:, k, 1152:1280], k == 0, k == 7, [w_in_t, h_t], pst)
                    ACT(VT[:, blks[b], :], ps[:, :128], AF.Copy, [pst], [VTt[blks[b]]])
            P.barrier()
        if upto == "A":
            dq = dscr("dQT", [64, 8, TT], BF16)
            dk = dscr("dKT", [64, 2, TT], BF16)
            dvv = dscr("dVT", [128, NBLK, 128], BF16)
            P.dma("sp", dq, QT[:], reads=QTt)
            P.dma("sp", dk, KT[:], reads=KTt)
            P.dma("sp", dvv, VT[:], reads=VTt)
            return finish(nc, P, outT)

        with ExitStack() as ph:
            msk, msk_t = sb("msk", [128, 2, 512], BF16, ph)
            mskf, mskf_t = sb("mskf", [128, 2, 512], F32, ph)
            P.dma("sp", mskf[:], masks, writes=[mskf_t])
            CP(msk[:], mskf[:], [mskf_t], [msk_t])
            esk, esk_t = sb("esk", [64, 2, 512], F32, ph)
            so = VO["sinks"]
            for hh in range(8):
                ACT(esk[:, hh // 4, (hh % 4) * 128:(hh % 4 + 1) * 128], ones_f[:64, :], AF.Exp, [onesf_t, Vt], [esk_t],
                    scale=V[:64, so + hh:so + hh + 1])
            pts = [sb("pt%d" % i, [128, 512], BF16, ph) for i in range(6)]
            dens = [sb("den%d" % i, [64, 512], F32, ph) for i in range(2)]
            c_s, c_pt, c_nd = [0], [0], [0]
            for tb in range(NBLK):
                if tb < 2:
                    kbs = [(0, None), (1, None)]
                else:
                    kbs = [(0, None), (1, None)]
                    if tb > 2:
                        kbs.append((tb - 1, 0))
                    kbs.append((tb, None))
                    if tb < NBLK - 1:
                        kbs.append((tb + 1, 1))
                for j in range(2):
                    ptl = []
                    for (kb, mi) in kbs:
                        ps, pst = P.ps([0, 1, 2, 3, 4, 5], c_s)
                        MM(ps[:, :].rearrange("p (h q) -> p h q", h=4), KT[:, j, kb * 128:(kb + 1) * 128],
                           QT[:, 4 * j:4 * j + 4, tb * 128:(tb + 1) * 128], True, True, [KTt[kb], QTt[tb]], pst)
                        pt, ptt = pts[c_pt[0] % 6]
                        c_pt[0] += 1
                        ACT(pt[:], ps[:], AF.Exp, [pst], [ptt], scale=0.125)
                        if mi is not None:
                            TTo(pt[:], pt[:], msk[:, mi, :], ALU.mult, [ptt, msk_t], [ptt])
                        ptl.append((pt, ptt, kb))
                    psn, psnt = P.psl[6]
                    psd, psdt = P.psl[7]
                    for ii, (pt, ptt, kb) in enumerate(ptl):
                        MM(psn[:64, :], VT[:, kb, j * 64:(j + 1) * 64], pt[:], ii == 0, ii == len(ptl) - 1, [VTt[kb], ptt], psnt)
                    for ii, (pt, ptt, kb) in enumerate(ptl):
                        MM(psd[:64, :], ones_bf[:, :64], pt[:], ii == 0, ii == len(ptl) - 1, [ones_t, ptt], psdt)
                    dn, dnt = dens[c_nd[0] % 2]
                    c_nd[0] += 1
                    TTo(dn[:], psd[:64, :], esk[:, j, :], ALU.add, [psdt, esk_t], [dnt])
                    P.op("dve", lambda e: e.reciprocal(dn[:], dn[:]), reads=[dnt], writes=[dnt])
                    TTo(QT[:, 4 * j:4 * j + 4, tb * 128:(tb + 1) * 128], psn[:64, :].rearrange("p (h q) -> p h q", h=4),
                        dn[:].rearrange("p (h q) -> p h q", h=4), ALU.mult, [psnt, dnt], [QTt[tb]])
            P.barrier()
        if upto == "B":
            dq = dscr("dAO", [64, 8, TT], BF16)
            P.dma("sp", dq, QT[:], reads=QTt)
            return finish(nc, P, outT)

        PO, _ = sb("PO", [128, 4, TT], BF16, mix)
        with ExitStack() as ph:
            pw, pw_t = sb("pw", [128, 4, 128], BF16, ph)
            P.dma("pool", pw[:], pool_w, writes=[pw_t])
            LP = LAT + 16
            U0, U0t = sb("U0", [128, LP], F32, ph)
            Ba, Bat = sb("Ba", [128, LP], F32, ph)
            Bb, Bbt = sb("Bb", [128, LP], F32, ph)
            ivc, ivct = sb("ivc", [128, LAT], F32, ph)
            dl, dlt = sb("dl", [128, LAT], BF16, ph)
            c_p = [0]
            pso = VO["pool_scale"]
            for g, w in enumerate((2, 4, 8, 16)):
                for (s0, L, ivsrc) in ((0, CTX, invc_ctx), (CTX, LAT, invc_lat)):
                    Lt = L + 16
                    P.op("dve", lambda e: e.memset(U0[:, 0:8], 0.0), writes=[U0t])
                    P.op("dve", lambda e: e.memset(U0[:, 8 + L:16 + L], 0.0), writes=[U0t])
                    P.dma("sp", U0[:, 8:8 + L], UT[g, :, s0:s0 + L], reads=[UTt], writes=[U0t])
                    P.dma("sp", ivc[:, :L], ivsrc[:, g, :], writes=[ivct])
                    src, srct = U0, U0t
                    sh = 1
                    ln = Lt
                    bufs = [(Ba, Bat), (Bb, Bbt)]
                    bi = 0
                    while sh < w:
                        dst, dstt = bufs[bi % 2]
                        bi += 1
                        ln = ln - sh
                        TTo(dst[:, :ln], src[:, :ln], src[:, sh:sh + ln], ALU.add, [srct], [dstt])
                        src, srct = dst, dstt
                        sh *= 2
                    o = 8 - w // 2
                    dst, dstt = bufs[bi % 2]
                    TTo(dst[:, :L], src[:, o:o + L], ivc[:, :L], ALU.mult, [srct, ivct], [dstt])
                    TTo(dl[:, :L], dst[:, :L], U0[:, 8:8 + L], ALU.subtract, [dstt, U0t], [dlt])
                    for c0 in range(0, L, 512):
                        n = min(512, L - c0)
                        ps, pst = P.ps([0, 1, 2, 3], c_p)
                        MM(ps[:, :n], pw[:, g, :], dl[:, c0:c0 + n], True, True, [pw_t, dlt], pst)
                        blks = list(range((s0 + c0) // 128, (s0 + c0 + n) // 128))
                        ACT(PO[:, g, s0 + c0:s0 + c0 + n], ps[:, :n], AF.Copy, [pst, Vt], [POt[b] for b in blks],
                            scale=V[:, pso + g:pso + g + 1])
            P.barrier()
        if upto == "C":
            dq = dscr("dPO", [128, 4, TT], BF16)
            P.dma("sp", dq, PO[:], reads=POt)
            return finish(nc, P, outT)

        with ExitStack() as ph:
            wop, wop_t = sb("wop", [128, 4, 1024], BF16, ph)
            woa, woa_t = sb("woa", [64, 8, 1024], BF16, ph)
            for k in range(4):
                P.dma("pool", wop[:, k, :], w0_outp[:, k, :], writes=[wop_t])
            for k in range(8):
                P.dma("pool", woa[:, k, :], w0_outa[:, k, :], writes=[woa_t])
            xTb = [sb("xTd%d" % i, [128, 8, 512], F32, ph) for i in range(1)]
            yb, yb_t = sb("yb", [128, 8, 512], F32, ph)
            sq, sq_t = sb("sqd", [128, 8, 512], BF16, ph)
            rstd, rstd_t = sb("rstdd", [128, 512], F32, ph)
            XMt = Tr()
            c_y = [0]

            def loadD(i):
                t0, n = TILES512[i]
                xT, xt = xTb[0]
                for k in range(8):
                    P.dma("sp", xT[:, k, :n], xin[k, :, t0:t0 + n], reads=[xin_t], writes=[xt])

            for i, (t0, n) in enumerate(TILES512):
                loadD(i)
                s = 1 if i == 0 else 0
                blks = list(range(t0 // 128, (t0 + n) // 128))
                xT, xt = xTb[0]
                rd = [POt[b] for b in blks] + [QTt[b] for b in blks]
                for m in range(8):
                    ps, pst = P.ps([1, 2, 3, 4], c_y)
                    for g in range(4):
                        MM(ps[:, :n], wop[:, g, m * 128:(m + 1) * 128], PO[:, g, t0:t0 + n], g == 0, False, [wop_t] + rd, pst)
                    for hh in range(8):
                        MM(ps[:, :n], woa[:, hh, m * 128:(m + 1) * 128], QT[:, hh, t0:t0 + n], False, hh == 7, [woa_t] + rd, pst)
                    ACT(yb[:, m, :n], ps[:, :n], AF.Copy, [pst], [yb_t])
                xout, xout_t = yb, yb_t
                post_res(yb, yb_t, n, 0, s, GG_M, xT, xt, sq, sq_t, rstd, rstd_t, 0, xout, xout_t)
                for k in range(8):
                    P.dma("sp", XM[k, :, t0:t0 + n], xout[:, k, :n], reads=[xout_t], writes=[XMt])
            P.barrier()
    if upto == "D":
        return finish(nc, P, outT)

    XMt, X1t = Tr(), Tr()
    with ExitStack() as ph:
        w1, w1_t = sb("w1", [128, 8, 2816], BF16, ph)
        w3, w3_t = sb("w3", [128, 8, 2816], BF16, ph)
        for k in range(8):
            P.dma("pool", w1[:, k, :], ffn_w1[:, k, :], writes=[w1_t])
            P.dma("pool", w3[:, k, :], ffn_w3[:, k, :], writes=[w3_t])
        w2b = [sb("w2b%d" % i, [128, 22, 128], BF16, ph) for i in range(2)]
        xTb = [sb("xTe%d" % i, [128, 8, 256], F32, ph) for i in range(2)]
        sq, sq_t = sb("sqe", [128, 8, 256], BF16, ph)
        rstd, rstd_t = sb("rstde", [128, 256], F32, ph)
        tmp, tmp_t = sb("tmpe", [128, 8, 256], F32, ph)
        h2, h2_t = sb("h2e", [128, 8, 256], BF16, ph)
        gg, gg_t = sb("gge", [128, 22, 256], BF16, ph)
        slb = [sb("sle%d" % i, [128, 256], F32, ph) for i in range(2)]
        xout, xout_t = sb("xoe", [128, 8, 256], F32, ph)
        c_a, c_b, c_w, c_s = [0], [0], [0], [0]

        def loadE(i):
            t0, n = TILES256[i]
            xT, xt = xTb[i % 2]
            for k in range(8):
                P.dma("sp", xT[:, k, :n], XM[k, :, t0:t0 + n], reads=[XMt], writes=[xt])

        loadE(0)
        for i, (t0, n) in enumerate(TILES256):
            if i + 1 < len(TILES256):
                loadE(i + 1)
            s = 1 if i == 0 else 0
            xT, xt = xTb[i % 2]
            norm_mod(xT, xt, n, 0, s, GM_F, SH_F, h2, h2_t, sq, sq_t, rstd, rstd_t, tmp, tmp_t, 0)
            for c in range(22):
                ps1, ps1t = P.ps([1, 2, 3, 4], c_a)
                for k in range(8):
                    MM(ps1[:, :n], w1[:, k, c * 128:(c + 1) * 128], h2[:, k, :n], k == 0, k == 7, [w1_t, h2_t], ps1t)
                ps3, ps3t = P.ps([1, 2, 3, 4], c_a)
                for k in range(8):
                    MM(ps3[:, :n], w3[:, k, c * 128:(c + 1) * 128], h2[:, k, :n], k == 0, k == 7, [w3_t, h2_t], ps3t)
                sl, slt = slb[c_s[0] % 2]
                c_s[0] += 1
                ACT(sl[:, :n], ps1[:, :n], AF.Silu, [ps1t], [slt])
                TTo(gg[:, c, :n], sl[:, :n], ps3[:, :n], ALU.mult, [slt, ps3t], [gg_t])
            for m in range(8):
                wb, wbt = w2b[c_w[0] % 2]
                c_w[0] += 1
                P.dma("pool", wb[:], ffn_w2[:, :, m * 128:(m + 1) * 128], writes=[wbt])
                ps, pst = P.ps([5, 6, 7], c_b)
                for c in range(22):
                    MM(ps[:, :n], wb[:, c, :], gg[:, c, :n], c == 0, c == 21, [wbt, gg_t], pst)
                ACT(tmp[:, m, :n], ps[:, :n], AF.Copy, [pst], [tmp_t])
            post_res(tmp, tmp_t, n, 0, s, GG_F, xT, xt, sq, sq_t, rstd, rstd_t, 0, xout, xout_t)
            for k in range(8):
                P.dma("sp", X1[k, :, t0:t0 + n], xout[:, k, :n], reads=[xout_t], writes=[X1t])
        P.barrier()
    if upto == "E":
        return finish(nc, P, outT)


    w1_in = din("w1_in", [128, 8, 4128])
    w1_out = din("w1_out", [128, 8, 1024])
    selc = din("selc", [16, 16, 128])
    gmask = din("gmask", [64, 4, 64])
    tri = din("tri", [64, 2, 64])
    router = din("router", [128, 8, 8])
    moe_w1 = [din("moe_w1_%d" % e, [1024, 3584]) for e in range(8)]
    moe_w3 = [din("moe_w3_%d" % e, [1024, 3584]) for e in range(8)]
    moe_w2 = [din("moe_w2_%d" % e, [3584, 1024]) for e in range(8)]
    QKV = dscr("QKV", [24, 128, TT], BF16)
    Z1 = dscr("Z1", [8, 128, TT])
    OD = dscr("OD", [2, 8, 128, TT])
    QKVt, Z1t, ODt = Tr(), Tr(), Tr()
    NCH = TT // 64

    def norm_mod_v(xs, x_t, n, l, s, which_gm, which_sh, hs, h_t, sq, sq_t, rstd, rstd_t, tmp, tmp_t, bank):
        rms_rstd(xs, x_t, n, sq, sq_t, rstd, rstd_t, bank)
        for k in range(8):
            TTo(tmp[:, k, :n], xs[:, k, :], rstd[:, :n], ALU.mult, [x_t, rstd_t], [tmp_t])
        for k in range(8):
            ACT(hs[:, k, :], tmp[:, k, :n], AF.Identity, [tmp_t, DVt], [h_t],
                scale=dv(l, s, which_gm, k), bias=dv(l, s, which_sh, k))

    with ExitStack() as l1:
        GT, GT_t = sb("GT", [64, NCH, 16], F32, l1)
        BT, BT_t = sb("BT", [64, NCH, 16], F32, l1)
        with ExitStack() as ph:
            w_in, w_in_t = sb("w1in", [128, 8, 4128], BF16, ph)
            for k in range(8):
                P.dma("pool", w_in[:, k, :], w1_in[:, k, :], writes=[w_in_t])
            nea, nea_t = sb("nea", [64, 16], F32, ph)
            alo, dbo, cwo = VO["a_log"], VO["dt_bias"], VO["conv_w"]
            ACT(nea[:], V[:64, alo:alo + 16], AF.Exp, [Vt], [nea_t])
            ACT(nea[:], nea[:], AF.Copy, [nea_t], [nea_t], scale=-1.0)
            eps128, eps128_t = sb("eps128", [128, 1], F32, ph)
            P.op("dve", lambda e: e.memset(eps128[:], 128.0 * EPS), writes=[eps128_t])
            W = 260
            xTb = [sb("xTf%d" % i, [128, 8, W], F32, ph) for i in range(2)]
            sq, sq_t = sb("sqf", [128, 8, W], BF16, ph)
            rstd, rstd_t = sb("rstdf", [128, W], F32, ph)
            tmp, tmp_t = sb("tmpf", [128, 8, W], F32, ph)
            h, h_t = sb("hf", [128, 8, W], BF16, ph)
            pcb = [sb("pc%d" % i, [128, W], F32, ph) for i in range(4)]
            accb = [sb("acc%d" % i, [128, 256], F32, ph) for i in range(4)]
            silb = [sb("sil%d" % i, [128, 256], F32, ph) for i in range(4)]
            sq2b = [sb("sq2%d" % i, [128, 256], BF16, ph) for i in range(4)]
            rs2b = [sb("rs2%d" % i, [128, 256], F32, ph) for i in range(4)]
            obb = [sb("ob%d" % i, [128, 256], BF16, ph) for i in range(4)]
            zbb = [sb("zb%d" % i, [128, 256], F32, ph) for i in range(2)]
            ta, ta_t = sb("ta", [64, 4, 16], F32, ph)
            te, te_t = sb("te", [64, 4, 16], F32, ph)
            c_a, c_n, c_m, c_o, c_z = [0], [0], [0], [0], [0]

            def rngF(i):
                t0 = 256 * i
                lo = 2 if i in (0, 1) else 0
                hi = 258 if i in (0, 16) else 260
                return t0, lo, hi

            def loadF(i):
                t0, lo, hi = rngF(i)
                xT, xt = xTb[i % 2]
                for k in range(8):
                    P.dma("sp", xT[:, k, lo:hi], X1[k, :, t0 - 2 + lo:t0 - 2 + hi], reads=[X1t], writes=[xt])

            NTF = int(os.environ.get('NTF', 17))
            loadF(0)
            for i in range(NTF):
                if i + 1 < NTF:
                    loadF(i + 1)
                t0, lo, hi = rngF(i)
                nv = hi - lo
                s = 1 if i == 0 else 0
                xT, xt = xTb[i % 2]
                norm_mod_v(xT[:, :, lo:hi], xt, nv, 1, s, GM_M, SH_M, h[:, :, lo:hi], h_t, sq, sq_t, rstd, rstd_t, tmp, tmp_t, 0)
                for m in range(32):
                    ps, pst = P.ps([1, 2, 3, 7], c_a)
                    for k in range(8):
                        MM(ps[:, :nv], w_in[:, k, m * 128:(m + 1) * 128], h[:, k, lo:hi], k == 0, k == 7, [w_in_t, h_t], pst)
                    if m >= 24:
                        zb, zbt = zbb[c_z[0] % 2]
                        c_z[0] += 1
                        ACT(zb[:], ps[:, 2 - lo:258 - lo], AF.Copy, [pst], [zbt])
                        P.dma("sp", Z1[m - 24, :, t0:t0 + 256], zb[:], reads=[zbt], writes=[Z1t])
                        continue
                    pc, pct = pcb[c_m[0] % 4]
                    acc, acct = accb[c_m[0] % 4]
                    sil, silt = silb[c_m[0] % 4]
                    sq2, sq2t = sq2b[c_m[0] % 4]
                    rs2, rs2t = rs2b[c_m[0] % 4]
                    c_m[0] += 1
                    if lo > 0:
                        P.op("dve", lambda e: e.memset(pc[:, 0:2], 0.0), writes=[pct])
                    if hi < W:
                        P.op("dve", lambda e: e.memset(pc[:, 258:260], 0.0), writes=[pct])
                    ACT(pc[:, lo:hi], ps[:, :nv], AF.Copy, [pst], [pct])
                    ACT(acc[:], pc[:, 0:256], AF.Copy, [pct, Vt], [acct], scale=V[:, cwo + m:cwo + m + 1])
                    for j in range(1, 5):
                        STT(acc[:], pc[:, j:j + 256], V[:, cwo + j * 24 + m:cwo + j * 24 + m + 1], acc[:], ALU.mult, ALU.add,
                            [pct, Vt, acct], [acct])
                    ACT(sil[:], acc[:], AF.Silu, [acct], [silt])
                    ob, obt = obb[c_o[0] % 4]
                    c_o[0] += 1
                    if m < 16:
                        ACT(sq2[:], sil[:], AF.Square, [silt], [sq2t])
                        pn, pnt = P.ps([4, 5], c_n)
                        MM(pn[:, :256], ones_bf[:], sq2[:], True, True, [ones_t, sq2t], pnt)
                        if m < 8:
                            ACT(rs2[:], pn[:, :256], AF.Sqrt, [pnt, eps128_t], [rs2t], scale=128.0, bias=eps128[:, 0:1])
                        else:
                            ACT(rs2[:], pn[:, :256], AF.Sqrt, [pnt, eps_t], [rs2t], scale=1.0, bias=epsc[:, 0:1])
                        P.op("dve", lambda e: e.reciprocal(rs2[:], rs2[:]), reads=[rs2t], writes=[rs2t])
                        TTo(ob[:], sil[:], rs2[:], ALU.mult, [silt, rs2t], [obt])
                    else:
                        CP(ob[:], sil[:], [silt], [obt])
                    P.dma("sp", QKV[m, :, t0:t0 + 256], ob[:], reads=[obt], writes=[QKVt])
                pab, pabt = P.psl[6]
                for cc in range(4):
                    for k in range(8):
                        MM(pab[:64, cc * 32:(cc + 1) * 32], h[:, k, 2 + cc * 64:2 + (cc + 1) * 64], w_in[:, k, 4096:4128],
                           k == 0, k == 7, [w_in_t, h_t], pabt)
                for cc in range(4):
                    pv = pab[:64, cc * 32:(cc + 1) * 32].rearrange("p (d ab h) -> p d ab h", d=2, ab=2)
                    TTo(ta[:, cc, :].rearrange("p (d h) -> p d h", d=2), pv[:, :, 0, :],
                        V[:64, dbo:dbo + 16].rearrange("p (d h) -> p d h", d=2), ALU.add, [pabt, Vt], [ta_t])
                ACT(te[:], ta[:], AF.Exp, [ta_t], [te_t])
                ACT(ta[:], te[:], AF.Ln, [te_t, onesf_t], [ta_t], bias=ones_f[:64, 0:1])
                for cc in range(4):
                    c = t0 // 64 + cc
                    pv = pab[:64, cc * 32:(cc + 1) * 32].rearrange("p (d ab h) -> p d ab h", d=2, ab=2)
                    TTo(GT[:, c, :], ta[:, cc, :], nea[:], ALU.mult, [ta_t, nea_t], [GT_t])
                    ACT(BT[:, c, :].rearrange("p (d h) -> p d h", d=2), pv[:, :, 1, :], AF.Sigmoid, [pabt], [BT_t])
            P.barrier()
        if upto == "F":
            dg = dscr("dGT", [64, NCH, 16])
            db = dscr("dBT", [64, NCH, 16])
            P.dma("sp", dg, GT[:], reads=[GT_t])
            P.dma("sp", db, BT[:], reads=[BT_t])
            return finish(nc, P, outT)

        with ExitStack() as ph:
            GAMc, GAMc_t = sb("GAMc", [64, NCH, 16], F32, ph)
            GLb, GLb_t = sb("GLb", [128, NCH, 16], F32, ph)
            CD, CD_t = sb("CD", [128, NCH, 16], F32, ph)
            CKD, CKD_t = sb("CKD", [64, NCH, 16], F32, ph)
            CKB, CKB_t = sb("CKB", [64, NCH, 16], F32, ph)
            GAMr, GAMr_t = sb("GAMr", [16, TT], F32, ph)
            NGAMr, NGAMr_t = sb("NGAMr", [16, TT], F32, ph)
            Sel, Sel_t = sb("Sel", [16, 16, 128], F32, ph)
            gm, gm_t = sb("gm", [64, 4, 64], F32, ph)
            trt, trt_t = sb("trt", [64, 2, 64], F32, ph)
            P.dma("sp", Sel[:], selc, writes=[Sel_t])
            P.dma("sp", gm[:], gmask, writes=[gm_t])
            P.dma("sp", trt[:], tri, writes=[trt_t])
            for c0 in range(0, NCH, 32):
                c1 = min(NCH, c0 + 32)
                n = (c1 - c0) * 16
                rhs = GT[:, c0:c1, :]
                psF, psFt = P.psl[0]
                psB, psBt = P.psl[1]
                psL, psLt = P.psl[2]
                MM(psF[:64, :n].rearrange("p (c x) -> p c x", x=16), trt[:, 0, :], rhs, True, True, [trt_t, GT_t], psFt)
                MM(psB[:64, :n].rearrange("p (c x) -> p c x", x=16), trt[:, 1, :], rhs, True, True, [trt_t, GT_t], psBt)
                MM(psL[:, :n].rearrange("p (c x) -> p c x", x=16), ones_f[:64, :], rhs, True, True, [onesf_t, GT_t], psLt)
                ACT(GAMc[:, c0:c1, 0:8], psF[:64, :n].rearrange("p (c x) -> p c x", x=16)[:, :, 0:8], AF.Copy, [psFt], [GAMc_t])
                ACT(GAMc[:, c0:c1, 8:16], psB[:64, :n].rearrange("p (c x) -> p c x", x=16)[:, :, 8:16], AF.Copy, [psBt], [GAMc_t])
                CP(GLb[:, c0:c1, :], psL[:, :n].rearrange("p (c x) -> p c x", x=16), [psLt], [GLb_t])
            ACT(CD[:], GLb[:], AF.Exp, [GLb_t], [CD_t])
            TTo(CKD[:], GLb[:64], GAMc[:], ALU.subtract, [GLb_t, GAMc_t], [CKD_t])
            ACT(CKD[:], CKD[:], AF.Exp, [CKD_t], [CKD_t])
            ACT(CKB[:], GAMc[:], AF.Exp, [GAMc_t], [CKB_t])
            TTo(CKB[:], CKB[:], BT[:], ALU.mult, [CKB_t, BT_t], [CKB_t])
            for c in range(NCH):
                psr, psrt = P.psl[3 + (c // 8) % 2]
                MM(psr[:16, (c % 8) * 64:(c % 8 + 1) * 64], GAMc[:, c, :], identF[:64, :64], True, True, [GAMc_t, identF_t], psrt)
                if c % 8 == 7 or c == NCH - 1:
                    cb = (c // 8) * 8
                    n = (c + 1 - cb) * 64
                    ACT(GAMr[:, cb * 64:cb * 64 + n], psr[:16, :n], AF.Copy, [psrt], [GAMr_t])
                    ACT(NGAMr[:, cb * 64:cb * 64 + n], psr[:16, :n], AF.Copy, [psrt], [NGAMr_t], scale=-1.0)
            P.barrier()

            cur = {"bank": 0, "used": 0}

            def psg(w=64):
                if cur["used"] + w > 256:
                    cur["bank"] = (cur["bank"] + 1) % 8
                    cur["used"] = 0
                b, o = cur["bank"], cur["used"]
                cur["used"] += w
                t, tr = P.psl[b]
                return t[:, o:o + w], tr

            CHN = []
            for hh in range(8):
                c = {}
                for nm, shp, dt in [("Dm", [64, 64], F32), ("E", [64, 64], F32), ("ELs", [64, 64], F32), ("ELi", [64, 64], F32),
                                    ("M", [64, 64], BF16), ("Y", [64, 64], BF16), ("QKm", [64, 64], BF16), ("QKmT", [64, 64], BF16),
                                    ("Xa", [64, 64], BF16), ("Xb", [64, 64], BF16), ("Ya", [64, 64], BF16), ("Yb", [64, 64], BF16),
                                    ("Pa", [64, 64], BF16), ("Pb", [64, 64], BF16),
                                    ("Kbg", [64, 128], BF16), ("Kd", [64, 128], BF16), ("Vb", [64, 128], BF16), ("vn", [64, 128], BF16),
                                    ("nWT", [128, 64], BF16), ("EGe", [128, 64], F32), ("qg", [128, 64], BF16),
                                    ("S", [128, 128], F32), ("Sb", [128, 128], BF16), ("OB", [128, 256], F32)]:
                    c[nm] = sb("%s%d" % (nm, hh), shp, dt, ph)
                CHN.append(c)
            stg = [sb("stg%d" % i, [128, 24, 256], BF16, ph) for i in range(2)]
            idB = identB[:64, :64]
            idF = identF[:64, :64]
            NEU = 5
            for d in range(2):
                order = list(range(NCH)) if d == 0 else [3, 2, 1, 0] + list(range(NCH - 1, 3, -1))
                order = order[:int(os.environ.get('NSTEP', len(order)))]
                groups = []
                for c in order:
                    if not groups or groups[-1] != c // 4:
                        groups.append(c // 4)
                gpos = {}

                def loadG(gi):
                    g4 = groups[gi]
                    st, stt = stg[gi % 2]
                    for m in range(24):
                        P.dma("sp", st[:, m, :], QKV[m, :, g4 * 256:(g4 + 1) * 256], reads=[QKVt], writes=[stt])
                    gpos[g4] = gi

                for hh in range(8):
                    S, St = CHN[hh]["S"]
                    Sb, Sbt = CHN[hh]["Sb"]
                    P.op("dve", lambda e: e.memset(S[:], 0.0), writes=[St])
                    P.op("dve", lambda e: e.memset(Sb[:], 0.0), writes=[Sbt])
                loadG(0)
                for c in order:
                    g4 = c // 4
                    gi = gpos[g4]
                    if c == order[0] or (c // 4 != prev_c // 4):
                        if gi + 1 < len(groups):
                            loadG(gi + 1)
                    prev_c = c
                    st, stt = stg[gi % 2]
                    o64 = (c % 4) * 64
                    tk = slice(c * 64, (c + 1) * 64)
                    is_lat = c >= 4
                    mS, mI = (0, 1) if d == 0 else (2, 3)
                    T = [dict() for _ in range(8)]
                    for hh in range(8):
                        dh = d * 8 + hh
                        T[hh]["qT"] = st[:, hh, o64:o64 + 64]
                        T[hh]["kT"] = st[:, 8 + hh, o64:o64 + 64]
                        T[hh]["vT"] = st[:, 16 + hh, o64:o64 + 64]
                        ps, pst = psg()
                        MM(ps[:64, :64], GAMr[:, tk], Sel[:, dh, :64], True, False, [GAMr_t, Sel_t], pst)
                        MM(ps[:64, :64], Sel[:, dh, :64], NGAMr[:, tk], False, True, [NGAMr_t, Sel_t], pst)
                        T[hh]["psD"] = (ps, pst)
                    for hh in range(8):
                        ps, pst = T[hh]["psD"]
                        Dm, Dmt = CHN[hh]["Dm"]
                        TS(Dm[:], ps[:64, :64], 0.0, 0.0, ALU.min, ALU.add, [pst], [Dmt])
                    for hh in range(8):
                        Dm, Dmt = CHN[hh]["Dm"]
                        E, Et = CHN[hh]["E"]
                        ACT(E[:], Dm[:], AF.Exp, [Dmt], [Et])
                    for hh in range(8):
                        E, Et = CHN[hh]["E"]
                        ELs, ELst = CHN[hh]["ELs"]
                        ELi, ELit = CHN[hh]["ELi"]
                        TTo(ELs[:], E[:], gm[:, mS, :], ALU.mult, [Et, gm_t], [ELst])
                        TTo(ELi[:], E[:], gm[:, mI, :], ALU.mult, [Et, gm_t], [ELit])
                    for hh in range(8):
                        ps, pst = psg()
                        MM(ps[:64, :64], T[hh]["kT"], T[hh]["kT"], True, True, [stt], pst)
                        T[hh]["psG"] = (ps, pst)
                        ps, pst = psg()
                        MM(ps[:64, :64], T[hh]["qT"], T[hh]["kT"], True, True, [stt], pst)
                        T[hh]["psQ"] = (ps, pst)
                    for hh in range(8):
                        dh = d * 8 + hh
                        ps, pst = T[hh]["psG"]
                        M, Mt = CHN[hh]["M"]
                        ELs, ELst = CHN[hh]["ELs"]
                        STT(M[:], ps[:64, :64], BT[:, c, dh:dh + 1], ELs[:], ALU.mult, ALU.mult, [pst, BT_t, ELst], [Mt])
                        ps, pst = T[hh]["psQ"]
                        QKm, QKmt = CHN[hh]["QKm"]
                        ELi, ELit = CHN[hh]["ELi"]
                        TTo(QKm[:], ps[:64, :64], ELi[:], ALU.mult, [pst, ELit], [QKmt])
                    for hh in range(8):
                        M, Mt = CHN[hh]["M"]
                        QKm, QKmt = CHN[hh]["QKm"]
                        ps, pst = psg()
                        MM(ps[:64, :64], M[:], idB, True, True, [Mt, identB_t], pst)
                        T[hh]["psY"] = (ps, pst)
                        ps, pst = psg()
                        MM(ps[:64, :64], QKm[:], idB, True, True, [QKmt, identB_t], pst)
                        T[hh]["psQT"] = (ps, pst)
                    for hh in range(8):
                        ps, pst = T[hh]["psY"]
                        Y, Yt = CHN[hh]["Y"]
                        Pa, Pat = CHN[hh]["Pa"]
                        ACT(Y[:], ps[:64, :64], AF.Copy, [pst], [Yt])
                        STT(Pa[:], ps[:64, :64], -1.0, idF, ALU.mult, ALU.add, [pst, identF_t], [Pat])
                        ps, pst = T[hh]["psQT"]
                        QKmT, QKmTt = CHN[hh]["QKmT"]
                        ACT(QKmT[:], ps[:64, :64], AF.Copy, [pst], [QKmTt])
                        T[hh]["X"] = CHN[hh]["M"]
                        T[hh]["Yc"] = CHN[hh]["Y"]
                        T[hh]["P"] = CHN[hh]["Pa"]
                    for r in range(NEU):
                        xn, yn, pn = ("Xa", "Ya", "Pb") if r % 2 == 0 else ("Xb", "Yb", "Pa")
                        for hh in range(8):
                            X, Xt = T[hh]["X"]
                            Yc, Yct = T[hh]["Yc"]
                            ps, pst = psg()
                            MM(ps[:64, :64], Yc[:], X[:], True, True, [Xt, Yct], pst)
                            T[hh]["psX"] = (ps, pst)
                            if r < NEU - 1:
                                ps, pst = psg()
                                MM(ps[:64, :64], X[:], Yc[:], True, True, [Xt, Yct], pst)
                                T[hh]["psYn"] = (ps, pst)
                        for hh in range(8):
                            Xn, Xnt = CHN[hh][xn]
                            ps, pst = T[hh]["psX"]
                            ACT(Xn[:], ps[:64, :64], AF.Copy, [pst], [Xnt])
                            if r < NEU - 1:
                                Yn, Ynt = CHN[hh][yn]
                                ps, pst = T[hh]["psYn"]
                                CP(Yn[:], ps[:64, :64], [pst], [Ynt])
                                T[hh]["Yc"] = CHN[hh][yn]
                            T[hh]["X"] = CHN[hh][xn]
                        for hh in range(8):
                            X, Xt = T[hh]["X"]
                            Pc, Pct = T[hh]["P"]
                            ps, pst = psg()
                            MM(ps[:64, :64], X[:], Pc[:], True, True, [Xt, Pct], pst)
                            T[hh]["psP"] = (ps, pst)
                        for hh in range(8):
                            Pc, Pct = T[hh]["P"]
                            Pn, Pnt = CHN[hh][pn]
                            ps, pst = T[hh]["psP"]
                            TTo(Pn[:], ps[:64, :64], Pc[:], ALU.add, [pst, Pct], [Pnt])
                            T[hh]["P"] = CHN[hh][pn]
                    for hh in range(8):
                        ps, pst = psg(128)
                        MM(ps[:64, :128], T[hh]["kT"], identB[:], True, True, [stt, identB_t], pst)
                        T[hh]["pskt"] = (ps, pst)
                        ps, pst = psg(128)
                        MM(ps[:64, :128], T[hh]["vT"], identB[:], True, True, [stt, identB_t], pst)
                        T[hh]["psvt"] = (ps, pst)
                    for hh in range(8):
                        dh = d * 8 + hh
                        ps, pst = T[hh]["pskt"]
                        Kbg, Kbgt = CHN[hh]["Kbg"]
                        Kd, Kdt = CHN[hh]["Kd"]
                        ACT(Kbg[:], ps[:64, :128], AF.Copy, [pst, CKB_t], [Kbgt], scale=CKB[:, c, dh:dh + 1])
                        TS(Kd[:], ps[:64, :128], CKD[:, c, dh:dh + 1], 0.0, ALU.mult, ALU.add, [pst, CKD_t], [Kdt])
                        ps, pst = T[hh]["psvt"]
                        Vb, Vbt = CHN[hh]["Vb"]
                        ACT(Vb[:], ps[:64, :128], AF.Copy, [pst, BT_t], [Vbt], scale=BT[:, c, dh:dh + 1])
                    for hh in range(8):
                        dh = d * 8 + hh
                        Kbg, Kbgt = CHN[hh]["Kbg"]
                        Pc, Pct = T[hh]["P"]
                        ps, pst = psg()
                        MM(ps[:, :64], Kbg[:], Pc[:], True, True, [Kbgt, Pct], pst)
                        T[hh]["psW"] = (ps, pst)
                        ps, pst = psg()
                        MM(ps[:, :64], Sel[:, dh, :], GAMr[:, tk], True, True, [Sel_t, GAMr_t], pst)
                        T[hh]["pse"] = (ps, pst)
                    for hh in range(8):
                        ps, pst = T[hh]["psW"]
                        nWT, nWTt = CHN[hh]["nWT"]
                        ACT(nWT[:], ps[:, :64], AF.Copy, [pst], [nWTt], scale=-1.0)
                        ps, pst = T[hh]["pse"]
                        EGe, EGet = CHN[hh]["EGe"]
                        qg, qgt = CHN[hh]["qg"]
                        ACT(EGe[:], ps[:, :64], AF.Exp, [pst], [EGet])
                        TTo(qg[:], T[hh]["qT"], EGe[:], ALU.mult, [stt, EGet], [qgt])
                    for hh in range(8):
                        Pc, Pct = T[hh]["P"]
                        Vb, Vbt = CHN[hh]["Vb"]
                        nWT, nWTt = CHN[hh]["nWT"]
                        Sb, Sbt = CHN[hh]["Sb"]
                        ps, pst = psg(128)
                        MM(ps[:64, :128], Pc[:], Vb[:], True, False, [Pct, Vbt], pst)
                        MM(ps[:64, :128], nWT[:], Sb[:], False, True, [nWTt, Sbt], pst)
                        T[hh]["psv"] = (ps, pst)
                    for hh in range(8):
                        ps, pst = T[hh]["psv"]
                        vn, vnt = CHN[hh]["vn"]
                        ACT(vn[:], ps[:64, :128], AF.Copy, [pst], [vnt])
                    for hh in range(8):
                        vn, vnt = CHN[hh]["vn"]
                        Sb, Sbt = CHN[hh]["Sb"]
                        qg, qgt = CHN[hh]["qg"]
                        QKmT, QKmTt = CHN[hh]["QKmT"]
                        Kd, Kdt = CHN[hh]["Kd"]
                        if is_lat:
                            ps, pst = psg()
                            MM(ps[:, :64], Sb[:], qg[:], True, False, [Sbt, qgt], pst)
                            MM(ps[:, :64], vn[:], QKmT[:], False, True, [vnt, QKmTt], pst)
                            T[hh]["pso"] = (ps, pst)
                        ps, pst = psg(128)
                        MM(ps[:, :128], Kd[:], vn[:], True, True, [Kdt, vnt], pst)
                        T[hh]["psS"] = (ps, pst)
                    for hh in range(8):
                        dh = d * 8 + hh
                        S, St = CHN[hh]["S"]
                        Sb, Sbt = CHN[hh]["Sb"]
                        OB, OBt = CHN[hh]["OB"]
                        if is_lat:
                            ps, pst = T[hh]["pso"]
                            CP(OB[:, o64:o64 + 64], ps[:, :64], [pst], [OBt])
                        ps, pst = T[hh]["psS"]
                        STT(S[:], S[:], CD[:, c, dh:dh + 1], ps[:, :128], ALU.mult, ALU.add, [St, CD_t, pst], [St])
                        ACT(Sb[:], S[:], AF.Copy, [St], [Sbt])
                        last_in_group = (c % 4 == 3) if d == 0 else (c % 4 == 0)
                        if is_lat and last_in_group:
                            P.dma("sp", OD[d, hh, :, g4 * 256:(g4 + 1) * 256], OB[:], reads=[OBt], writes=[ODt])
            P.barrier()
    if upto == "G":
        return finish(nc, P, outT)


    LT512 = [(256 + 512 * i, 512) for i in range(8)]
    with ExitStack() as ph:
        wo, wo_t = sb("wo1", [128, 8, 1024], BF16, ph)
        for k in range(8):
            P.dma("pool", wo[:, k, :], w1_out[:, k, :], writes=[wo_t])
        ono = VO["out_norm"]
        xT, xt = sb("xTh", [128, 8, 512], F32, ph)
        o0b = [sb("o0b%d" % i, [128, 512], F32, ph) for i in range(2)]
        o1b = [sb("o1b%d" % i, [128, 512], F32, ph) for i in range(2)]
        zbh = [sb("zbh%d" % i, [128, 512], F32, ph) for i in range(2)]
        sqh = [sb("sqh%d" % i, [128, 512], BF16, ph) for i in range(2)]
        rsh = [sb("rsh%d" % i, [128, 512], F32, ph) for i in range(2)]
        yg, yg_t = sb("yg", [128, 8, 512], BF16, ph)
        yb, yb_t = sb("ybh", [128, 8, 512], F32, ph)
        sq, sq_t = sb("sqhh", [128, 8, 512], BF16, ph)
        rstd, rstd_t = sb("rstdh", [128, 512], F32, ph)
        c_h, c_n, c_y = [0], [0], [0]
        XM2t = Tr()
        for (t0, n) in LT512:
            for k in range(8):
                P.dma("sp", xT[:, k, :], X1[k, :, t0:t0 + n], reads=[X1t], writes=[xt])
            for hh in range(8):
                o0, o0t = o0b[c_h[0] % 2]
                o1, o1t = o1b[c_h[0] % 2]
                zb, zbt = zbh[c_h[0] % 2]
                sqq, sqqt = sqh[c_h[0] % 2]
                rs, rst = rsh[c_h[0] % 2]
                c_h[0] += 1
                P.dma("sp", o0[:], OD[0, hh, :, t0:t0 + n], reads=[ODt], writes=[o0t])
                P.dma("sp", o1[:], OD[1, hh, :, t0:t0 + n], reads=[ODt], writes=[o1t])
                P.dma("sp", zb[:], Z1[hh, :, t0:t0 + n], reads=[Z1t], writes=[zbt])
                TTo(o0[:], o0[:], o1[:], ALU.add, [o0t, o1t], [o0t])
                ACT(sqq[:], o0[:], AF.Square, [o0t], [sqqt])
                pn, pnt = P.ps([4, 5], c_n)
                MM(pn[:, :n], ones_bf[:], sqq[:], True, True, [ones_t, sqqt], pnt)
                ACT(rs[:], pn[:, :n], AF.Sqrt, [pnt, eps_t], [rst], scale=1.0 / 128, bias=epsc[:, 0:1])
                P.op("dve", lambda e: e.reciprocal(rs[:], rs[:]), reads=[rst], writes=[rst])
                ACT(zb[:], zb[:], AF.Silu, [zbt], [zbt])
                TTo(o0[:], o0[:], rs[:], ALU.mult, [o0t, rst], [o0t])
                STT(yg[:, hh, :], o0[:], V[:, ono:ono + 1], zb[:], ALU.mult, ALU.mult, [o0t, Vt, zbt], [yg_t])
            for m in range(8):
                ps, pst = P.ps([1, 2, 3], c_y)
                for hh in range(8):
                    MM(ps[:, :n], wo[:, hh, m * 128:(m + 1) * 128], yg[:, hh, :], hh == 0, hh == 7, [wo_t, yg_t], pst)
                ACT(yb[:, m, :], ps[:, :n], AF.Copy, [pst], [yb_t])
            post_res(yb, yb_t, n, 1, 0, GG_M, xT, xt, sq, sq_t, rstd, rstd_t, 0, yb, yb_t)
            for k in range(8):
                P.dma("sp", XM[k, :, t0:t0 + n], yb[:, k, :], reads=[yb_t], writes=[XM2t])
        P.barrier()
    if upto == "H":
        return finish(nc, P, outT)

    with ExitStack() as ph:
        NTP = 2
        rt, rt_t = sb("rt", [128, 8, 8], F32, ph)
        P.dma("sp", rt[:], router, writes=[rt_t])
        Sel, Sel_t = sb("Sel2", [16, 16, 128], F32, ph)
        P.dma("sp", Sel[:], selc, writes=[Sel_t])
        xT, xt = sb("xTi", [128, 8, 512], F32, ph)
        tmp, tmp_t = sb("tmpi", [128, 8, 512], F32, ph)
        sq, sq_t = sb("sqi", [128, 8, 512], BF16, ph)
        rstd, rstd_t = sb("rstdi", [128, 512], F32, ph)
        h2s = [sb("h2i%d" % i, [128, 8, 512], BF16, ph) for i in range(NTP)]
        yaccs = [sb("yacc%d" % i, [128, 8, 512], F32, ph) for i in range(NTP)]
        cwbs = [sb("cwb%d" % i, [128, 8, 512], BF16, ph) for i in range(NTP)]
        ggb = [sb("ggi%d" % i, [128, 4, 512], BF16, ph) for i in range(3)]
        w1bb = [sb("w1b%d" % i, [128, 8, 512], BF16, ph) for i in range(2)]
        w3bb = [sb("w3b%d" % i, [128, 8, 512], BF16, ph) for i in range(2)]
        w2bb = [sb("w2bi%d" % i, [128, 4, 1024], BF16, ph) for i in range(2)]
        slb = [sb("sli%d" % i, [128, 512], F32, ph) for i in range(2)]
        tb_ = [sb("tbi%d" % i, [128, 512], F32, ph) for i in range(2)]
        lg, lg_t = sb("lg", [128, 4, 8], F32, ph)
        l2, l2_t = sb("l2", [128, 4, 8], F32, ph)
        mk1, mk1_t = sb("mk1", [128, 4, 8], F32, ph)
        mk2, mk2_t = sb("mk2", [128, 4, 8], F32, ph)
        comb, comb_t = sb("comb", [128, 4, 8], F32, ph)
        m1, m1_t = sb("m1", [128, 4], F32, ph)
        m2, m2_t = sb("m2", [128, 4], F32, ph)
        g1, g1_t = sb("g1", [128, 4], F32, ph)
        g2, g2_t = sb("g2", [128, 4], F32, ph)
        cT, cT_t = sb("cTm", [8, 512], F32, ph)
        c_a, c_b, c_w, c_s, c_g = [0], [0], [0], [0], [0]
        OUTt = Tr()
        NEXP = int(os.environ.get('NEXP', 8))
        NTI = int(os.environ.get('NTI', 8))
        for tp in range(0, NTI, NTP):
            tiles = LT512[tp:tp + NTP]
            for ti, (t0, n) in enumerate(tiles):
                h2, h2_t = h2s[ti]
                cwb, cwb_t = cwbs[ti]
                for k in range(8):
                    P.dma("sp", xT[:, k, :], XM[k, :, t0:t0 + n], reads=[XM2t], writes=[xt])
                rms_rstd(xT[:, :, :], xt, n, sq, sq_t, rstd, rstd_t, 0)
                for k in range(8):
                    TTo(tmp[:, k, :], xT[:, k, :], rstd[:, :], ALU.mult, [xt, rstd_t], [tmp_t])
                for k in range(8):
                    ACT(tmp[:, k, :], tmp[:, k, :], AF.Identity, [tmp_t, DVt], [tmp_t], scale=dv(1, 0, GM_F, k), bias=dv(1, 0, SH_F, k))
                for k in range(8):
                    CP(h2[:, k, :], tmp[:, k, :], [tmp_t], [h2_t])
                pr, prt = P.psl[6]
                for b in range(4):
                    for k in range(8):
                        MM(pr[:, b * 8:(b + 1) * 8], tmp[:, k, b * 128:(b + 1) * 128], rt[:, k, :], k == 0, k == 7, [tmp_t, rt_t], prt)
                ACT(lg[:].rearrange("p b e -> p (b e)"), pr[:, :32], AF.Copy, [prt], [lg_t])
                P.op("dve", lambda e: e.tensor_reduce(out=m1[:], in_=lg[:], axis=AX.X, op=ALU.max), reads=[lg_t], writes=[m1_t])
                for b in range(4):
                    TS(mk1[:, b, :], lg[:, b, :], m1[:, b:b + 1], 0.0, ALU.is_equal, ALU.add, [lg_t, m1_t], [mk1_t])
                STT(l2[:], mk1[:], -1e30, lg[:], ALU.mult, ALU.add, [mk1_t, lg_t], [l2_t])
                P.op("dve", lambda e: e.tensor_reduce(out=m2[:], in_=l2[:], axis=AX.X, op=ALU.max), reads=[l2_t], writes=[m2_t])
                for b in range(4):
                    TS(mk2[:, b, :], l2[:, b, :], m2[:, b:b + 1], 0.0, ALU.is_equal, ALU.add, [l2_t, m2_t], [mk2_t])
                TTo(g1[:], m1[:], m2[:], ALU.subtract, [m1_t, m2_t], [g1_t])
                ACT(g1[:], g1[:], AF.Sigmoid, [g1_t], [g1_t])
                TS(g2[:], g1[:], -1.0, 1.0, ALU.mult, ALU.add, [g1_t], [g2_t])
                for b in range(4):
                    TS(comb[:, b, :], mk1[:, b, :], g1[:, b:b + 1], 0.0, ALU.mult, ALU.add, [mk1_t, g1_t], [comb_t])
                    STT(comb[:, b, :], mk2[:, b, :], g2[:, b:b + 1], comb[:, b, :], ALU.mult, ALU.add, [mk2_t, g2_t, comb_t], [comb_t])
                pt, ptt = P.psl[7]
                for b in range(4):
                    MM(pt[:8, b * 128:(b + 1) * 128], comb[:, b, :], identF[:], True, True, [comb_t, identF_t], ptt)
                ACT(cT[:], pt[:8, :512], AF.Copy, [ptt], [cT_t])
                for e in range(8):
                    ps, pst = P.ps([1, 2, 3, 4], c_a)
                    MM(ps[:, :512], Sel[:8, e, :], cT[:], True, True, [Sel_t, cT_t], pst)
                    ACT(cwb[:, e, :], ps[:, :512], AF.Copy, [pst], [cwb_t])
            for e in range(NEXP):
                w1v = moe_w1[e].rearrange("(k p) n -> p k n", p=128)
                w3v = moe_w3[e].rearrange("(k p) n -> p k n", p=128)
                w2v = moe_w2[e].rearrange("(c p) n -> p c n", p=128)
                for cb in range(7):
                    w1b, w1bt = w1bb[c_w[0] % 2]
                    w3b, w3bt = w3bb[c_w[0] % 2]
                    w2b, w2bt = w2bb[c_w[0] % 2]
                    c_w[0] += 1
                    for k in range(8):
                        P.dma("pool", w1b[:, k, :], w1v[:, k, cb * 512:(cb + 1) * 512], writes=[w1bt])
                        P.dma("pool", w3b[:, k, :], w3v[:, k, cb * 512:(cb + 1) * 512], writes=[w3bt])
                    for c4 in range(4):
                        P.dma("pool", w2b[:, c4, :], w2v[:, cb * 4 + c4, :], writes=[w2bt])
                    for ti, (t0, n) in enumerate(tiles):
                        h2, h2_t = h2s[ti]
                        cwb, cwb_t = cwbs[ti]
                        yacc, yacc_t = yaccs[ti]
                        gg, gg_t = ggb[c_g[0] % 3]
                        c_g[0] += 1
                        for c4 in range(4):
                            ps1, ps1t = P.ps([1, 2, 3, 4], c_a)
                            for k in range(8):
                                MM(ps1[:, :n], w1b[:, k, c4 * 128:(c4 + 1) * 128], h2[:, k, :], k == 0, k == 7, [w1bt, h2_t], ps1t)
                            ps3, ps3t = P.ps([1, 2, 3, 4], c_a)
                            for k in range(8):
                                MM(ps3[:, :n], w3b[:, k, c4 * 128:(c4 + 1) * 128], h2[:, k, :], k == 0, k == 7, [w3bt, h2_t], ps3t)
                            sl, slt = slb[c_s[0] % 2]
                            tb, tbt = tb_[c_s[0] % 2]
                            c_s[0] += 1
                            ACT(sl[:], ps1[:, :n], AF.Silu, [ps1t], [slt])
                            TTo(tb[:], sl[:], ps3[:, :n], ALU.mult, [slt, ps3t], [tbt])
                            TTo(gg[:, c4, :], tb[:], cwb[:, e, :], ALU.mult, [tbt, cwb_t], [gg_t])
                        for m in range(8):
                            ps, pst = P.ps([5, 6, 7], c_b)
                            for c4 in range(4):
                                MM(ps[:, :n], w2b[:, c4, m * 128:(m + 1) * 128], gg[:, c4, :], c4 == 0, c4 == 3, [w2bt, gg_t], pst)
                            if e == 0 and cb == 0:
                                ACT(yacc[:, m, :], ps[:, :n], AF.Copy, [pst], [yacc_t])
                            else:
                                TTo(yacc[:, m, :], yacc[:, m, :], ps[:, :n], ALU.add, [pst, yacc_t], [yacc_t])
            for ti, (t0, n) in enumerate(tiles):
                yacc, yacc_t = yaccs[ti]
                for k in range(8):
                    P.dma("sp", xT[:, k, :], XM[k, :, t0:t0 + n], reads=[XM2t], writes=[xt])
                post_res(yacc, yacc_t, n, 1, 0, GG_F, xT, xt, sq, sq_t, rstd, rstd_t, 0, yacc, yacc_t)
                for k in range(8):
                    P.dma("sp", outT[k, :, t0 - CTX:t0 - CTX + n], yacc[:, k, :], reads=[yacc_t], writes=[OUTt])
        P.barrier()
    return finish(nc, P, outT)


def finish(nc, P, outT):
    P.barrier()
    return nc, P


def _wk(w):
    w = np.asarray(w, np.float32)
    K, N = w.shape
    return np.ascontiguousarray(w.reshape(K // 128, 128, N).transpose(1, 0, 2))


def host_inputs(inp):
    shared = {}
    shared["vecs"] = vec_layout(inp).pack()
    for l in (0, 1):
        shared["l%d_mod_w" % l] = _wk(inp["l%d_mod_w" % l])
    shared["w0_in"] = _wk(inp["l0_w_in"])
    shared["pool_w"] = np.ascontiguousarray(np.asarray(inp["l0_pool_w"], np.float32).transpose(1, 0, 2))
    wo = np.asarray(inp["l0_w_out"], np.float32)
    shared["w0_outp"] = _wk(wo[:512])
    shared["w0_outa"] = np.ascontiguousarray(wo[512:].reshape(8, 64, 1024).transpose(1, 0, 2))
    shared["ffn_w1"] = _wk(inp["l0_ffn_w1"])
    shared["ffn_w3"] = _wk(inp["l0_ffn_w3"])
    shared["ffn_w2"] = _wk(inp["l0_ffn_w2"])
    cos, sin = _rope_tables()
    shared["rope_cos"], shared["rope_sin"] = np.ascontiguousarray(cos[:64]), np.ascontiguousarray(sin[:64])
    shared["rotT"] = np.ascontiguousarray(_rot_lhsT()[:64, :64])
    shared["identf"] = np.eye(128, dtype=np.float32)
    kk = np.arange(128)[:, None]
    qq = np.arange(128)[None, :]
    m = np.zeros((128, 2, 512), np.float32)
    m[:, 0, :] = np.tile((qq <= kk).astype(np.float32), (1, 4))
    m[:, 1, :] = np.tile((kk <= qq).astype(np.float32), (1, 4))
    shared["masks"] = m
    shared["invc_lat"] = np.ascontiguousarray(np.broadcast_to(_pool_invcnt(LAT)[None], (128, 4, LAT)))
    shared["invc_ctx"] = np.ascontiguousarray(np.broadcast_to(_pool_invcnt(CTX)[None], (128, 4, CTX)))
    shared["w1_in"] = _wk(inp["l1_w_in"])
    shared["w1_out"] = _wk(inp["l1_w_out"])
    sel = np.zeros((16, 16, 128), np.float32)
    for k0 in range(16):
        sel[k0, k0, :] = 1.0
    shared["selc"] = sel
    ii = np.arange(64)[:, None]
    jj = np.arange(64)[None, :]
    gmk = np.zeros((64, 4, 64), np.float32)
    gmk[:, 0, :] = ii > jj
    gmk[:, 1, :] = ii >= jj
    gmk[:, 2, :] = ii < jj
    gmk[:, 3, :] = ii <= jj
    shared["gmask"] = gmk
    trm = np.zeros((64, 2, 64), np.float32)
    trm[:, 0, :] = ii <= jj
    trm[:, 1, :] = ii >= jj
    shared["tri"] = trm
    shared["router"] = _wk(inp["l1_router"])
    for e in range(8):
        shared["moe_w1_%d" % e] = np.ascontiguousarray(np.asarray(inp["l1_moe_w1"][e], np.float32))
        shared["moe_w3_%d" % e] = np.ascontiguousarray(np.asarray(inp["l1_moe_w3"][e], np.float32))
        shared["moe_w2_%d" % e] = np.ascontiguousarray(np.asarray(inp["l1_moe_w2"][e], np.float32))
    maps = []
    x = np.asarray(inp["x"], np.float32)
    ctx = np.asarray(inp["ctx"], np.float32)
    c = np.asarray(inp["c"], np.float32)
    cc = np.asarray(inp["c_ctx"], np.float32)
    for b in range(NCORES):
        d = dict(shared)
        xt = np.concatenate([ctx[b], x[b]], axis=0).T
        d["xin"] = np.ascontiguousarray(xt.reshape(8, 128, TT))
        d["cT"] = np.ascontiguousarray(np.stack([_colvec(c[b]), _colvec(cc)], axis=-1))
        maps.append(d)
    return maps


_CACHE = {}


def kernel(**inputs):
    if "nc" not in _CACHE:
        _CACHE["nc"] = build()[0]
    nc = _CACHE["nc"]
    maps = host_inputs(inputs)
    res = run_bass_kernel_spmd(nc, maps, core_ids=list(range(NCORES)))
    out = np.stack([np.ascontiguousarray(r["outT"].reshape(1024, LAT).T) for r in res.results], axis=0)
    return out.astype(np.float32)
```

```python
import os
import numpy as np
from contextlib import ExitStack
import concourse.bass as bass
import concourse.mybir as mybir
from concourse.bass_utils import run_bass_kernel_spmd

F32 = mybir.dt.float32
BF16 = mybir.dt.bfloat16
AF = mybir.ActivationFunctionType
ALU = mybir.AluOpType
AX = mybir.AxisListType

D = 1024
LAT = 4096
CTX = 256
TT = LAT + CTX
EPS = 1e-6
NCORES = 8


class Tr:
    __slots__ = ("w", "r", "x")

    def __init__(self, x=False):
        self.w = None
        self.r = {}
        self.x = x


class Prog:
    NDMA = 24

    def __init__(self, nc):
        self.nc = nc
        self.es = ExitStack()
        self.eng = {"pe": nc.tensor, "act": nc.scalar, "dve": nc.vector, "pool": nc.gpsimd, "sp": nc.sync}
        self.semh = {}
        for k in self.eng:
            self.semh[k] = self.es.enter_context(nc.semaphore("s_" + k))
        self.cnt = {k: 0 for k in self.eng}
        self.epoch = {k: 0 for k in self.eng}
        self.ekey = {k: k for k in self.eng}
        self.hist = []
        self.waited = {k: {} for k in self.eng}
        self.dcnt = [0] * self.NDMA
        self.dnext = 0
        for i in range(self.NDMA):
            self.semh["d%d" % i] = self.es.enter_context(nc.semaphore("sd%d" % i))
        self.nps = 0
        self.psl = []
        for i in range(8):
            t = self.es.enter_context(nc.psum_tensor("ps%d" % i, [128, 512], F32))
            self.psl.append((t, Tr(True)))
        self.n_ins = 0

    def ps(self, banks, ctr):
        t = self.psl[banks[ctr[0] % len(banks)]]
        ctr[0] += 1
        return t

    def _wait(self, e, deps):
        best = {}
        for d in deps:
            if d is None:
                continue
            k, v = d
            if v > best.get(k, 0):
                best[k] = v
        for k, v in best.items():
            if e == "pe" and (k == "pe" or k.startswith("pe#")):
                continue
            if self.waited[e].get(k, 0) >= v:
                continue
            self.eng[e].wait_ge(self.semh[k], v)
            self.waited[e][k] = v

    @staticmethod
    def _deps(reads, writes):
        deps = []
        for t in reads:
            if t.w is not None:
                deps.append(t.w)
            if t.x:
                deps.extend(t.r.items())
        for t in writes:
            if t.w is not None:
                deps.append(t.w)
            deps.extend(t.r.items())
        return deps

    @staticmethod
    def _mark(me, reads, writes):
        k, v = me
        for t in reads:
            if t.r.get(k, 0) < v:
                t.r[k] = v
        for t in writes:
            t.w = me
            t.r = {}

    EPOCH = 12000

    def op(self, e, fn, reads=(), writes=()):
        if self.cnt[e] >= self.EPOCH:
            self.epoch[e] += 1
            self.ekey[e] = "%s#%d" % (e, self.epoch[e])
            self.semh[self.ekey[e]] = self.es.enter_context(self.nc.semaphore("s_%s_%d" % (e, self.epoch[e])))
            self.cnt[e] = 0
        self._wait(e, self._deps(reads, writes))
        ins = fn(self.eng[e])
        self.cnt[e] += 1
        key = self.ekey[e]
        ins.then_inc(self.semh[key], 1)
        self._mark((key, self.cnt[e]), reads, writes)
        self.n_ins += 1
        return ins

    def dma(self, q, out, in_, reads=(), writes=()):
        slot = self.dnext
        self.dnext = (slot + 1) % self.NDMA
        deps = self._deps(reads, writes)
        key = "d%d" % slot
        if self.dcnt[slot] > 0:
            deps.append((key, 16 * self.dcnt[slot]))
        self._wait(q, deps)
        ins = self.eng[q].dma_start(out=out, in_=in_)
        self.dcnt[slot] += 1
        ins.then_inc(self.semh[key], 16)
        self._mark((key, 16 * self.dcnt[slot]), reads, writes)
        self.n_ins += 1
        return ins

    def barrier(self):
        deps = [(self.ekey[k], self.cnt[k]) for k in self.eng if self.cnt[k] > 0]
        deps += [("d%d" % i, 16 * self.dcnt[i]) for i in range(self.NDMA) if self.dcnt[i] > 0]
        for e in self.eng:
            best = {}
            for k, v in deps:
                best[k] = v
            for k, v in best.items():
                if self.waited[e].get(k, 0) >= v:
                    continue
                self.eng[e].wait_ge(self.semh[k], v)
                self.waited[e][k] = v
        for (t, tr) in self.psl:
            tr.w = None
            tr.r = {}


def _rope_tables():
    half = 32
    inv = 10000.0 ** (-np.arange(0, half, 2, dtype=np.float32) / half)
    t = np.arange(LAT)
    rows = (t // 64).astype(np.float32)
    cols = (t % 64).astype(np.float32)
    cos = np.zeros((64, LAT), np.float32)
    sin = np.zeros((64, LAT), np.float32)
    for i in range(64):
        pos = rows if i < 32 else cols
        ang = pos * inv[i % 16]
        cos[i] = np.cos(ang)
        sin[i] = np.sin(ang)
    cos = np.concatenate([cos, cos], 0)
    sin = np.concatenate([sin, sin], 0)
    return cos, sin


def _rot_lhsT():
    R = np.zeros((128, 128), np.float32)
    for hb in (0, 64):
        for blk in (0, 32):
            for i in range(16):
                R[hb + blk + i, hb + blk + i + 16] = -1.0
                R[hb + blk + i + 16, hb + blk + i] = 1.0
    return np.ascontiguousarray(R.T)


def _pool_invcnt(L):
    t = np.arange(L)
    out = np.zeros((4, L), np.float32)
    for gi, w in enumerate((2, 4, 8, 16)):
        lo = np.clip(t - w // 2, 0, L)
        hi = np.clip(t + w // 2, 0, L)
        out[gi] = 1.0 / (hi - lo).astype(np.float32)
    return out


def _colvec(v):
    return np.ascontiguousarray(np.asarray(v, np.float32).reshape(-1, 128).T)


class VecPack:
    def __init__(self):
        self.cols = []
        self.off = {}
        self.n = 0

    def add(self, name, arr):
        arr = np.asarray(arr, np.float32)
        assert arr.shape[0] == 128
        self.off[name] = self.n
        self.cols.append(arr)
        self.n += arr.shape[1]

    def pack(self):
        return np.ascontiguousarray(np.concatenate(self.cols, axis=1))


_VEC_INPUTS = ("l0_mix_pre", "l0_mix_post", "l0_ffn_pre", "l0_ffn_post", "l0_mod_b", "l0_mod_w",
               "l1_mix_pre", "l1_mix_post", "l1_ffn_pre", "l1_ffn_post", "l1_mod_b", "l1_mod_w")


def vec_layout(inp=None):
    z = lambda *s: np.zeros(s, np.float32)
    g = (lambda k, shp: np.asarray(inp[k], np.float32)) if inp is not None else (lambda k, shp: z(*shp))
    vp = VecPack()
    for l in (0, 1):
        for nm in ("mix_pre", "mix_post", "ffn_pre", "ffn_post"):
            vp.add("l%d_%s" % (l, nm), _colvec(g("l%d_%s" % (l, nm), (1024,))))
        vp.add("l%d_mod_b" % l, _colvec(g("l%d_mod_b" % l, (6144,))))
    vp.add("pool_scale", _colvec(g("l0_pool_scale", (512,))))
    sk = np.asarray(g("l0_sinks", (8,)), np.float32).reshape(1, 8)
    vp.add("sinks", np.broadcast_to(sk, (128, 8)))
    cw = g("l1_conv_w", (5, 3072))
    vp.add("conv_w", np.concatenate([_colvec(cw[j]) for j in range(5)], axis=1))
    vp.add("out_norm", np.asarray(g("l1_out_norm", (128,)), np.float32).reshape(128, 1))
    al = g("l1_a_log", (2, 8)).reshape(1, 16)
    db = g("l1_dt_bias", (2, 8)).reshape(1, 16)
    vp.add("a_log", np.broadcast_to(al, (128, 16)))
    vp.add("dt_bias", np.broadcast_to(db, (128, 16)))
    return vp


TILES512 = [(0, 256)] + [(256 + 512 * i, 512) for i in range(8)]
TILES256 = [(256 * i, 256) for i in range(17)]
NBLK = TT // 128


def build(upto="all", dbg=()):
    nc = bass.Bass("TRN2", target_bir_lowering=False)
    P = Prog(nc)
    es = P.es
    VO = vec_layout(None).off
    NV = vec_layout(None).n

    def din(name, shape, dt=F32):
        return nc.dram_tensor(name, list(shape), dt, kind="ExternalInput").ap()

    def dscr(name, shape, dt=F32):
        kind = "ExternalOutput" if name in dbg else "Internal"
        return nc.dram_tensor(name, list(shape), dt, kind=kind).ap()

    xin = din("xin", [8, 128, TT])
    cT = din("cT", [128, 8, 2])
    vecs = din("vecs", [128, NV])
    modw = [din("l%d_mod_w" % l, [128, 8, 6144]) for l in (0, 1)]
    w0_in = din("w0_in", [128, 8, 1280])
    pool_w = din("pool_w", [128, 4, 128])
    w0_outp = din("w0_outp", [128, 4, 1024])
    w0_outa = din("w0_outa", [64, 8, 1024])
    ffn_w1 = din("ffn_w1", [128, 8, 2816])
    ffn_w3 = din("ffn_w3", [128, 8, 2816])
    ffn_w2 = din("ffn_w2", [128, 22, 1024])
    rope_cos = din("rope_cos", [64, LAT])
    rope_sin = din("rope_sin", [64, LAT])
    rotT = din("rotT", [64, 64])
    masks = din("masks", [128, 2, 512])
    invc_lat = din("invc_lat", [128, 4, LAT])
    invc_ctx = din("invc_ctx", [128, 4, CTX])
    identf = din("identf", [128, 128])
    outT = nc.dram_tensor("outT", [8, 128, LAT], F32, kind="ExternalOutput").ap()

    UT = dscr("UT", [4, 128, TT])
    XM = dscr("XM", [8, 128, TT])
    X1 = dscr("X1", [8, 128, TT])

    def sb(name, shape, dt=F32, stack=None):
        t = (stack or es).enter_context(nc.sbuf_tensor(name, list(shape), dt))
        return t, Tr()

    def ACT(out, in_, func, reads, writes, **kw):
        return P.op("act", lambda e: e.activation(out=out, in_=in_, func=func, **kw), reads, writes)

    def TTo(out, a, b, op, reads, writes, eng="dve"):
        return P.op(eng, lambda e: e.tensor_tensor(out=out, in0=a, in1=b, op=op), reads, writes)

    def STT(out, in0, scalar, in1, op0, op1, reads, writes):
        return P.op("dve", lambda e: e.scalar_tensor_tensor(out=out, in0=in0, scalar=scalar, in1=in1, op0=op0, op1=op1),
                    reads, writes)

    def TS(out, in0, s1, s2, op0, op1, reads, writes):
        return P.op("dve", lambda e: e.tensor_scalar(out=out, in0=in0, scalar1=s1, scalar2=s2, op0=op0, op1=op1),
                    reads, writes)

    def CP(out, in_, reads, writes, eng="dve"):
        return P.op(eng, lambda e: e.tensor_copy(out=out, in_=in_), reads, writes)

    def MM(ps, lhsT, rhs, start, stop, reads, pst):
        return P.op("pe", lambda e: e.matmul(ps, lhsT, rhs, start=start, stop=stop), reads, [pst])

    V, Vt = sb("V", [128, NV])
    DV, DVt = sb("DV", [128, 2 * 2 * 48])
    ones_bf, ones_t = sb("ones_bf", [128, 128], BF16)
    ones_f, onesf_t = sb("ones_f", [128, 128], F32)
    epsc, eps_t = sb("epsc", [128, 1])
    identF, identF_t = sb("identF", [128, 128], F32)
    identB, identB_t = sb("identB", [128, 128], BF16)
    P.op("dve", lambda e: e.memset(ones_bf[:], 1.0), writes=[ones_t])
    P.op("dve", lambda e: e.memset(ones_f[:], 1.0), writes=[onesf_t])
    P.op("dve", lambda e: e.memset(epsc[:], EPS), writes=[eps_t])
    P.dma("sp", V[:], vecs, writes=[Vt])
    P.dma("sp", identF[:], identf, writes=[identF_t])
    CP(identB[:], identF[:], [identF_t], [identB_t])

    def dv(l, s, which, k=None):
        o = ((l * 2 + s) * 6 + which) * 8
        return DV[:, o:o + 8] if k is None else DV[:, o + k:o + k + 1]

    GM_M, SH_M, GG_M, GM_F, SH_F, GG_F = range(6)

    with ExitStack() as ph:
        scT, sc_t = sb("scT", [128, 8, 2], F32, ph)
        modT, mod_t = sb("modT", [128, 48, 2], F32, ph)
        wbuf = [sb("modw%d" % i, [128, 8, 1024], F32, ph) for i in range(2)]
        P.dma("sp", scT[:], cT, writes=[sc_t])
        ACT(scT[:], scT[:], AF.Silu, [sc_t], [sc_t])
        for l in (0, 1):
            ps, pst = P.psl[l]
            for j in range(6):
                wb, wbt = wbuf[j % 2]
                P.dma("sp", wb[:], modw[l][:, :, j * 1024:(j + 1) * 1024], writes=[wbt])
                for kc in range(8):
                    col = (j * 8 + kc) * 2
                    for k in range(8):
                        MM(ps[:, col:col + 2], wb[:, k, kc * 128:(kc + 1) * 128], scT[:, k, :], k == 0, k == 7, [wbt, sc_t], pst)
            mb = VO["l%d_mod_b" % l]
            for s in (0, 1):
                TTo(modT[:, :, s], ps[:, 0:96].rearrange("p (j t) -> p j t", t=2)[:, :, s], V[:, mb:mb + 48], ALU.add,
                    [pst, Vt], [mod_t])
                pre = "l%d_" % l
                for (which_gm, which_sh, which_gg, base, npre, npost) in (
                        (GM_M, SH_M, GG_M, 0, "mix_pre", "mix_post"), (GM_F, SH_F, GG_F, 24, "ffn_pre", "ffn_post")):
                    o_pre = VO[pre + npre]
                    o_post = VO[pre + npost]
                    STT(dv(l, s, which_gm), modT[:, base + 8:base + 16, s], 1.0, V[:, o_pre:o_pre + 8], ALU.add, ALU.mult,
                        [mod_t, Vt], [DVt])
                    CP(dv(l, s, which_sh), modT[:, base:base + 8, s], [mod_t], [DVt])
                    TTo(dv(l, s, which_gg), modT[:, base + 16:base + 24, s], V[:, o_post:o_post + 8], ALU.mult,
                        [mod_t, Vt], [DVt])
        P.barrier()

    def rms_rstd(src, src_t, n, sq, sq_t, rstd, rstd_t, bank, nch=8, scale=1.0 / 1024, npart=128):
        ACT(sq[:, :nch, :n], src, AF.Square, [src_t], [sq_t])
        ps, pst = P.psl[bank]
        for k in range(nch):
            MM(ps[:npart, :n], ones_bf[:, :npart], sq[:, k, :n], k == 0, k == nch - 1, [sq_t, ones_t], pst)
        ACT(rstd[:npart, :n], ps[:npart, :n], AF.Sqrt, [pst, eps_t], [rstd_t], scale=scale, bias=epsc[:npart, 0:1])
        P.op("dve", lambda e: e.reciprocal(rstd[:npart, :n], rstd[:npart, :n]), reads=[rstd_t], writes=[rstd_t])

    def norm_mod(xT, x_t, n, l, s, which_gm, which_sh, h, h_t, sq, sq_t, rstd, rstd_t, tmp, tmp_t, bank):
        rms_rstd(xT[:, :, :n], x_t, n, sq, sq_t, rstd, rstd_t, bank)
        for k in range(8):
            TTo(tmp[:, k, :n], xT[:, k, :n], rstd[:, :n], ALU.mult, [x_t, rstd_t], [tmp_t])
        for k in range(8):
            ACT(h[:, k, :n], tmp[:, k, :n], AF.Identity, [tmp_t, DVt], [h_t],
                scale=dv(l, s, which_gm, k), bias=dv(l, s, which_sh, k))

    def post_res(y, y_t, n, l, s, which_gg, xres, xres_t, sq, sq_t, rstd, rstd_t, bank, out, out_t):
        rms_rstd(y[:, :, :n], y_t, n, sq, sq_t, rstd, rstd_t, bank)
        for k in range(8):
            TTo(y[:, k, :n], y[:, k, :n], rstd[:, :n], ALU.mult, [y_t, rstd_t], [y_t])
        for k in range(8):
            STT(out[:, k, :n], y[:, k, :n], dv(l, s, which_gg, k), xres[:, k, :n], ALU.mult, ALU.add,
                [y_t, xres_t, DVt], [out_t])

    if upto == "p0":
        return finish(nc, P, outT)

    with ExitStack() as mix:
        QT, _ = sb("QT", [64, 8, TT], BF16, mix)
        KT, _ = sb("KT", [64, 2, TT], BF16, mix)
        VT, _ = sb("VT", [128, NBLK, 128], BF16, mix)
        QTt = [Tr() for _ in range(NBLK)]
        KTt = [Tr() for _ in range(NBLK)]
        VTt = [Tr() for _ in range(NBLK)]
        POt = [Tr() for _ in range(NBLK)]
        UTt = Tr()
        xin_t = Tr()

        with ExitStack() as ph:
            w_in, w_in_t = sb("w_in", [128, 8, 1280], BF16, ph)
            rotf, rotf_t = sb("rotf", [64, 64], F32, ph)
            rot, rot_t = sb("rot", [64, 64], BF16, ph)
            for k in range(8):
                P.dma("pool", w_in[:, k, :], w0_in[:, k, :], writes=[w_in_t])
            P.dma("sp", rotf[:], rotT, writes=[rotf_t])
            CP(rot[:], rotf[:], [rotf_t], [rot_t])
            xTb = [sb("xT%d" % i, [128, 8, 512], F32, ph) for i in range(2)]
            cosb = [sb("cos%d" % i, [64, 512], F32, ph) for i in range(2)]
            sinb = [sb("sin%d" % i, [64, 512], F32, ph) for i in range(2)]
            sq, sq_t = sb("sq", [128, 8, 512], BF16, ph)
            rstd, rstd_t = sb("rstd", [128, 512], F32, ph)
            tmp, tmp_t = sb("tmp", [128, 8, 512], F32, ph)
            h, h_t = sb("h", [128, 8, 512], BF16, ph)
            ub = [sb("ub%d" % i, [128, 512], F32, ph) for i in range(2)]
            qb = [sb("qb%d" % i, [64, 512], BF16, ph) for i in range(2)]
            t1 = [sb("t1_%d" % i, [64, 512], F32, ph) for i in range(2)]
            t2 = [sb("t2_%d" % i, [64, 512], F32, ph) for i in range(2)]
            c_proj, c_rot, c_v, c_misc = [0], [0], [0], [0]

            def loadA(i):
                t0, n = TILES512[i]
                xT, xt = xTb[i % 2]
                for k in range(8):
                    P.dma("sp", xT[:, k, :n], xin[k, :, t0:t0 + n], reads=[xin_t], writes=[xt])
                if i > 0:
                    l0 = t0 - CTX
                    P.dma("sp", cosb[i % 2][0][:, :n], rope_cos[:, l0:l0 + n], writes=[cosb[i % 2][1]])
                    P.dma("sp", sinb[i % 2][0][:, :n], rope_sin[:, l0:l0 + n], writes=[sinb[i % 2][1]])

            loadA(0)
            NTA = int(os.environ.get('NTA', len(TILES512)))
            for i, (t0, n) in enumerate(TILES512[:NTA]):
                if i + 1 < NTA:
                    loadA(i + 1)
                s = 1 if i == 0 else 0
                blks = list(range(t0 // 128, (t0 + n) // 128))
                xT, xt = xTb[i % 2]
                norm_mod(xT, xt, n, 0, s, GM_M, SH_M, h, h_t, sq, sq_t, rstd, rstd_t, tmp, tmp_t, 0)
                for m in range(0 if os.environ.get('NOU') else 4):
                    ps, pst = P.ps([1, 2, 3], c_proj)
                    for k in range(8):
                        MM(ps[:, :n], w_in[:, k, m * 128:(m + 1) * 128], h[:, k, :n], k == 0, k == 7, [w_in_t, h_t], pst)
                    u, ut = ub[c_misc[0] % 2]
                    c_misc[0] += 1
                    ACT(u[:, :n], ps[:, :n], AF.Copy, [pst], [ut])
                    P.dma("sp", UT[m, :, t0:t0 + n], u[:, :n], reads=[ut], writes=[UTt])
                for hh in range(int(os.environ.get('NHH', 10))):
                    ps, pst = P.ps([1, 2, 3], c_proj)
                    c0 = 512 + hh * 64
                    for k in range(8):
                        MM(ps[:64, :n], w_in[:, k, c0:c0 + 64], h[:, k, :n], k == 0, k == 7, [w_in_t, h_t], pst)
                    dst = QT[:, hh, t0:t0 + n] if hh < 8 else KT[:, hh - 8, t0:t0 + n]
                    dst_t = [QTt[b] for b in blks] if hh < 8 else [KTt[b] for b in blks]
                    if i == 0 or os.environ.get('NOROPE'):
                        ACT(dst, ps[:64, :n], AF.Copy, [pst], dst_t)
                    else:
                        q, qt = qb[c_misc[0] % 2]
                        a1, a1t = t1[c_misc[0] % 2]
                        a2, a2t = t2[c_misc[0] % 2]
                        c_misc[0] += 1
                        cs, cst = cosb[i % 2]
                        sn, snt = sinb[i % 2]
                        ACT(q[:, :n], ps[:64, :n], AF.Copy, [pst], [qt])
                        pr, prt = P.ps([4, 5], c_rot)
                        MM(pr[:64, :n], rot[:], q[:, :n], True, True, [rot_t, qt], prt)
                        if os.environ.get('ROPEV') == '1':
                            TTo(a1[:, :n], q[:, :n], cs[:, :n], ALU.mult, [qt, cst], [a1t])
                        else:
                            TTo(a1[:, :n], ps[:64, :n], cs[:, :n], ALU.mult, [pst, cst], [a1t])
                        TTo(a2[:, :n], pr[:64, :n], sn[:, :n], ALU.mult, [prt, snt], [a2t])
                        TTo(dst, a1[:, :n], a2[:, :n], ALU.add, [a1t, a2t], dst_t)
                for b in range(0 if os.environ.get('NOV') else n // 128):
                    ps, pst = P.ps([6, 7], c_v)
                    for k in range(8):
                        MM(ps[:, :128], h[:, k, b * 128:(b + 1) * 128], w_in[:, k, 1152:1280], k == 0, k == 7, [w_in_t, h_t], pst)
                    ACT(VT[:, blks[b], :], ps[:, :128], AF.Copy, [pst], [VTt[blks[b]]])
            P.barrier()
        if upto == "A":
            dq = dscr("dQT", [64, 8, TT], BF16)
            dk = dscr("dKT", [64, 2, TT], BF16)
            dvv = dscr("dVT", [128, NBLK, 128], BF16)
            P.dma("sp", dq, QT[:], reads=QTt)
            P.dma("sp", dk, KT[:], reads=KTt)
            P.dma("sp", dvv, VT[:], reads=VTt)
            return finish(nc, P, outT)

        with ExitStack() as ph:
            msk, msk_t = sb("msk", [128, 2, 512], BF16, ph)
            mskf, mskf_t = sb("mskf", [128, 2, 512], F32, ph)
            P.dma("sp", mskf[:], masks, writes=[mskf_t])
            CP(msk[:], mskf[:], [mskf_t], [msk_t])
            esk, esk_t = sb("esk", [64, 2, 512], F32, ph)
            so = VO["sinks"]
            for hh in range(8):
                ACT(esk[:, hh // 4, (hh % 4) * 128:(hh % 4 + 1) * 128], ones_f[:64, :], AF.Exp, [onesf_t, Vt], [esk_t],
                    scale=V[:64, so + hh:so + hh + 1])
            pts = [sb("pt%d" % i, [128, 512], BF16, ph) for i in range(6)]
            dens = [sb("den%d" % i, [64, 512], F32, ph) for i in range(2)]
            c_s, c_pt, c_nd = [0], [0], [0]
            for tb in range(NBLK):
                if tb < 2:
                    kbs = [(0, None), (1, None)]
                else:
                    kbs = [(0, None), (1, None)]
                    if tb > 2:
                        kbs.append((tb - 1, 0))
                    kbs.append((tb, None))
                    if tb < NBLK - 1:
                        kbs.append((tb + 1, 1))
                for j in range(2):
                    ptl = []
                    for (kb, mi) in kbs:
                        ps, pst = P.ps([0, 1, 2, 3, 4, 5], c_s)
                        MM(ps[:, :].rearrange("p (h q) -> p h q", h=4), KT[:, j, kb * 128:(kb + 1) * 128],
                           QT[:, 4 * j:4 * j + 4, tb * 128:(tb + 1) * 128], True, True, [KTt[kb], QTt[tb]], pst)
                        pt, ptt = pts[c_pt[0] % 6]
                        c_pt[0] += 1
                        ACT(pt[:], ps[:], AF.Exp, [pst], [ptt], scale=0.125)
                        if mi is not None:
                            TTo(pt[:], pt[:], msk[:, mi, :], ALU.mult, [ptt, msk_t], [ptt])
                        ptl.append((pt, ptt, kb))
                    psn, psnt = P.psl[6]
                    psd, psdt = P.psl[7]
                    for ii, (pt, ptt, kb) in enumerate(ptl):
                        MM(psn[:64, :], VT[:, kb, j * 64:(j + 1) * 64], pt[:], ii == 0, ii == len(ptl) - 1, [VTt[kb], ptt], psnt)
                    for ii, (pt, ptt, kb) in enumerate(ptl):
                        MM(psd[:64, :], ones_bf[:, :64], pt[:], ii == 0, ii == len(ptl) - 1, [ones_t, ptt], psdt)
                    dn, dnt = dens[c_nd[0] % 2]
                    c_nd[0] += 1
                    TTo(dn[:], psd[:64, :], esk[:, j, :], ALU.add, [psdt, esk_t], [dnt])
                    P.op("dve", lambda e: e.reciprocal(dn[:], dn[:]), reads=[dnt], writes=[dnt])
                    TTo(QT[:, 4 * j:4 * j + 4, tb * 128:(tb + 1) * 128], psn[:64, :].rearrange("p (h q) -> p h q", h=4),
                        dn[:].rearrange("p (h q) -> p h q", h=4), ALU.mult, [psnt, dnt], [QTt[tb]])
            P.barrier()
        if upto == "B":
            dq = dscr("dAO", [64, 8, TT], BF16)
            P.dma("sp", dq, QT[:], reads=QTt)
            return finish(nc, P, outT)

        PO, _ = sb("PO", [128, 4, TT], BF16, mix)
        with ExitStack() as ph:
            pw, pw_t = sb("pw", [128, 4, 128], BF16, ph)
            P.dma("pool", pw[:], pool_w, writes=[pw_t])
            LP = LAT + 16
            U0, U0t = sb("U0", [128, LP], F32, ph)
            Ba, Bat = sb("Ba", [128, LP], F32, ph)
            Bb, Bbt = sb("Bb", [128, LP], F32, ph)
            ivc, ivct = sb("ivc", [128, LAT], F32, ph)
            dl, dlt = sb("dl", [128, LAT], BF16, ph)
            c_p = [0]
            pso = VO["pool_scale"]
            for g, w in enumerate((2, 4, 8, 16)):
                for (s0, L, ivsrc) in ((0, CTX, invc_ctx), (CTX, LAT, invc_lat)):
                    Lt = L + 16
                    P.op("dve", lambda e: e.memset(U0[:, 0:8], 0.0), writes=[U0t])
                    P.op("dve", lambda e: e.memset(U0[:, 8 + L:16 + L], 0.0), writes=[U0t])
                    P.dma("sp", U0[:, 8:8 + L], UT[g, :, s0:s0 + L], reads=[UTt], writes=[U0t])
                    P.dma("sp", ivc[:, :L], ivsrc[:, g, :], writes=[ivct])
                    src, srct = U0, U0t
                    sh = 1
                    ln = Lt
                    bufs = [(Ba, Bat), (Bb, Bbt)]
                    bi = 0
                    while sh < w:
                        dst, dstt = bufs[bi % 2]
                        bi += 1
                        ln = ln - sh
                        TTo(dst[:, :ln], src[:, :ln], src[:, sh:sh + ln], ALU.add, [srct], [dstt])
                        src, srct = dst, dstt
                        sh *= 2
                    o = 8 - w // 2
                    dst, dstt = bufs[bi % 2]
                    TTo(dst[:, :L], src[:, o:o + L], ivc[:, :L], ALU.mult, [srct, ivct], [dstt])
                    TTo(dl[:, :L], dst[:, :L], U0[:, 8:8 + L], ALU.subtract, [dstt, U0t], [dlt])
                    for c0 in range(0, L, 512):
                        n = min(512, L - c0)
                        ps, pst = P.ps([0, 1, 2, 3], c_p)
                        MM(ps[:, :n], pw[:, g, :], dl[:, c0:c0 + n], True, True, [pw_t, dlt], pst)
                        blks = list(range((s0 + c0) // 128, (s0 + c0 + n) // 128))
                        ACT(PO[:, g, s0 + c0:s0 + c0 + n], ps[:, :n], AF.Copy, [pst, Vt], [POt[b] for b in blks],
                            scale=V[:, pso + g:pso + g + 1])
            P.barrier()
        if upto == "C":
            dq = dscr("dPO", [128, 4, TT], BF16)
            P.dma("sp", dq, PO[:], reads=POt)
            return finish(nc, P, outT)

        with ExitStack() as ph:
            wop, wop_t = sb("wop", [128, 4, 1024], BF16, ph)
            woa, woa_t = sb("woa", [64, 8, 1024], BF16, ph)
            for k in range(4):
                P.dma("pool", wop[:, k, :], w0_outp[:, k, :], writes=[wop_t])
            for k in range(8):
                P.dma("pool", woa[:, k, :], w0_outa[:, k, :], writes=[woa_t])
            xTb = [sb("xTd%d" % i, [128, 8, 512], F32, ph) for i in range(1)]
            yb, yb_t = sb("yb", [128, 8, 512], F32, ph)
            sq, sq_t = sb("sqd", [128, 8, 512], BF16, ph)
            rstd, rstd_t = sb("rstdd", [128, 512], F32, ph)
            XMt = Tr()
            c_y = [0]

            def loadD(i):
                t0, n = TILES512[i]
                xT, xt = xTb[0]
                for k in range(8):
                    P.dma("sp", xT[:, k, :n], xin[k, :, t0:t0 + n], reads=[xin_t], writes=[xt])

            for i, (t0, n) in enumerate(TILES512):
                loadD(i)
                s = 1 if i == 0 else 0
                blks = list(range(t0 // 128, (t0 + n) // 128))
                xT, xt = xTb[0]
                rd = [POt[b] for b in blks] + [QTt[b] for b in blks]
                for m in range(8):
                    ps, pst = P.ps([1, 2, 3, 4], c_y)
                    for g in range(4):
                        MM(ps[:, :n], wop[:, g, m * 128:(m + 1) * 128], PO[:, g, t0:t0 + n], g == 0, False, [wop_t] + rd, pst)
                    for hh in range(8):
                        MM(ps[:, :n], woa[:, hh, m * 128:(m + 1) * 128], QT[:, hh, t0:t0 + n], False, hh == 7, [woa_t] + rd, pst)
                    ACT(yb[:, m, :n], ps[:, :n], AF.Copy, [pst], [yb_t])
                xout, xout_t = yb, yb_t
                post_res(yb, yb_t, n, 0, s, GG_M, xT, xt, sq, sq_t, rstd, rstd_t, 0, xout, xout_t)
                for k in range(8):
                    P.dma("sp", XM[k, :, t0:t0 + n], xout[:, k, :n], reads=[xout_t], writes=[XMt])
            P.barrier()
    if upto == "D":
        return finish(nc, P, outT)

    XMt, X1t = Tr(), Tr()
    with ExitStack() as ph:
        w1, w1_t = sb("w1", [128, 8, 2816], BF16, ph)
        w3, w3_t = sb("w3", [128, 8, 2816], BF16, ph)
        for k in range(8):
            P.dma("pool", w1[:, k, :], ffn_w1[:, k, :], writes=[w1_t])
            P.dma("pool", w3[:, k, :], ffn_w3[:, k, :], writes=[w3_t])
        w2b = [sb("w2b%d" % i, [128, 22, 128], BF16, ph) for i in range(2)]
        xTb = [sb("xTe%d" % i, [128, 8, 256], F32, ph) for i in range(2)]
        sq, sq_t = sb("sqe", [128, 8, 256], BF16, ph)
        rstd, rstd_t = sb("rstde", [128, 256], F32, ph)
        tmp, tmp_t = sb("tmpe", [128, 8, 256], F32, ph)
        h2, h2_t = sb("h2e", [128, 8, 256], BF16, ph)
        gg, gg_t = sb("gge", [128, 22, 256], BF16, ph)
        slb = [sb("sle%d" % i, [128, 256], F32, ph) for i in range(2)]
        xout, xout_t = sb("xoe", [128, 8, 256], F32, ph)
        c_a, c_b, c_w, c_s = [0], [0], [0], [0]

        def loadE(i):
            t0, n = TILES256[i]
            xT, xt = xTb[i % 2]
            for k in range(8):
                P.dma("sp", xT[:, k, :n], XM[k, :, t0:t0 + n], reads=[XMt], writes=[xt])

        loadE(0)
        for i, (t0, n) in enumerate(TILES256):
            if i + 1 < len(TILES256):
                loadE(i + 1)
            s = 1 if i == 0 else 0
            xT, xt = xTb[i % 2]
            norm_mod(xT, xt, n, 0, s, GM_F, SH_F, h2, h2_t, sq, sq_t, rstd, rstd_t, tmp, tmp_t, 0)
            for c in range(22):
                ps1, ps1t = P.ps([1, 2, 3, 4], c_a)
                for k in range(8):
                    MM(ps1[:, :n], w1[:, k, c * 128:(c + 1) * 128], h2[:, k, :n], k == 0, k == 7, [w1_t, h2_t], ps1t)
                ps3, ps3t = P.ps([1, 2, 3, 4], c_a)
                for k in range(8):
                    MM(ps3[:, :n], w3[:, k, c * 128:(c + 1) * 128], h2[:, k, :n], k == 0, k == 7, [w3_t, h2_t], ps3t)
                sl, slt = slb[c_s[0] % 2]
                c_s[0] += 1
                ACT(sl[:, :n], ps1[:, :n], AF.Silu, [ps1t], [slt])
                TTo(gg[:, c, :n], sl[:, :n], ps3[:, :n], ALU.mult, [slt, ps3t], [gg_t])
            for m in range(8):
                wb, wbt = w2b[c_w[0] % 2]
                c_w[0] += 1
                P.dma("pool", wb[:], ffn_w2[:, :, m * 128:(m + 1) * 128], writes=[wbt])
                ps, pst = P.ps([5, 6, 7], c_b)
                for c in range(22):
                    MM(ps[:, :n], wb[:, c, :], gg[:, c, :n], c == 0, c == 21, [wbt, gg_t], pst)
                ACT(tmp[:, m, :n], ps[:, :n], AF.Copy, [pst], [tmp_t])
            post_res(tmp, tmp_t, n, 0, s, GG_F, xT, xt, sq, sq_t, rstd, rstd_t, 0, xout, xout_t)
            for k in range(8):
                P.dma("sp", X1[k, :, t0:t0 + n], xout[:, k, :n], reads=[xout_t], writes=[X1t])
        P.barrier()
    if upto == "E":
        return finish(nc, P, outT)


    w1_in = din("w1_in", [128, 8, 4128])
    w1_out = din("w1_out", [128, 8, 1024])
    selc = din("selc", [16, 16, 128])
    gmask = din("gmask", [64, 4, 64])
    tri = din("tri", [64, 2, 64])
    router = din("router", [128, 8, 8])
    moe_w1 = [din("moe_w1_%d" % e, [1024, 3584]) for e in range(8)]
    moe_w3 = [din("moe_w3_%d" % e, [1024, 3584]) for e in range(8)]
    moe_w2 = [din("moe_w2_%d" % e, [3584, 1024]) for e in range(8)]
    QKV = dscr("QKV", [24, 128, TT], BF16)
    Z1 = dscr("Z1", [8, 128, TT])
    OD = dscr("OD", [2, 8, 128, TT])
    QKVt, Z1t, ODt = Tr(), Tr(), Tr()
    NCH = TT // 64

    def norm_mod_v(xs, x_t, n, l, s, which_gm, which_sh, hs, h_t, sq, sq_t, rstd, rstd_t, tmp, tmp_t, bank):
        rms_rstd(xs, x_t, n, sq, sq_t, rstd, rstd_t, bank)
        for k in range(8):
            TTo(tmp[:, k, :n], xs[:, k, :], rstd[:, :n], ALU.mult, [x_t, rstd_t], [tmp_t])
        for k in range(8):
            ACT(hs[:, k, :], tmp[:, k, :n], AF.Identity, [tmp_t, DVt], [h_t],
                scale=dv(l, s, which_gm, k), bias=dv(l, s, which_sh, k))

    with ExitStack() as l1:
        GT, GT_t = sb("GT", [64, NCH, 16], F32, l1)
        BT, BT_t = sb("BT", [64, NCH, 16], F32, l1)
        with ExitStack() as ph:
            w_in, w_in_t = sb("w1in", [128, 8, 4128], BF16, ph)
            for k in range(8):
                P.dma("pool", w_in[:, k, :], w1_in[:, k, :], writes=[w_in_t])
            nea, nea_t = sb("nea", [64, 16], F32, ph)
            alo, dbo, cwo = VO["a_log"], VO["dt_bias"], VO["conv_w"]
            ACT(nea[:], V[:64, alo:alo + 16], AF.Exp, [Vt], [nea_t])
            ACT(nea[:], nea[:], AF.Copy, [nea_t], [nea_t], scale=-1.0)
            eps128, eps128_t = sb("eps128", [128, 1], F32, ph)
            P.op("dve", lambda e: e.memset(eps128[:], 128.0 * EPS), writes=[eps128_t])
            W = 260
            xTb = [sb("xTf%d" % i, [128, 8, W], F32, ph) for i in range(2)]
            sq, sq_t = sb("sqf", [128, 8, W], BF16, ph)
            rstd, rstd_t = sb("rstdf", [128, W], F32, ph)
            tmp, tmp_t = sb("tmpf", [128, 8, W], F32, ph)
            h, h_t = sb("hf", [128, 8, W], BF16, ph)
            pcb = [sb("pc%d" % i, [128, W], F32, ph) for i in range(4)]
            accb = [sb("acc%d" % i, [128, 256], F32, ph) for i in range(4)]
            silb = [sb("sil%d" % i, [128, 256], F32, ph) for i in range(4)]
            sq2b = [sb("sq2%d" % i, [128, 256], BF16, ph) for i in range(4)]
            rs2b = [sb("rs2%d" % i, [128, 256], F32, ph) for i in range(4)]
            obb = [sb("ob%d" % i, [128, 256], BF16, ph) for i in range(4)]
            zbb = [sb("zb%d" % i, [128, 256], F32, ph) for i in range(2)]
            ta, ta_t = sb("ta", [64, 4, 16], F32, ph)
            te, te_t = sb("te", [64, 4, 16], F32, ph)
            c_a, c_n, c_m, c_o, c_z = [0], [0], [0], [0], [0]

            def rngF(i):
                t0 = 256 * i
                lo = 2 if i in (0, 1) else 0
                hi = 258 if i in (0, 16) else 260
                return t0, lo, hi

            def loadF(i):
                t0, lo, hi = rngF(i)
                xT, xt = xTb[i % 2]
                for k in range(8):
                    P.dma("sp", xT[:, k, lo:hi], X1[k, :, t0 - 2 + lo:t0 - 2 + hi], reads=[X1t], writes=[xt])

            NTF = int(os.environ.get('NTF', 17))
            loadF(0)
            for i in range(NTF):
                if i + 1 < NTF:
                    loadF(i + 1)
                t0, lo, hi = rngF(i)
                nv = hi - lo
                s = 1 if i == 0 else 0
                xT, xt = xTb[i % 2]
                norm_mod_v(xT[:, :, lo:hi], xt, nv, 1, s, GM_M, SH_M, h[:, :, lo:hi], h_t, sq, sq_t, rstd, rstd_t, tmp, tmp_t, 0)
                for m in range(32):
                    ps, pst = P.ps([1, 2, 3, 7], c_a)
                    for k in range(8):
                        MM(ps[:, :nv], w_in[:, k, m * 128:(m + 1) * 128], h[:, k, lo:hi], k == 0, k == 7, [w_in_t, h_t], pst)
                    if m >= 24:
                        zb, zbt = zbb[c_z[0] % 2]
                        c_z[0] += 1
                        ACT(zb[:], ps[:, 2 - lo:258 - lo], AF.Copy, [pst], [zbt])
                        P.dma("sp", Z1[m - 24, :, t0:t0 + 256], zb[:], reads=[zbt], writes=[Z1t])
                        continue
                    pc, pct = pcb[c_m[0] % 4]
                    acc, acct = accb[c_m[0] % 4]
                    sil, silt = silb[c_m[0] % 4]
                    sq2, sq2t = sq2b[c_m[0] % 4]
                    rs2, rs2t = rs2b[c_m[0] % 4]
                    c_m[0] += 1
                    if lo > 0:
                        P.op("dve", lambda e: e.memset(pc[:, 0:2], 0.0), writes=[pct])
                    if hi < W:
                        P.op("dve", lambda e: e.memset(pc[:, 258:260], 0.0), writes=[pct])
                    ACT(pc[:, lo:hi], ps[:, :nv], AF.Copy, [pst], [pct])
                    ACT(acc[:], pc[:, 0:256], AF.Copy, [pct, Vt], [acct], scale=V[:, cwo + m:cwo + m + 1])
                    for j in range(1, 5):
                        STT(acc[:], pc[:, j:j + 256], V[:, cwo + j * 24 + m:cwo + j * 24 + m + 1], acc[:], ALU.mult, ALU.add,
                            [pct, Vt, acct], [acct])
                    ACT(sil[:], acc[:], AF.Silu, [acct], [silt])
                    ob, obt = obb[c_o[0] % 4]
                    c_o[0] += 1
                    if m < 16:
                        ACT(sq2[:], sil[:], AF.Square, [silt], [sq2t])
                        pn, pnt = P.ps([4, 5], c_n)
                        MM(pn[:, :256], ones_bf[:], sq2[:], True, True, [ones_t, sq2t], pnt)
                        if m < 8:
                            ACT(rs2[:], pn[:, :256], AF.Sqrt, [pnt, eps128_t], [rs2t], scale=128.0, bias=eps128[:, 0:1])
                        else:
                            ACT(rs2[:], pn[:, :256], AF.Sqrt, [pnt, eps_t], [rs2t], scale=1.0, bias=epsc[:, 0:1])
                        P.op("dve", lambda e: e.reciprocal(rs2[:], rs2[:]), reads=[rs2t], writes=[rs2t])
                        TTo(ob[:], sil[:], rs2[:], ALU.mult, [silt, rs2t], [obt])
                    else:
                        CP(ob[:], sil[:], [silt], [obt])
                    P.dma("sp", QKV[m, :, t0:t0 + 256], ob[:], reads=[obt], writes=[QKVt])
                pab, pabt = P.psl[6]
                for cc in range(4):
                    for k in range(8):
                        MM(pab[:64, cc * 32:(cc + 1) * 32], h[:, k, 2 + cc * 64:2 + (cc + 1) * 64], w_in[:, k, 4096:4128],
                           k == 0, k == 7, [w_in_t, h_t], pabt)
                for cc in range(4):
                    pv = pab[:64, cc * 32:(cc + 1) * 32].rearrange("p (d ab h) -> p d ab h", d=2, ab=2)
                    TTo(ta[:, cc, :].rearrange("p (d h) -> p d h", d=2), pv[:, :, 0, :],
                        V[:64, dbo:dbo + 16].rearrange("p (d h) -> p d h", d=2), ALU.add, [pabt, Vt], [ta_t])
                ACT(te[:], ta[:], AF.Exp, [ta_t], [te_t])
                ACT(ta[:], te[:], AF.Ln, [te_t, onesf_t], [ta_t], bias=ones_f[:64, 0:1])
                for cc in range(4):
                    c = t0 // 64 + cc
                    pv = pab[:64, cc * 32:(cc + 1) * 32].rearrange("p (d ab h) -> p d ab h", d=2, ab=2)
                    TTo(GT[:, c, :], ta[:, cc, :], nea[:], ALU.mult, [ta_t, nea_t], [GT_t])
                    ACT(BT[:, c, :].rearrange("p (d h) -> p d h", d=2), pv[:, :, 1, :], AF.Sigmoid, [pabt], [BT_t])
            P.barrier()
        if upto == "F":
            dg = dscr("dGT", [64, NCH, 16])
            db = dscr("dBT", [64, NCH, 16])
            P.dma("sp", dg, GT[:], reads=[GT_t])
            P.dma("sp", db, BT[:], reads=[BT_t])
            return finish(nc, P, outT)

        with ExitStack() as ph:
            GAMc, GAMc_t = sb("GAMc", [64, NCH, 16], F32, ph)
            GLb, GLb_t = sb("GLb", [128, NCH, 16], F32, ph)
            CD, CD_t = sb("CD", [128, NCH, 16], F32, ph)
            CKD, CKD_t = sb("CKD", [64, NCH, 16], F32, ph)
            CKB, CKB_t = sb("CKB", [64, NCH, 16], F32, ph)
            GAMr, GAMr_t = sb("GAMr", [16, TT], F32, ph)
            NGAMr, NGAMr_t = sb("NGAMr", [16, TT], F32, ph)
            Sel, Sel_t = sb("Sel", [16, 16, 128], F32, ph)
            gm, gm_t = sb("gm", [64, 4, 64], F32, ph)
            trt, trt_t = sb("trt", [64, 2, 64], F32, ph)
            P.dma("sp", Sel[:], selc, writes=[Sel_t])
            P.dma("sp", gm[:], gmask, writes=[gm_t])
            P.dma("sp", trt[:], tri, writes=[trt_t])
            for c0 in range(0, NCH, 32):
                c1 = min(NCH, c0 + 32)
                n = (c1 - c0) * 16
                rhs = GT[:, c0:c1, :]
                psF, psFt = P.psl[0]
                psB, psBt = P.psl[1]
                psL, psLt = P.psl[2]
                MM(psF[:64, :n].rearrange("p (c x) -> p c x", x=16), trt[:, 0, :], rhs, True, True, [trt_t, GT_t], psFt)
                MM(psB[:64, :n].rearrange("p (c x) -> p c x", x=16), trt[:, 1, :], rhs, True, True, [trt_t, GT_t], psBt)
                MM(psL[:, :n].rearrange("p (c x) -> p c x", x=16), ones_f[:64, :], rhs, True, True, [onesf_t, GT_t], psLt)
                ACT(GAMc[:, c0:c1, 0:8], psF[:64, :n].rearrange("p (c x) -> p c x", x=16)[:, :, 0:8], AF.Copy, [psFt], [GAMc_t])
                ACT(GAMc[:, c0:c1, 8:16], psB[:64, :n].rearrange("p (c x) -> p c x", x=16)[:, :, 8:16], AF.Copy, [psBt], [GAMc_t])
                CP(GLb[:, c0:c1, :], psL[:, :n].rearrange("p (c x) -> p c x", x=16), [psLt], [GLb_t])
            ACT(CD[:], GLb[:], AF.Exp, [GLb_t], [CD_t])
            TTo(CKD[:], GLb[:64], GAMc[:], ALU.subtract, [GLb_t, GAMc_t], [CKD_t])
            ACT(CKD[:], CKD[:], AF.Exp, [CKD_t], [CKD_t])
            ACT(CKB[:], GAMc[:], AF.Exp, [GAMc_t], [CKB_t])
            TTo(CKB[:], CKB[:], BT[:], ALU.mult, [CKB_t, BT_t], [CKB_t])
            for c in range(NCH):
                psr, psrt = P.psl[3 + (c // 8) % 2]
                MM(psr[:16, (c % 8) * 64:(c % 8 + 1) * 64], GAMc[:, c, :], identF[:64, :64], True, True, [GAMc_t, identF_t], psrt)
                if c % 8 == 7 or c == NCH - 1:
                    cb = (c // 8) * 8
                    n = (c + 1 - cb) * 64
                    ACT(GAMr[:, cb * 64:cb * 64 + n], psr[:16, :n], AF.Copy, [psrt], [GAMr_t])
                    ACT(NGAMr[:, cb * 64:cb * 64 + n], psr[:16, :n], AF.Copy, [psrt], [NGAMr_t], scale=-1.0)
            P.barrier()

            bankc = [0]

            def alloc(nb=1):
                out = []
                for _ in range(nb):
                    out.append(P.psl[bankc[0] % 8])
                    bankc[0] += 1
                return out

            def b2(t):
                return t[:].rearrange("p a b -> p (a b)")

            def mk(nm, shp, dt):
                return sb("g_" + nm, shp, dt, ph)

            Dm, Dm_t = mk("Dm", [64, 8, 64], F32)
            E, E_t = mk("E", [64, 8, 64], F32)
            ELs, ELs_t = mk("ELs", [64, 8, 64], F32)
            ELi, ELi_t = mk("ELi", [64, 8, 64], F32)
            ELb, ELb_t = mk("ELb", [64, 8, 64], F32)
            bfn = {}
            for nm in ("M", "Y", "QKm", "QKmT", "Xa", "Xb", "Ya", "Yb", "Pa", "Pb"):
                bfn[nm] = mk(nm, [64, 8, 64], BF16)
            Kbg, Kbg_t = mk("Kbg", [64, 8, 128], BF16)
            Kd, Kd_t = mk("Kd", [64, 8, 128], BF16)
            Vb, Vb_t = mk("Vb", [64, 8, 128], BF16)
            vn, vn_t = mk("vn", [64, 8, 128], BF16)
            nWT, nWT_t = mk("nWT", [128, 8, 64], BF16)
            EGe, EGe_t = mk("EGe", [128, 8, 64], F32)
            qg, qg_t = mk("qg", [128, 8, 64], BF16)
            S, S_t = mk("S", [128, 8, 128], F32)
            Sb, Sb_t = mk("Sb", [128, 8, 128], BF16)
            OB, OB_t = mk("OB", [128, 8, 256], F32)
            I8, I8_t = mk("I8", [64, 8, 64], F32)
            gm8, gm8_t = mk("gm8", [64, 4, 8, 64], F32)
            for hh in range(8):
                CP(I8[:, hh, :], identF[:64, :64], [identF_t], [I8_t])
                for mi in range(4):
                    CP(gm8[:, mi, hh, :], gm[:, mi, :], [gm_t], [gm8_t])
            stg = [sb("stg%d" % i, [128, 24, 256], BF16, ph) for i in range(2)]
            idB = identB[:64, :64]
            NEU = 5
            for d in range(2):
                d8 = d * 8
                order = list(range(NCH)) if d == 0 else [3, 2, 1, 0] + list(range(NCH - 1, 3, -1))
                order = order[:int(os.environ.get('NSTEP', len(order)))]
                groups = []
                for c in order:
                    if not groups or groups[-1] != c // 4:
                        groups.append(c // 4)
                gpos = {}

                def loadG(gi):
                    g4 = groups[gi]
                    st, stt = stg[gi % 2]
                    for m in range(24):
                        P.dma("sp", st[:, m, :], QKV[m, :, g4 * 256:(g4 + 1) * 256], reads=[QKVt], writes=[stt])
                    gpos[g4] = gi

                P.op("dve", lambda e: e.memset(b2(S), 0.0), writes=[S_t])
                P.op("dve", lambda e: e.memset(b2(Sb), 0.0), writes=[Sb_t])
                loadG(0)
                prev_c = None
                for c in order:
                    g4 = c // 4
                    gi = gpos[g4]
                    if prev_c is None or (c // 4 != prev_c // 4):
                        if gi + 1 < len(groups):
                            loadG(gi + 1)
                    prev_c = c
                    st, stt = stg[gi % 2]
                    o64 = (c % 4) * 64
                    tk = slice(c * 64, (c + 1) * 64)
                    is_lat = c >= 4
                    mS, mI = (0, 1) if d == 0 else (2, 3)

                    def qT(hh):
                        return st[:, hh, o64:o64 + 64]

                    def kT(hh):
                        return st[:, 8 + hh, o64:o64 + 64]

                    def vT(hh):
                        return st[:, 16 + hh, o64:o64 + 64]

                    def cs(hh, w=64):
                        return slice(hh * w, (hh + 1) * w)

                    (psD, psDt), = alloc()
                    for hh in range(8):
                        MM(psD[:64, cs(hh)], GAMr[:, tk], Sel[:, d8 + hh, :64], True, False, [GAMr_t, Sel_t], psDt)
                        MM(psD[:64, cs(hh)], Sel[:, d8 + hh, :64], NGAMr[:, tk], False, True, [NGAMr_t, Sel_t], psDt)
                    TS(b2(Dm), psD[:64, :512], 0.0, 0.0, ALU.min, ALU.add, [psDt], [Dm_t])
                    ACT(b2(E), b2(Dm), AF.Exp, [Dm_t], [E_t])
                    TTo(b2(ELs), b2(E), gm8[:, mS].rearrange("p a b -> p (a b)"), ALU.mult, [E_t, gm8_t], [ELs_t])
                    TTo(b2(ELi), b2(E), gm8[:, mI].rearrange("p a b -> p (a b)"), ALU.mult, [E_t, gm8_t], [ELi_t])
                    TTo(ELb[:], ELs[:], BT[:, c, d8:d8 + 8].unsqueeze(2).to_broadcast([64, 8, 64]), ALU.mult, [ELs_t, BT_t], [ELb_t])
                    (psG, psGt), (psQ, psQt) = alloc(2)
                    for hh in range(8):
                        MM(psG[:64, cs(hh)], kT(hh), kT(hh), True, True, [stt], psGt)
                    for hh in range(8):
                        MM(psQ[:64, cs(hh)], qT(hh), kT(hh), True, True, [stt], psQt)
                    M, M_t = bfn["M"]
                    QKm, QKm_t = bfn["QKm"]
                    TTo(b2(M), psG[:64, :512], b2(ELb), ALU.mult, [psGt, ELb_t], [M_t])
                    TTo(b2(QKm), psQ[:64, :512], b2(ELi), ALU.mult, [psQt, ELi_t], [QKm_t])
                    (psY, psYt), (psQT, psQTt) = alloc(2)
                    for hh in range(8):
                        MM(psY[:64, cs(hh)], M[:, hh, :], idB, True, True, [M_t, identB_t], psYt)
                    for hh in range(8):
                        MM(psQT[:64, cs(hh)], QKm[:, hh, :], idB, True, True, [QKm_t, identB_t], psQTt)
                    Y, Y_t = bfn["Y"]
                    Pa, Pa_t = bfn["Pa"]
                    QKmT, QKmT_t = bfn["QKmT"]
                    ACT(b2(Y), psY[:64, :512], AF.Copy, [psYt], [Y_t])
                    STT(b2(Pa), psY[:64, :512], -1.0, b2(I8), ALU.mult, ALU.add, [psYt, I8_t], [Pa_t])
                    ACT(b2(QKmT), psQT[:64, :512], AF.Copy, [psQTt], [QKmT_t])
                    X, X_t = bfn["M"]
                    Yc, Yc_t = bfn["Y"]
                    Pc, Pc_t = bfn["Pa"]
                    for r in range(NEU):
                        xn, yn, pn = ("Xa", "Ya", "Pb") if r % 2 == 0 else ("Xb", "Yb", "Pa")
                        Xn, Xn_t = bfn[xn]
                        (psX, psXt), = alloc()
                        for hh in range(8):
                            MM(psX[:64, cs(hh)], Yc[:, hh, :], X[:, hh, :], True, True, [X_t, Yc_t], psXt)
                        if r < NEU - 1:
                            Yn, Yn_t = bfn[yn]
                            (psYn, psYnt), = alloc()
                            for hh in range(8):
                                MM(psYn[:64, cs(hh)], X[:, hh, :], Yc[:, hh, :], True, True, [X_t, Yc_t], psYnt)
                        ACT(b2(Xn), psX[:64, :512], AF.Copy, [psXt], [Xn_t])
                        if r < NEU - 1:
                            CP(b2(Yn), psYn[:64, :512], [psYnt], [Yn_t])
                            Yc, Yc_t = Yn, Yn_t
                        X, X_t = Xn, Xn_t
                        (psP, psPt), = alloc()
                        for hh in range(8):
                            MM(psP[:64, cs(hh)], X[:, hh, :], Pc[:, hh, :], True, True, [X_t, Pc_t], psPt)
                        Pn, Pn_t = bfn[pn]
                        TTo(b2(Pn), psP[:64, :512], b2(Pc), ALU.add, [psPt, Pc_t], [Pn_t])
                        Pc, Pc_t = Pn, Pn_t
                    kbk = alloc(2)
                    vbk = alloc(2)
                    for hh in range(8):
                        t, tr = kbk[hh // 4]
                        MM(t[:64, cs(hh % 4, 128)], kT(hh), identB[:], True, True, [stt, identB_t], tr)
                    for hh in range(8):
                        t, tr = vbk[hh // 4]
                        MM(t[:64, cs(hh % 4, 128)], vT(hh), identB[:], True, True, [stt, identB_t], tr)
                    for half in range(2):
                        hs = slice(half * 4, half * 4 + 4)
                        cs4 = slice(d8 + half * 4, d8 + half * 4 + 4)
                        t, tr = kbk[half]
                        pk = t[:64, :512].rearrange("p (a b) -> p a b", a=4)
                        TTo(Kbg[:, hs, :], pk, CKB[:, c, cs4].unsqueeze(2).to_broadcast([64, 4, 128]), ALU.mult, [tr, CKB_t], [Kbg_t])
                        TTo(Kd[:, hs, :], pk, CKD[:, c, cs4].unsqueeze(2).to_broadcast([64, 4, 128]), ALU.mult, [tr, CKD_t], [Kd_t])
                        t, tr = vbk[half]
                        pk = t[:64, :512].rearrange("p (a b) -> p a b", a=4)
                        TTo(Vb[:, hs, :], pk, BT[:, c, cs4].unsqueeze(2).to_broadcast([64, 4, 128]), ALU.mult, [tr, BT_t], [Vb_t])
                    (psW, psWt), (pse, pset) = alloc(2)
                    for hh in range(8):
                        MM(psW[:, cs(hh)], Kbg[:, hh, :], Pc[:, hh, :], True, True, [Kbg_t, Pc_t], psWt)
                    for hh in range(8):
                        MM(pse[:, cs(hh)], Sel[:, d8 + hh, :], GAMr[:, tk], True, True, [Sel_t, GAMr_t], pset)
                    ACT(b2(nWT), psW[:, :512], AF.Copy, [psWt], [nWT_t], scale=-1.0)
                    ACT(b2(EGe), pse[:, :512], AF.Exp, [pset], [EGe_t])
                    TTo(qg[:], st[:, 0:8, o64:o64 + 64], EGe[:], ALU.mult, [stt, EGe_t], [qg_t])
                    pvb = alloc(2)
                    for hh in range(8):
                        t, tr = pvb[hh // 4]
                        MM(t[:64, cs(hh % 4, 128)], Pc[:, hh, :], Vb[:, hh, :], True, False, [Pc_t, Vb_t], tr)
                        MM(t[:64, cs(hh % 4, 128)], nWT[:, hh, :], Sb[:, hh, :], False, True, [nWT_t, Sb_t], tr)
                    for half in range(2):
                        hs = slice(half * 4, half * 4 + 4)
                        t, tr = pvb[half]
                        ACT(vn[:, hs, :].rearrange("p a b -> p (a b)"), t[:64, :512], AF.Copy, [tr], [vn_t])
                    if is_lat:
                        (pso, psot), = alloc()
                        for hh in range(8):
                            MM(pso[:, cs(hh)], Sb[:, hh, :], qg[:, hh, :], True, False, [Sb_t, qg_t], psot)
                            MM(pso[:, cs(hh)], vn[:, hh, :], QKmT[:, hh, :], False, True, [vn_t, QKmT_t], psot)
                    pSb = alloc(2)
                    for hh in range(8):
                        t, tr = pSb[hh // 4]
                        MM(t[:, cs(hh % 4, 128)], Kd[:, hh, :], vn[:, hh, :], True, True, [Kd_t, vn_t], tr)
                    if is_lat:
                        CP(OB[:, :, o64:o64 + 64], pso[:, :512].rearrange("p (a b) -> p a b", a=8), [psot], [OB_t])
                    TTo(S[:], S[:], CD[:, c, d8:d8 + 8].unsqueeze(2).to_broadcast([128, 8, 128]), ALU.mult, [S_t, CD_t], [S_t])
                    for half in range(2):
                        hs = slice(half * 4, half * 4 + 4)
                        t, tr = pSb[half]
                        sv = S[:, hs, :].rearrange("p a b -> p (a b)")
                        TTo(sv, sv, t[:, :512], ALU.add, [S_t, tr], [S_t])
                    ACT(b2(Sb), b2(S), AF.Copy, [S_t], [Sb_t])
                    last_in_group = (c % 4 == 3) if d == 0 else (c % 4 == 0)
                    if is_lat and last_in_group:
                        for hh in range(8):
                            P.dma("sp", OD[d, hh, :, g4 * 256:(g4 + 1) * 256], OB[:, hh, :], reads=[OB_t], writes=[ODt])
            P.barrier()
    if upto == "G":
        return finish(nc, P, outT)


    LT512 = [(256 + 512 * i, 512) for i in range(8)]
    with ExitStack() as ph:
        wo, wo_t = sb("wo1", [128, 8, 1024], BF16, ph)
        for k in range(8):
            P.dma("pool", wo[:, k, :], w1_out[:, k, :], writes=[wo_t])
        ono = VO["out_norm"]
        xT, xt = sb("xTh", [128, 8, 512], F32, ph)
        o0b = [sb("o0b%d" % i, [128, 512], F32, ph) for i in range(2)]
        o1b = [sb("o1b%d" % i, [128, 512], F32, ph) for i in range(2)]
        zbh = [sb("zbh%d" % i, [128, 512], F32, ph) for i in range(2)]
        sqh = [sb("sqh%d" % i, [128, 512], BF16, ph) for i in range(2)]
        rsh = [sb("rsh%d" % i, [128, 512], F32, ph) for i in range(2)]
        yg, yg_t = sb("yg", [128, 8, 512], BF16, ph)
        yb, yb_t = sb("ybh", [128, 8, 512], F32, ph)
        sq, sq_t = sb("sqhh", [128, 8, 512], BF16, ph)
        rstd, rstd_t = sb("rstdh", [128, 512], F32, ph)
        c_h, c_n, c_y = [0], [0], [0]
        XM2t = Tr()
        for (t0, n) in LT512:
            for k in range(8):
                P.dma("sp", xT[:, k, :], X1[k, :, t0:t0 + n], reads=[X1t], writes=[xt])
            for hh in range(8):
                o0, o0t = o0b[c_h[0] % 2]
                o1, o1t = o1b[c_h[0] % 2]
                zb, zbt = zbh[c_h[0] % 2]
                sqq, sqqt = sqh[c_h[0] % 2]
                rs, rst = rsh[c_h[0] % 2]
                c_h[0] += 1
                P.dma("sp", o0[:], OD[0, hh, :, t0:t0 + n], reads=[ODt], writes=[o0t])
                P.dma("sp", o1[:], OD[1, hh, :, t0:t0 + n], reads=[ODt], writes=[o1t])
                P.dma("sp", zb[:], Z1[hh, :, t0:t0 + n], reads=[Z1t], writes=[zbt])
                TTo(o0[:], o0[:], o1[:], ALU.add, [o0t, o1t], [o0t])
                ACT(sqq[:], o0[:], AF.Square, [o0t], [sqqt])
                pn, pnt = P.ps([4, 5], c_n)
                MM(pn[:, :n], ones_bf[:], sqq[:], True, True, [ones_t, sqqt], pnt)
                ACT(rs[:], pn[:, :n], AF.Sqrt, [pnt, eps_t], [rst], scale=1.0 / 128, bias=epsc[:, 0:1])
                P.op("dve", lambda e: e.reciprocal(rs[:], rs[:]), reads=[rst], writes=[rst])
                ACT(zb[:], zb[:], AF.Silu, [zbt], [zbt])
                TTo(o0[:], o0[:], rs[:], ALU.mult, [o0t, rst], [o0t])
                STT(yg[:, hh, :], o0[:], V[:, ono:ono + 1], zb[:], ALU.mult, ALU.mult, [o0t, Vt, zbt], [yg_t])
            for m in range(8):
                ps, pst = P.ps([1, 2, 3], c_y)
                for hh in range(8):
                    MM(ps[:, :n], wo[:, hh, m * 128:(m + 1) * 128], yg[:, hh, :], hh == 0, hh == 7, [wo_t, yg_t], pst)
                ACT(yb[:, m, :], ps[:, :n], AF.Copy, [pst], [yb_t])
            post_res(yb, yb_t, n, 1, 0, GG_M, xT, xt, sq, sq_t, rstd, rstd_t, 0, yb, yb_t)
            for k in range(8):
                P.dma("sp", XM[k, :, t0:t0 + n], yb[:, k, :], reads=[yb_t], writes=[XM2t])
        P.barrier()
    if upto == "H":
        return finish(nc, P, outT)

    with ExitStack() as ph:
        NTP = 2
        rt, rt_t = sb("rt", [128, 8, 8], F32, ph)
        P.dma("sp", rt[:], router, writes=[rt_t])
        Sel, Sel_t = sb("Sel2", [16, 16, 128], F32, ph)
        P.dma("sp", Sel[:], selc, writes=[Sel_t])
        xT, xt = sb("xTi", [128, 8, 512], F32, ph)
        tmp, tmp_t = sb("tmpi", [128, 8, 512], F32, ph)
        sq, sq_t = sb("sqi", [128, 8, 512], BF16, ph)
        rstd, rstd_t = sb("rstdi", [128, 512], F32, ph)
        h2s = [sb("h2i%d" % i, [128, 8, 512], BF16, ph) for i in range(NTP)]
        yaccs = [sb("yacc%d" % i, [128, 8, 512], F32, ph) for i in range(NTP)]
        cwbs = [sb("cwb%d" % i, [128, 8, 512], BF16, ph) for i in range(NTP)]
        ggb = [sb("ggi%d" % i, [128, 4, 512], BF16, ph) for i in range(3)]
        w1bb = [sb("w1b%d" % i, [128, 8, 512], BF16, ph) for i in range(2)]
        w3bb = [sb("w3b%d" % i, [128, 8, 512], BF16, ph) for i in range(2)]
        w2bb = [sb("w2bi%d" % i, [128, 4, 1024], BF16, ph) for i in range(2)]
        slb = [sb("sli%d" % i, [128, 512], F32, ph) for i in range(2)]
        tb_ = [sb("tbi%d" % i, [128, 512], F32, ph) for i in range(2)]
        lg, lg_t = sb("lg", [128, 4, 8], F32, ph)
        l2, l2_t = sb("l2", [128, 4, 8], F32, ph)
        mk1, mk1_t = sb("mk1", [128, 4, 8], F32, ph)
        mk2, mk2_t = sb("mk2", [128, 4, 8], F32, ph)
        comb, comb_t = sb("comb", [128, 4, 8], F32, ph)
        m1, m1_t = sb("m1", [128, 4], F32, ph)
        m2, m2_t = sb("m2", [128, 4], F32, ph)
        g1, g1_t = sb("g1", [128, 4], F32, ph)
        g2, g2_t = sb("g2", [128, 4], F32, ph)
        cT, cT_t = sb("cTm", [8, 512], F32, ph)
        c_a, c_b, c_w, c_s, c_g = [0], [0], [0], [0], [0]
        OUTt = Tr()
        NEXP = int(os.environ.get('NEXP', 8))
        NTI = int(os.environ.get('NTI', 8))
        for tp in range(0, NTI, NTP):
            tiles = LT512[tp:tp + NTP]
            for ti, (t0, n) in enumerate(tiles):
                h2, h2_t = h2s[ti]
                cwb, cwb_t = cwbs[ti]
                for k in range(8):
                    P.dma("sp", xT[:, k, :], XM[k, :, t0:t0 + n], reads=[XM2t], writes=[xt])
                rms_rstd(xT[:, :, :], xt, n, sq, sq_t, rstd, rstd_t, 0)
                for k in range(8):
                    TTo(tmp[:, k, :], xT[:, k, :], rstd[:, :], ALU.mult, [xt, rstd_t], [tmp_t])
                for k in range(8):
                    ACT(tmp[:, k, :], tmp[:, k, :], AF.Identity, [tmp_t, DVt], [tmp_t], scale=dv(1, 0, GM_F, k), bias=dv(1, 0, SH_F, k))
                for k in range(8):
                    CP(h2[:, k, :], tmp[:, k, :], [tmp_t], [h2_t])
                pr, prt = P.psl[6]
                for b in range(4):
                    for k in range(8):
                        MM(pr[:, b * 8:(b + 1) * 8], tmp[:, k, b * 128:(b + 1) * 128], rt[:, k, :], k == 0, k == 7, [tmp_t, rt_t], prt)
                ACT(lg[:].rearrange("p b e -> p (b e)"), pr[:, :32], AF.Copy, [prt], [lg_t])
                P.op("dve", lambda e: e.tensor_reduce(out=m1[:], in_=lg[:], axis=AX.X, op=ALU.max), reads=[lg_t], writes=[m1_t])
                for b in range(4):
                    TS(mk1[:, b, :], lg[:, b, :], m1[:, b:b + 1], 0.0, ALU.is_equal, ALU.add, [lg_t, m1_t], [mk1_t])
                STT(l2[:], mk1[:], -1e30, lg[:], ALU.mult, ALU.add, [mk1_t, lg_t], [l2_t])
                P.op("dve", lambda e: e.tensor_reduce(out=m2[:], in_=l2[:], axis=AX.X, op=ALU.max), reads=[l2_t], writes=[m2_t])
                for b in range(4):
                    TS(mk2[:, b, :], l2[:, b, :], m2[:, b:b + 1], 0.0, ALU.is_equal, ALU.add, [l2_t, m2_t], [mk2_t])
                TTo(g1[:], m1[:], m2[:], ALU.subtract, [m1_t, m2_t], [g1_t])
                ACT(g1[:], g1[:], AF.Sigmoid, [g1_t], [g1_t])
                TS(g2[:], g1[:], -1.0, 1.0, ALU.mult, ALU.add, [g1_t], [g2_t])
                for b in range(4):
                    TS(comb[:, b, :], mk1[:, b, :], g1[:, b:b + 1], 0.0, ALU.mult, ALU.add, [mk1_t, g1_t], [comb_t])
                    STT(comb[:, b, :], mk2[:, b, :], g2[:, b:b + 1], comb[:, b, :], ALU.mult, ALU.add, [mk2_t, g2_t, comb_t], [comb_t])
                pt, ptt = P.psl[7]
                for b in range(4):
                    MM(pt[:8, b * 128:(b + 1) * 128], comb[:, b, :], identF[:], True, True, [comb_t, identF_t], ptt)
                ACT(cT[:], pt[:8, :512], AF.Copy, [ptt], [cT_t])
                for e in range(8):
                    ps, pst = P.ps([1, 2, 3, 4], c_a)
                    MM(ps[:, :512], Sel[:8, e, :], cT[:], True, True, [Sel_t, cT_t], pst)
                    ACT(cwb[:, e, :], ps[:, :512], AF.Copy, [pst], [cwb_t])
            for e in range(NEXP):
                w1v = moe_w1[e].rearrange("(k p) n -> p k n", p=128)
                w3v = moe_w3[e].rearrange("(k p) n -> p k n", p=128)
                w2v = moe_w2[e].rearrange("(c p) n -> p c n", p=128)
                for cb in range(7):
                    w1b, w1bt = w1bb[c_w[0] % 2]
                    w3b, w3bt = w3bb[c_w[0] % 2]
                    w2b, w2bt = w2bb[c_w[0] % 2]
                    c_w[0] += 1
                    for k in range(8):
                        P.dma("pool", w1b[:, k, :], w1v[:, k, cb * 512:(cb + 1) * 512], writes=[w1bt])
                        P.dma("pool", w3b[:, k, :], w3v[:, k, cb * 512:(cb + 1) * 512], writes=[w3bt])
                    for c4 in range(4):
                        P.dma("pool", w2b[:, c4, :], w2v[:, cb * 4 + c4, :], writes=[w2bt])
                    for ti, (t0, n) in enumerate(tiles):
                        h2, h2_t = h2s[ti]
                        cwb, cwb_t = cwbs[ti]
                        yacc, yacc_t = yaccs[ti]
                        gg, gg_t = ggb[c_g[0] % 3]
                        c_g[0] += 1
                        for c4 in range(4):
                            ps1, ps1t = P.ps([1, 2, 3, 4], c_a)
                            for k in range(8):
                                MM(ps1[:, :n], w1b[:, k, c4 * 128:(c4 + 1) * 128], h2[:, k, :], k == 0, k == 7, [w1bt, h2_t], ps1t)
                            ps3, ps3t = P.ps([1, 2, 3, 4], c_a)
                            for k in range(8):
                                MM(ps3[:, :n], w3b[:, k, c4 * 128:(c4 + 1) * 128], h2[:, k, :], k == 0, k == 7, [w3bt, h2_t], ps3t)
                            sl, slt = slb[c_s[0] % 2]
                            tb, tbt = tb_[c_s[0] % 2]
                            c_s[0] += 1
                            ACT(sl[:], ps1[:, :n], AF.Silu, [ps1t], [slt])
                            TTo(tb[:], sl[:], ps3[:, :n], ALU.mult, [slt, ps3t], [tbt])
                            TTo(gg[:, c4, :], tb[:], cwb[:, e, :], ALU.mult, [tbt, cwb_t], [gg_t])
                        for m in range(8):
                            ps, pst = P.ps([5, 6, 7], c_b)
                            for c4 in range(4):
                                MM(ps[:, :n], w2b[:, c4, m * 128:(m + 1) * 128], gg[:, c4, :], c4 == 0, c4 == 3, [w2bt, gg_t], pst)
                            if e == 0 and cb == 0:
                                ACT(yacc[:, m, :], ps[:, :n], AF.Copy, [pst], [yacc_t])
                            else:
                                TTo(yacc[:, m, :], yacc[:, m, :], ps[:, :n], ALU.add, [pst, yacc_t], [yacc_t])
            for ti, (t0, n) in enumerate(tiles):
                yacc, yacc_t = yaccs[ti]
                for k in range(8):
                    P.dma("sp", xT[:, k, :], XM[k, :, t0:t0 + n], reads=[XM2t], writes=[xt])
                post_res(yacc, yacc_t, n, 1, 0, GG_F, xT, xt, sq, sq_t, rstd, rstd_t, 0, yacc, yacc_t)
                for k in range(8):
                    P.dma("sp", outT[k, :, t0 - CTX:t0 - CTX + n], yacc[:, k, :], reads=[yacc_t], writes=[OUTt])
        P.barrier()
    return finish(nc, P, outT)


def finish(nc, P, outT):
    P.barrier()
    return nc, P


def _wk(w):
    w = np.asarray(w, np.float32)
    K, N = w.shape
    return np.ascontiguousarray(w.reshape(K // 128, 128, N).transpose(1, 0, 2))


def host_inputs(inp):
    shared = {}
    shared["vecs"] = vec_layout(inp).pack()
    for l in (0, 1):
        shared["l%d_mod_w" % l] = _wk(inp["l%d_mod_w" % l])
    shared["w0_in"] = _wk(inp["l0_w_in"])
    shared["pool_w"] = np.ascontiguousarray(np.asarray(inp["l0_pool_w"], np.float32).transpose(1, 0, 2))
    wo = np.asarray(inp["l0_w_out"], np.float32)
    shared["w0_outp"] = _wk(wo[:512])
    shared["w0_outa"] = np.ascontiguousarray(wo[512:].reshape(8, 64, 1024).transpose(1, 0, 2))
    shared["ffn_w1"] = _wk(inp["l0_ffn_w1"])
    shared["ffn_w3"] = _wk(inp["l0_ffn_w3"])
    shared["ffn_w2"] = _wk(inp["l0_ffn_w2"])
    cos, sin = _rope_tables()
    shared["rope_cos"], shared["rope_sin"] = np.ascontiguousarray(cos[:64]), np.ascontiguousarray(sin[:64])
    shared["rotT"] = np.ascontiguousarray(_rot_lhsT()[:64, :64])
    shared["identf"] = np.eye(128, dtype=np.float32)
    kk = np.arange(128)[:, None]
    qq = np.arange(128)[None, :]
    m = np.zeros((128, 2, 512), np.float32)
    m[:, 0, :] = np.tile((qq <= kk).astype(np.float32), (1, 4))
    m[:, 1, :] = np.tile((kk <= qq).astype(np.float32), (1, 4))
    shared["masks"] = m
    shared["invc_lat"] = np.ascontiguousarray(np.broadcast_to(_pool_invcnt(LAT)[None], (128, 4, LAT)))
    shared["invc_ctx"] = np.ascontiguousarray(np.broadcast_to(_pool_invcnt(CTX)[None], (128, 4, CTX)))
    shared["w1_in"] = _wk(inp["l1_w_in"])
    shared["w1_out"] = _wk(inp["l1_w_out"])
    sel = np.zeros((16, 16, 128), np.float32)
    for k0 in range(16):
        sel[k0, k0, :] = 1.0
    shared["selc"] = sel
    ii = np.arange(64)[:, None]
    jj = np.arange(64)[None, :]
    gmk = np.zeros((64, 4, 64), np.float32)
    gmk[:, 0, :] = ii > jj
    gmk[:, 1, :] = ii >= jj
    gmk[:, 2, :] = ii < jj
    gmk[:, 3, :] = ii <= jj
    shared["gmask"] = gmk
    trm = np.zeros((64, 2, 64), np.float32)
    trm[:, 0, :] = ii <= jj
    trm[:, 1, :] = ii >= jj
    shared["tri"] = trm
    shared["router"] = _wk(inp["l1_router"])
    for e in range(8):
        shared["moe_w1_%d" % e] = np.ascontiguousarray(np.asarray(inp["l1_moe_w1"][e], np.float32))
        shared["moe_w3_%d" % e] = np.ascontiguousarray(np.asarray(inp["l1_moe_w3"][e], np.float32))
        shared["moe_w2_%d" % e] = np.ascontiguousarray(np.asarray(inp["l1_moe_w2"][e], np.float32))
    maps = []
    x = np.asarray(inp["x"], np.float32)
    ctx = np.asarray(inp["ctx"], np.float32)
    c = np.asarray(inp["c"], np.float32)
    cc = np.asarray(inp["c_ctx"], np.float32)
    for b in range(NCORES):
        d = dict(shared)
        xt = np.concatenate([ctx[b], x[b]], axis=0).T
        d["xin"] = np.ascontiguousarray(xt.reshape(8, 128, TT))
        d["cT"] = np.ascontiguousarray(np.stack([_colvec(c[b]), _colvec(cc)], axis=-1))
        maps.append(d)
    return maps


_CACHE = {}


def kernel(**inputs):
    if "nc" not in _CACHE:
        _CACHE["nc"] = build()[0]
    nc = _CACHE["nc"]
    maps = host_inputs(inputs)
    res = run_bass_kernel_spmd(nc, maps, core_ids=list(range(NCORES)))
    out = np.stack([np.ascontiguousarray(r["outT"].reshape(1024, LAT).T) for r in res.results], axis=0)
    return out.astype(np.float32)
```
